# Optimizing a Trainium2 kernel written in Bass

```python
import jax, jax.numpy as jnp
from jax import lax
import numpy as np

D_MODEL = 1024
BATCH = 2
SEQ = 8192
DEPTH = 4

GRID_W = 64
CTX_LEN = 256
N_MIXERS = 3
HEAD_DIM = 64
N_HEADS = D_MODEL // HEAD_DIM
A_KV_HEADS = 4
C_KV_HEADS = 2
Q_BLOCK = 128
WINDOW = 128
ROPE_THETA = 10000.0
RWKV_HEAD = 64
RWKV_HEADS = D_MODEL // RWKV_HEAD
DECAY_LORA = 64
ICLR_LORA = 64
GATE_LORA = 128
N_GROUPS = 4
EXPERTS_PER_GROUP = 8
N_EXPERTS = N_GROUPS * EXPERTS_PER_GROUP
TOP_K = 2
D_EXPERT = 768
MOE_BLOCK = 128
N_A_LAYERS = (DEPTH + 2) // 3
N_B_LAYERS = (DEPTH + 1) // 3
N_C_LAYERS = DEPTH // 3
NORM_EPS = 1e-6
LN_X_EPS = 64e-5
NEG_INF = -1e30

kernel_name = 'hybrid_dit_gqa_rwkv7_swa_hmoe'


def rms_norm(x, g):
    xf = x.astype(jnp.float32)
    y = xf * lax.rsqrt(jnp.mean(xf * xf, axis=-1, keepdims=True) + NORM_EPS)
    return (y * g.astype(jnp.float32)).astype(x.dtype)


def modulate(x, g, shift, scale):
    return rms_norm(x, g) * (1 + scale) + shift


def axial_rope_tables(rows):
    row = jnp.repeat(jnp.arange(rows, dtype=jnp.float32), GRID_W)
    col = jnp.tile(jnp.arange(GRID_W, dtype=jnp.float32), rows)
    axis_dim = HEAD_DIM // 2
    inv_freq = ROPE_THETA ** (-jnp.arange(0, axis_dim, 2, dtype=jnp.float32) / axis_dim)
    a_row = row[:, None] * inv_freq
    a_col = col[:, None] * inv_freq
    ang = jnp.concatenate([a_row, a_row, a_col, a_col], axis=-1)[:, None, :]
    return jnp.cos(ang), jnp.sin(ang)


def apply_rope(x, cos, sin):
    xr = x.reshape(*x.shape[:-1], 2, 2, HEAD_DIM // 4)
    rot = jnp.concatenate([-xr[..., 1:, :], xr[..., :1, :]], axis=-2).reshape(x.shape)
    return x * cos.astype(x.dtype) + rot * sin.astype(x.dtype)


def split_qkv(qkv, n_kv):
    b, s, _ = qkv.shape
    qd, kd = N_HEADS * HEAD_DIM, n_kv * HEAD_DIM
    q = qkv[..., :qd].reshape(b, s, N_HEADS, HEAD_DIM)
    k = qkv[..., qd:qd + kd].reshape(b, s, n_kv, HEAD_DIM)
    v = qkv[..., qd + kd:].reshape(b, s, n_kv, HEAD_DIM)
    return q, k, v


def attend(q, keys, values, mask=None, sink=None):
    s = jnp.einsum('bqhgd,bkhd->bhgqk', q, keys).astype(jnp.float32) * (HEAD_DIM ** -0.5)
    if mask is not None:
        s = jnp.where(mask, s, NEG_INF)
    if sink is not None:
        sink_col = jnp.broadcast_to(sink.astype(jnp.float32)[None, :, :, None, None], s.shape[:-1] + (1,))
        p = jax.nn.softmax(jnp.concatenate([s, sink_col], axis=-1), axis=-1)[..., :-1]
    else:
        p = jax.nn.softmax(s, axis=-1)
    return jnp.einsum('bhgqk,bkhd->bqhgd', p.astype(values.dtype), values)


def dense_gqa_mixer(h_lat, h_ctx, w_qkv, w_o, q_gain, k_gain, cos, sin, with_ctx_out):
    b, s, _ = h_lat.shape
    n_ctx = h_ctx.shape[1]
    grp = N_HEADS // A_KV_HEADS
    q_l, k_l, v_l = split_qkv(h_lat @ w_qkv, A_KV_HEADS)
    q_c, k_c, v_c = split_qkv(h_ctx @ w_qkv, A_KV_HEADS)
    q_l = apply_rope(rms_norm(q_l, q_gain), cos, sin)
    k_l = apply_rope(rms_norm(k_l, k_gain), cos, sin)
    q_c, k_c = rms_norm(q_c, q_gain), rms_norm(k_c, k_gain)
    k_all = jnp.concatenate([k_c, k_l], axis=1)
    v_all = jnp.concatenate([v_c, v_l], axis=1)
    q_blocks = q_l.reshape(b, s // Q_BLOCK, Q_BLOCK, A_KV_HEADS, grp, HEAD_DIM).transpose(1, 0, 2, 3, 4, 5)
    o_l = lax.map(lambda qb: attend(qb, k_all, v_all), q_blocks)
    y_l = o_l.transpose(1, 0, 2, 3, 4, 5).reshape(b, s, D_MODEL) @ w_o
    y_c = None
    if with_ctx_out:
        o_c = attend(q_c.reshape(b, n_ctx, A_KV_HEADS, grp, HEAD_DIM), k_c, v_c)
        y_c = o_c.reshape(b, n_ctx, D_MODEL) @ w_o
    return y_l, y_c


def window_gqa_mixer(h_lat, h_ctx, w_qkv, w_o, sink, cos, sin, with_ctx_out):
    b, s, _ = h_lat.shape
    n_ctx = h_ctx.shape[1]
    grp = N_HEADS // C_KV_HEADS
    span = Q_BLOCK + 2 * WINDOW
    q_l, k_l, v_l = split_qkv(h_lat @ w_qkv, C_KV_HEADS)
    q_c, k_c, v_c = split_qkv(h_ctx @ w_qkv, C_KV_HEADS)
    q_l = apply_rope(q_l, cos, sin).reshape(b, s, C_KV_HEADS, grp, HEAD_DIM)
    k_l = apply_rope(k_l, cos, sin)
    pad = ((0, 0), (WINDOW, WINDOW), (0, 0), (0, 0))
    k_pad, v_pad = jnp.pad(k_l, pad), jnp.pad(v_l, pad)
    sink = sink.reshape(C_KV_HEADS, grp)
    ctx_mask = jnp.ones((Q_BLOCK, n_ctx), dtype=bool)

    def band_block(blk):
        start = blk * Q_BLOCK
        qb = lax.dynamic_slice_in_dim(q_l, start, Q_BLOCK, axis=1)
        kb = lax.dynamic_slice_in_dim(k_pad, start, span, axis=1)
        vb = lax.dynamic_slice_in_dim(v_pad, start, span, axis=1)
        q_pos = start + jnp.arange(Q_BLOCK)
        k_pos = start - WINDOW + jnp.arange(span)
        band = (jnp.abs(q_pos[:, None] - k_pos[None, :]) <= WINDOW) & (k_pos >= 0)[None, :] & (k_pos < s)[None, :]
        keys = jnp.concatenate([k_c, kb], axis=1)
        vals = jnp.concatenate([v_c, vb], axis=1)
        return attend(qb, keys, vals, jnp.concatenate([ctx_mask, band], axis=1), sink)

    o_l = lax.map(band_block, jnp.arange(s // Q_BLOCK))
    y_l = o_l.transpose(1, 0, 2, 3, 4, 5).reshape(b, s, D_MODEL) @ w_o
    y_c = None
    if with_ctx_out:
        o_c = attend(q_c.reshape(b, n_ctx, C_KV_HEADS, grp, HEAD_DIM), k_c, v_c, sink=sink)
        y_c = o_c.reshape(b, n_ctx, D_MODEL) @ w_o
    return y_l, y_c


def token_shift_centred(x):
    prev = jnp.pad(x[:, :-1], ((0, 0), (1, 0), (0, 0)))
    nxt = jnp.pad(x[:, 1:], ((0, 0), (0, 1), (0, 0)))
    return 0.5 * (prev + nxt)


def rwkv_heads(t):
    return t.reshape(*t.shape[:-1], RWKV_HEADS, RWKV_HEAD)


def rwkv7_features(h, mu, w_r, w_k, w_v, w0, w1, w2, a0, a1, a2, g1, g2, k_k, k_a):
    xx = token_shift_centred(h) - h
    xr, xw, xk, xv, xa, xg = [h + xx * mu[i] for i in range(6)]
    r, k, v = xr @ w_r, xk @ w_k, xv @ w_v
    g = jax.nn.sigmoid(xg @ g1) @ g2
    w_log = w0[:, None, None, :] + jnp.einsum('nbsr,nrd->nbsd', jnp.tanh(jnp.einsum('bsd,ndr->nbsr', xw, w1)), w2)
    decay = jnp.exp(-jnp.exp(-jax.nn.softplus(-w_log.astype(jnp.float32)) - 0.5))
    iclr = jax.nn.sigmoid((a0[:, None, None, :] + jnp.einsum('nbsr,nrd->nbsd', jnp.einsum('bsd,ndr->nbsr', xa, a1), a2)).astype(jnp.float32))
    kk = rwkv_heads((k * k_k).astype(jnp.float32))
    kk = (kk / jnp.maximum(jnp.linalg.norm(kk, axis=-1, keepdims=True), 1e-12)).reshape(k.shape)
    k_dir = k[None].astype(jnp.float32) * (1 + (iclr - 1) * k_a.astype(jnp.float32))
    return r, v, g, decay, iclr, kk, k_dir


def rwkv7_scan(state0, r, decay, k, v, kk, iclr, reverse):
    xs = tuple(jnp.moveaxis(rwkv_heads(t.astype(jnp.float32)), 1, 0) for t in (r, decay, k, v, kk, iclr))

    def step(st, inp):
        r_t, w_t, k_t, v_t, kk_t, a_t = inp
        sa = jnp.einsum('bhvk,bhk->bhv', st, -kk_t)
        st = st * w_t[:, :, None, :] + sa[..., None] * (kk_t * a_t)[:, :, None, :] + v_t[..., None] * k_t[:, :, None, :]
        return st, jnp.einsum('bhvk,bhk->bhv', st, r_t)

    s_final, ys = lax.scan(step, state0, xs, reverse=reverse)
    y = jnp.moveaxis(ys, 0, 1)
    return s_final, y.reshape(*y.shape[:2], D_MODEL)


def rwkv7_bidir(feats, state_fwd, state_bwd):
    r, v, _, decay, iclr, kk, k_dir = feats
    s_f, y_f = rwkv7_scan(state_fwd, r, decay[0], k_dir[0], v, kk, iclr[0], reverse=False)
    s_b, y_b = rwkv7_scan(state_bwd, r, decay[1], k_dir[1], v, kk, iclr[1], reverse=True)
    return s_f, s_b, y_f + y_b


def rwkv7_output(y, feats, r_k, ln_w, ln_b, w_o):
    r, v, g, _, _, _, k_dir = feats
    yh = rwkv_heads(y)
    mean = jnp.mean(yh, axis=-1, keepdims=True)
    var = jnp.mean(jnp.square(yh - mean), axis=-1, keepdims=True)
    yn = ((yh - mean) * lax.rsqrt(var + LN_X_EPS)).reshape(y.shape) * ln_w + ln_b
    bonus = jnp.sum(rwkv_heads(r.astype(jnp.float32) * (k_dir[0] + k_dir[1])) * r_k, axis=-1, keepdims=True) * rwkv_heads(v.astype(jnp.float32))
    out = (yn + bonus.reshape(y.shape)).astype(g.dtype) * g
    return out @ w_o


def rwkv7_mixer(h_lat, h_ctx, p, with_ctx_out):
    (mu, w_r, w_k, w_v, w_o, w0, w1, w2, a0, a1, a2, g1, g2, k_k, k_a, r_k, ln_w, ln_b) = p
    f_c = rwkv7_features(h_ctx, mu, w_r, w_k, w_v, w0, w1, w2, a0, a1, a2, g1, g2, k_k, k_a)
    f_l = rwkv7_features(h_lat, mu, w_r, w_k, w_v, w0, w1, w2, a0, a1, a2, g1, g2, k_k, k_a)
    zero = jnp.zeros((h_lat.shape[0], RWKV_HEADS, RWKV_HEAD, RWKV_HEAD), jnp.float32)
    s_f, s_b, y_c = rwkv7_bidir(f_c, zero, zero)
    _, _, y_l = rwkv7_bidir(f_l, s_f, s_b)
    y_lat = rwkv7_output(y_l, f_l, r_k, ln_w, ln_b, w_o).astype(h_lat.dtype)
    y_ctx = rwkv7_output(y_c, f_c, r_k, ln_w, ln_b, w_o).astype(h_ctx.dtype) if with_ctx_out else None
    return y_lat, y_ctx


def hier_moe(h, w_group, b_group, w_expert, b_expert, w1, w3, w2):
    n_tok, d = h.shape
    g_logit = (h @ w_group).astype(jnp.float32) + b_group.astype(jnp.float32)
    g_idx = jnp.argmax(g_logit, axis=-1)
    g_w = jnp.take_along_axis(jax.nn.softmax(g_logit, axis=-1), g_idx[:, None], axis=-1)
    e_logit = ((h @ w_expert).astype(jnp.float32) + b_expert.astype(jnp.float32)).reshape(n_tok, N_GROUPS, EXPERTS_PER_GROUP)
    e_logit = jnp.take_along_axis(e_logit, g_idx[:, None, None], axis=1)[:, 0]
    top_p, top_i = lax.top_k(jax.nn.softmax(e_logit, axis=-1), TOP_K)
    weight = (g_w * top_p / jnp.sum(top_p, axis=-1, keepdims=True)).reshape(-1)
    expert = (g_idx[:, None] * EXPERTS_PER_GROUP + top_i).reshape(-1)
    token = jnp.repeat(jnp.arange(n_tok, dtype=jnp.int32), TOP_K)
    n_assign = n_tok * TOP_K
    n_blocks = -(-n_assign // MOE_BLOCK) + N_EXPERTS
    order = jnp.argsort(expert)
    e_sorted = expert[order]
    counts = jax.ops.segment_sum(jnp.ones_like(expert), expert, num_segments=N_EXPERTS)
    start = jnp.cumsum(counts) - counts
    padded = (counts + MOE_BLOCK - 1) // MOE_BLOCK * MOE_BLOCK
    p_end = jnp.cumsum(padded)
    dest = (p_end - padded)[e_sorted] + jnp.arange(n_assign) - start[e_sorted]
    tok_pad = jnp.full((n_blocks * MOE_BLOCK,), n_tok, jnp.int32).at[dest].set(token[order])
    w_pad = jnp.zeros((n_blocks * MOE_BLOCK,), jnp.float32).at[dest].set(weight[order])
    blk_expert = jnp.minimum(jnp.searchsorted(p_end, jnp.arange(n_blocks) * MOE_BLOCK, side='right'), N_EXPERTS - 1)
    h_pad = jnp.concatenate([h, jnp.zeros((1, d), h.dtype)], axis=0)

    def expert_block(args):
        tok_b, w_b, e = args
        xb = h_pad[tok_b]
        hid = jax.nn.silu(xb @ w1[e]) * (xb @ w3[e])
        return (hid @ w2[e]) * w_b[:, None].astype(h.dtype)

    ys = lax.map(expert_block, (tok_pad.reshape(n_blocks, MOE_BLOCK), w_pad.reshape(n_blocks, MOE_BLOCK), blk_expert))
    out = jnp.zeros_like(h_pad).at[tok_pad].add(ys.reshape(-1, d))
    return out[:n_tok]


def setup_inputs(seed: int = 0) -> dict:
    key = jax.random.key(seed)
    ks = iter(jax.random.split(key, 64))

    def nrm(shape, scale):
        return jax.random.normal(next(ks), shape, jnp.float32) * scale

    def gain(shape):
        return 1.0 + nrm(shape, 0.05)

    d = D_MODEL
    qkv_a = (N_HEADS + 2 * A_KV_HEADS) * HEAD_DIM
    qkv_c = (N_HEADS + 2 * C_KV_HEADS) * HEAD_DIM
    decay_ramp = jnp.linspace(-6.5, -1.5, d, dtype=jnp.float32)
    return {
        'x': nrm((BATCH, SEQ, d), 1.0),
        'c': nrm((BATCH, d), 1.0),
        'ctx': nrm((BATCH, CTX_LEN, d), 1.0),
        'c_ctx': nrm((d,), 1.0),
        'mod_w': nrm((DEPTH, d, 6 * d), 0.5 * d ** -0.5),
        'mod_b': nrm((DEPTH, 6 * d), 0.02),
        'norm1_g': gain((DEPTH, d)),
        'norm2_g': gain((DEPTH, d)),
        'a_w_qkv': nrm((N_A_LAYERS, d, qkv_a), d ** -0.5),
        'a_w_o': nrm((N_A_LAYERS, d, d), d ** -0.5),
        'a_q_gain': gain((N_A_LAYERS, HEAD_DIM)),
        'a_k_gain': gain((N_A_LAYERS, HEAD_DIM)),
        'b_mu': jax.random.uniform(next(ks), (N_B_LAYERS, 6, d), jnp.float32),
        'b_w_r': nrm((N_B_LAYERS, d, d), d ** -0.5),
        'b_w_k': nrm((N_B_LAYERS, d, d), d ** -0.5),
        'b_w_v': nrm((N_B_LAYERS, d, d), d ** -0.5),
        'b_w_o': nrm((N_B_LAYERS, d, d), d ** -0.5),
        'b_decay_w0': decay_ramp + nrm((N_B_LAYERS, 2, d), 0.1),
        'b_decay_w1': nrm((N_B_LAYERS, 2, d, DECAY_LORA), 0.5 * d ** -0.5),
        'b_decay_w2': nrm((N_B_LAYERS, 2, DECAY_LORA, d), 0.5 * DECAY_LORA ** -0.5),
        'b_iclr_a0': nrm((N_B_LAYERS, 2, d), 0.3),
        'b_iclr_a1': nrm((N_B_LAYERS, 2, d, ICLR_LORA), d ** -0.5),
        'b_iclr_a2': nrm((N_B_LAYERS, 2, ICLR_LORA, d), 0.5 * ICLR_LORA ** -0.5),
        'b_gate_g1': nrm((N_B_LAYERS, d, GATE_LORA), d ** -0.5),
        'b_gate_g2': nrm((N_B_LAYERS, GATE_LORA, d), GATE_LORA ** -0.5),
        'b_k_k': 0.85 + nrm((N_B_LAYERS, d), 0.05),
        'b_k_a': gain((N_B_LAYERS, d)),
        'b_r_k': nrm((N_B_LAYERS, RWKV_HEADS, RWKV_HEAD), 0.1),
        'b_ln_w': gain((N_B_LAYERS, d)),
        'b_ln_b': nrm((N_B_LAYERS, d), 0.02),
        'c_w_qkv': nrm((N_C_LAYERS, d, qkv_c), d ** -0.5),
        'c_w_o': nrm((N_C_LAYERS, d, d), d ** -0.5),
        'c_sink': nrm((N_C_LAYERS, N_HEADS), 0.5),
        'moe_w_group': nrm((DEPTH, d, N_GROUPS), d ** -0.5),
        'moe_b_group': nrm((DEPTH, N_GROUPS), 0.01),
        'moe_w_expert': nrm((DEPTH, d, N_EXPERTS), d ** -0.5),
        'moe_b_expert': nrm((DEPTH, N_EXPERTS), 0.01),
        'moe_w1': nrm((DEPTH, N_EXPERTS, d, D_EXPERT), d ** -0.5),
        'moe_w3': nrm((DEPTH, N_EXPERTS, d, D_EXPERT), d ** -0.5),
        'moe_w2': nrm((DEPTH, N_EXPERTS, D_EXPERT, d), D_EXPERT ** -0.5),
        'final_g': gain((d,)),
    }


def reference(x, c, ctx, c_ctx, mod_w, mod_b, norm1_g, norm2_g, a_w_qkv, a_w_o, a_q_gain, a_k_gain,
              b_mu, b_w_r, b_w_k, b_w_v, b_w_o, b_decay_w0, b_decay_w1, b_decay_w2, b_iclr_a0, b_iclr_a1,
              b_iclr_a2, b_gate_g1, b_gate_g2, b_k_k, b_k_a, b_r_k, b_ln_w, b_ln_b, c_w_qkv, c_w_o, c_sink,
              moe_w_group, moe_b_group, moe_w_expert, moe_b_expert, moe_w1, moe_w3, moe_w2, final_g):
    b, s, d = x.shape
    n_ctx = ctx.shape[1]
    rows = s // GRID_W
    cos, sin = axial_rope_tables(rows)
    cond_lat = jax.nn.silu(c)
    cond_ctx = jax.nn.silu(c_ctx)
    x_lat, x_ctx = x, ctx
    for l in range(DEPTH):
        last = l == DEPTH - 1
        m_lat = jnp.split((cond_lat @ mod_w[l] + mod_b[l])[:, None, :], 6, axis=-1)
        m_ctx = jnp.split(cond_ctx @ mod_w[l] + mod_b[l], 6, axis=-1)
        h_lat = modulate(x_lat, norm1_g[l], m_lat[0], m_lat[1])
        h_ctx = modulate(x_ctx, norm1_g[l], m_ctx[0], m_ctx[1])
        kind, j = l % N_MIXERS, l // N_MIXERS
        if kind == 0:
            y_lat, y_ctx = dense_gqa_mixer(h_lat, h_ctx, a_w_qkv[j], a_w_o[j], a_q_gain[j], a_k_gain[j], cos, sin, not last)
        elif kind == 1:
            params = (b_mu[j], b_w_r[j], b_w_k[j], b_w_v[j], b_w_o[j], b_decay_w0[j], b_decay_w1[j], b_decay_w2[j],
                      b_iclr_a0[j], b_iclr_a1[j], b_iclr_a2[j], b_gate_g1[j], b_gate_g2[j], b_k_k[j], b_k_a[j],
                      b_r_k[j], b_ln_w[j], b_ln_b[j])
            y_lat, y_ctx = rwkv7_mixer(h_lat, h_ctx, params, not last)
        else:
            y_lat, y_ctx = window_gqa_mixer(h_lat, h_ctx, c_w_qkv[j], c_w_o[j], c_sink[j], cos, sin, not last)
        x_lat = x_lat + m_lat[2] * y_lat
        h2_lat = modulate(x_lat, norm2_g[l], m_lat[3], m_lat[4])
        moe_p = (moe_w_group[l], moe_b_group[l], moe_w_expert[l], moe_b_expert[l], moe_w1[l], moe_w3[l], moe_w2[l])
        if last:
            f_lat = hier_moe(h2_lat.reshape(-1, d), *moe_p).reshape(b, s, d)
            x_lat = x_lat + m_lat[5] * f_lat
        else:
            x_ctx = x_ctx + m_ctx[2] * y_ctx
            h2_ctx = modulate(x_ctx, norm2_g[l], m_ctx[3], m_ctx[4])
            f_all = hier_moe(jnp.concatenate([h2_ctx.reshape(-1, d), h2_lat.reshape(-1, d)], axis=0), *moe_p)
            x_ctx = x_ctx + m_ctx[5] * f_all[:b * n_ctx].reshape(b, n_ctx, d)
            x_lat = x_lat + m_lat[5] * f_all[b * n_ctx:].reshape(b, s, d)
    return rms_norm(x_lat, final_g)
```

```python
import contextlib
import numpy as np
import concourse.bass as bass
import concourse.mybir as mybir
from concourse.bass_utils import run_bass_kernel_spmd

F32 = mybir.dt.float32
BF16 = mybir.dt.bfloat16
I32 = mybir.dt.int32
AF = mybir.ActivationFunctionType
ALU = mybir.AluOpType
AX = mybir.AxisListType


class Buf:
    __slots__ = ("ap", "w", "r", "name", "psum")

    def __init__(self, ap, name="", psum=False):
        self.psum = psum
        self.ap = ap
        self.w = None
        self.r = []
        self.name = name

    def __getitem__(self, idx):
        return self.ap[idx]


class Ins:
    __slots__ = ("eng", "fn", "deps", "is_dma", "idx", "slot", "target", "need", "cnt", "prev_slot")

    def __init__(self, eng, fn, is_dma):
        self.eng = eng
        self.fn = fn
        self.deps = {}
        self.is_dma = is_dma
        self.need = False
        self.cnt = 0
        self.slot = None
        self.target = 0
        self.prev_slot = None


COMPUTE = ("pe", "dve", "act", "pool")
NSLOT = 8


class Prog:
    def __init__(self, nc):
        self.nc = nc
        self.es = contextlib.ExitStack()
        self.q = {e: [] for e in ("pe", "dve", "act", "pool", "sp")}
        self.dma_count = {e: 0 for e in self.q}
        self.slot_last = {}
        self.n_sb = 0

    def sb(self, shape, dt, name=None):
        self.n_sb += 1
        t = self.es.enter_context(self.nc.sbuf_tensor(name or f"sb{self.n_sb}", list(shape), dt))
        return t

    def ps(self, shape, dt, name=None):
        self.n_sb += 1
        t = self.es.enter_context(self.nc.psum_tensor(name or f"ps{self.n_sb}", list(shape), dt))
        return t

    def buf(self, shape, dt, name=None):
        return Buf(self.sb(shape, dt, name), name or "")

    def pbuf(self, shape, dt, name=None):
        return Buf(self.ps(shape, dt, name), name or "", psum=True)

    def _deps(self, ins, reads, writes):
        cand = []
        for b in reads:
            if b.w is not None:
                cand.append(b.w)
            if b.psum:
                cand.extend(x for x in b.r if x.eng != ins.eng)
        for b in writes:
            if b.w is not None:
                cand.append(b.w)
            cand.extend(b.r)
        for d in cand:
            if d is ins:
                continue
            if d.is_dma:
                ins.deps[("dma", id(d))] = d
            else:
                if d.eng == "pe" and ins.eng == "pe" and not ins.is_dma:
                    continue
                k = ("c", d.eng)
                if k not in ins.deps or ins.deps[k].idx < d.idx:
                    ins.deps[k] = d
        for b in reads:
            b.r.append(ins)
        for b in writes:
            b.w = ins
            b.r = []

    def op(self, eng, name, *args, reads=(), writes=(), **kw):
        fn = (lambda e: getattr(e, name)(*args, **kw))
        ins = Ins(eng, fn, False)
        ins.idx = len(self.q[eng])
        self._deps(ins, reads, writes)
        self.q[eng].append(ins)
        return ins

    def dma(self, eng, out, in_, reads=(), writes=(), **kw):
        ins = Ins(eng, lambda e: e.dma_start(out=out, in_=in_, **kw), True)
        ins.idx = len(self.q[eng])
        k = self.dma_count[eng]
        self.dma_count[eng] += 1
        ins.slot = (eng, k % NSLOT)
        ins.target = 16 * (k // NSLOT + 1)
        ins.prev_slot = self.slot_last.get(ins.slot)
        self.slot_last[ins.slot] = ins
        self._deps(ins, reads, writes)
        self.q[eng].append(ins)
        return ins

    def emit(self, final_waits=()):
        nc = self.nc
        es = self.es
        sem = {e: es.enter_context(nc.semaphore(f"s_{e}")) for e in COMPUTE}
        dsem = {}
        for e in ("sp", "pool", "act"):
            if self.dma_count[e]:
                for s in range(NSLOT):
                    dsem[(e, s)] = es.enter_context(nc.semaphore(f"d_{e}{s}"))
        for e, lst in self.q.items():
            for ins in lst:
                for k, d in ins.deps.items():
                    if k[0] == "c":
                        d.need = True
        for e in COMPUTE:
            c = 0
            for ins in self.q[e]:
                if ins.is_dma:
                    continue
                if ins.need:
                    c += 1
                    ins.cnt = c
        engobj = {"pe": "tensor", "dve": "vector", "act": "scalar", "pool": "gpsimd", "sp": "sync"}
        final = list(final_waits)
        block = es.enter_context(nc.Block())

        def run(e):
            def body(eng):
                waited = {}
                def wait(s, key, v):
                    if waited.get(key, 0) >= v:
                        return
                    waited[key] = v
                    eng.wait_ge(s, v)
                for ins in self.q[e]:
                    if ins.is_dma and ins.prev_slot is not None:
                        wait(dsem[ins.slot], ("d",) + ins.slot, ins.prev_slot.target)
                    for k, d in ins.deps.items():
                        if k[0] == "c":
                            wait(sem[d.eng], ("c", d.eng), d.cnt)
                        else:
                            wait(dsem[d.slot], ("d",) + d.slot, d.target)
                    bi = ins.fn(eng)
                    if ins.is_dma:
                        bi.then_inc(dsem[ins.slot], 16)
                    elif ins.need:
                        bi.then_inc(sem[e], 1)
                if e == "sp":
                    for d in final:
                        wait(dsem[d.slot], ("d",) + d.slot, d.target)
            return body
        for e in ("sp", "pe", "dve", "act", "pool"):
            if not self.q[e] and e != "sp":
                continue
            getattr(block, engobj[e])(run(e))
        es.close()


def mk_ident(p, dt_list=(BF16,)):
    identf = p.buf([128, 128], F32, "identf")
    p.op("pool", "memset", identf[:], 0.0, writes=[identf])
    p.op("pool", "affine_select", out=identf[:], in_=identf[:], pattern=[[-1, 128]],
                                           compare_op=ALU.not_equal, fill=1.0, base=0, channel_multiplier=1,
         reads=[identf], writes=[identf])
    outs = {F32: identf}
    for d in dt_list:
        if d == F32:
            continue
        t = p.buf([128, 128], d, "identb")
        p.op("dve", "tensor_copy", out=t[:], in_=identf[:], reads=[identf], writes=[t])
        outs[d] = t
    return outs


def mk_sel2(p):
    sels = []
    for r in range(2):
        s = p.buf([2, 128], F32, f"sel{r}")
        p.op("pool", "memset", s[:], 1.0, writes=[s])
        p.op("pool", "affine_select", out=s[:], in_=s[:], pattern=[[0, 128]],
                                                         compare_op=ALU.is_equal, fill=0.0, base=-r, channel_multiplier=1,
             reads=[s], writes=[s])
        sels.append(s)
    return sels


def modulation(p, cnd, modw, modb, ncol, ps_small, stage, consumer):
    sc = p.buf([128, 8, 2], F32, "sc")
    p.dma("sp", sc[:], cnd[:, :, :], writes=[sc])
    p.op("act", "activation", out=sc[:], in_=sc[:], func=AF.Silu, reads=[sc], writes=[sc])
    Mc = p.buf([2, 512], F32, "Mc")
    mb = p.buf([2, 512], F32, "mb")
    for c in range(ncol // 512):
        p.dma("sp", mb[:], modb[0:1, c * 512:(c + 1) * 512].to_broadcast([2, 512]), writes=[mb])
        stl = stage if isinstance(stage, list) else [stage]
        nper = 8 // len(stl)
        for si, st in enumerate(stl):
            p.dma("sp", st[:, 0:nper * 512].rearrange("p (k n) -> p k n", n=512),
                  modw[si * nper * 128:(si + 1) * nper * 128, c * 512:(c + 1) * 512].rearrange("(k q) n -> q k n", q=128),
                  writes=[st])
        for k in range(8):
            st = stl[k // nper]
            p.op("pe", "matmul", ps_small[0:2, 0:512], lhsT=sc[:, k, :], rhs=st[:, (k % nper) * 512:(k % nper + 1) * 512],
                 start=(k == 0), stop=(k == 7), reads=[sc, st], writes=[ps_small])
        p.op("dve", "tensor_tensor", out=Mc[:], in0=ps_small[0:2, 0:512], in1=mb[:], op=ALU.add,
             reads=[ps_small, mb], writes=[Mc])
        consumer(c, Mc)


def bcast_chunk(p, sels, src, dst_lat, dst_ctx, col0, ps):
    for r, dst in ((0, dst_lat), (1, dst_ctx)):
        if dst is None:
            continue
        p.op("pe", "matmul", ps[:, 0:512], lhsT=sels[r][:, :], rhs=src[:, :], start=True, stop=True,
             reads=[sels[r], src], writes=[ps])
        p.op("act", "copy", out=dst[:, col0:col0 + 512], in_=ps[:, 0:512], reads=[ps], writes=[dst])


def bcast_rows(p, sels, src, col0, dst_lat, dst_ctx, ps):
    for r, dst in ((0, dst_lat), (1, dst_ctx)):
        if dst is None:
            continue
        for c in range(2):
            p.op("pe", "matmul", ps[:, 0:512], lhsT=sels[r][:, :],
                                                    rhs=src[:, col0 + c * 512: col0 + (c + 1) * 512],
                                                    start=True, stop=True,
                 reads=[sels[r], src], writes=[ps])
            p.op("act", "copy", out=dst[:, c * 512:(c + 1) * 512], in_=ps[:, 0:512],
                 reads=[ps], writes=[dst])


def build_attn(dense):
    NKV = 4 if dense else 2
    KVW = NKV * 64
    QKVC = 1024 + 2 * KVW
    NKT = 64 if dense else 18
    NT = 2 + NKT
    GRP = 16 // NKV
    nc = bass.Bass("TRN2", target_bir_lowering=False)

    def din(name, shape, dt=F32):
        return nc.dram_tensor(name, list(shape), dt, kind="ExternalInput").ap()
    xkv = din("xkv", [NKT * 128, 1024])
    xq = din("xq", [2048, 1024])
    xc = din("xc", [256, 1024])
    cnd = din("cnd", [128, 8, 2])
    modw = din("modw", [1024, 2048])
    modb = din("modb", [1, 2048])
    n1g = din("n1g", [1, 1024])
    wqkv = din("wqkv", [1024, QKVC])
    qg = din("qg", [1, 64])
    kg = din("kg", [1, 64])
    sink = din("sink", [1, 16])
    cos_kv = din("cos_kv", [NKT * 128, 64])
    sin_kv = din("sin_kv", [NKT * 128, 64])
    cos_q = din("cos_q", [2048, 64])
    sin_q = din("sin_q", [2048, 64])
    kvalid = din("kvalid", [128, NT])
    o_lat = nc.dram_tensor("o_lat", [2048, 1024], F32, kind="ExternalOutput").ap()
    o_ctx = nc.dram_tensor("o_ctx", [256, 1024], F32, kind="ExternalOutput").ap()

    p = Prog(nc)
    ident = mk_ident(p)[BF16]
    sels = mk_sel2(p)
    pT = p.pbuf([128, 1024], BF16, "pT")
    pq = p.pbuf([128, 1024], F32, "pq")
    pkv = p.pbuf([128, 512], F32, "pkv")
    pss = [p.pbuf([128, 512], F32, f"pss{i}") for i in range(2)]
    pacc = [p.pbuf([128, 512], F32, f"pacc{i}") for i in range(2)]

    stage = p.buf([128, 4096], F32, "stage")
    g2 = p.buf([2, 1024], F32, "g2")
    p.dma("sp", g2[:], n1g[0:1, :].to_broadcast([2, 1024]), writes=[g2])
    Gvc = p.buf([2, 512], F32, "Gvc")
    Gb = [p.buf([128, 1024], F32, f"Gb{r}") for r in range(2)]
    Sb = [p.buf([128, 1024], F32, f"Sb{r}") for r in range(2)]

    def mod_consumer(c, Mc):
        if c < 2:
            bcast_chunk(p, sels, Mc, Sb[0], Sb[1], c * 512, pq)
        else:
            cc = c - 2
            p.op("dve", "scalar_tensor_tensor", out=Gvc[:], in0=Mc[:], scalar=1.0, in1=g2[:, cc * 512:(cc + 1) * 512],
                 op0=ALU.add, op1=ALU.mult, reads=[Mc, g2], writes=[Gvc])
            bcast_chunk(p, sels, Gvc, Gb[0], Gb[1], cc * 512, pq)
    modulation(p, cnd, modw, modb, 2048, pkv, stage, mod_consumer)

    eps = p.buf([128, 1], F32, "eps")
    p.op("pool", "memset", eps[:], 1e-6, writes=[eps])
    qgb = p.buf([128, 64], F32, "qgb")
    kgb = p.buf([128, 64], F32, "kgb")
    p.dma("sp", qgb[:], qg[0:1, :].to_broadcast([128, 64]), writes=[qgb])
    p.dma("sp", kgb[:], kg[0:1, :].to_broadcast([128, 64]), writes=[kgb])
    esink = p.buf([128, 16], F32, "esink")
    p.dma("sp", esink[:], sink[0:1, :].to_broadcast([128, 16]), writes=[esink])
    p.op("act", "activation", out=esink[:], in_=esink[:], func=AF.Exp, reads=[esink], writes=[esink])
    kval = p.buf([128, NT], F32, "kval")
    p.dma("sp", kval[:], kvalid[:, :], writes=[kval])

    wb = p.buf([128, 8, QKVC], BF16, "wb")
    for k in range(8):
        p.dma("sp", stage[:, 0:QKVC], wqkv[k * 128:(k + 1) * 128, :], writes=[stage])
        p.op("pool", "tensor_copy", out=wb[:, k, :], in_=stage[:, 0:QKVC], reads=[stage], writes=[wb])

    mprev = p.buf([128, 128], BF16, "mprev")
    mnext = p.buf([128, 128], BF16, "mnext")
    if not dense:
        mf = p.buf([128, 128], F32, "mf")
        for (dst, sg) in ((mprev, 1), (mnext, -1)):
            p.op("pool", "memset", mf[:], 1.0, writes=[mf])
            p.op("pool", "affine_select", out=mf[:], in_=mf[:], pattern=[[-sg, 128]], compare_op=ALU.is_ge,
                                                            fill=0.0, base=0, channel_multiplier=sg,
                 reads=[mf], writes=[mf])
            p.op("dve", "tensor_copy", out=dst[:], in_=mf[:], reads=[mf], writes=[dst])

    KT = p.sb([128, NKV // 2, NT * 128], BF16, "KT")
    VE = p.sb([128, NT, NKV, 65], BF16, "VE")
    KTb = [Buf(KT, f"KT{t}") for t in range(NT)]
    VEb = [Buf(VE, f"VE{t}") for t in range(NT)]
    veall = Buf(VE, "VEall")
    ins0 = p.op("pool", "memset", VE[:, :, :, 64:65], 1.0, writes=[veall] + VEb)
    QT = [p.buf([128, 8, 512], BF16, f"QT{i}") for i in range(2)]

    xt = [p.buf([128, 1024], F32, f"xt{i}") for i in range(2)]
    junk = p.buf([128, 1024], F32, "junk")
    ss = p.buf([128, 1], F32, "ss")
    hn = p.buf([128, 1024], F32, "hn")
    hb = p.buf([128, 1024], BF16, "hb")
    hT = p.buf([128, 1024], BF16, "hT")
    qn = p.buf([128, 1024], F32, "qn")
    t1 = p.buf([128, 1024], F32, "t1")
    qr = p.buf([128, 1024], BF16, "qr")
    ssq = p.buf([128, 16], F32, "ssq")
    kn = p.buf([128, KVW], F32, "kn")
    k1 = p.buf([128, KVW], F32, "k1")
    kr = p.buf([128, KVW], BF16, "kr")
    cs = [p.buf([128, 64], F32, f"cs{i}") for i in range(2)]
    sn = [p.buf([128, 64], F32, f"sn{i}") for i in range(2)]
    cnt = [0]

    def headnorm(src_ps, dst, nh, gainb):
        w = nh * 64
        if not dense:
            p.op("act", "copy", out=dst[:, 0:w], in_=src_ps, reads=[srcbuf[0]], writes=[dst])
            return
        p.op("act", "activation", out=junk[:, 0:w], in_=src_ps, func=AF.Square, reads=[srcbuf[0]], writes=[junk])
        p.op("dve", "tensor_reduce", out=ssq[:, 0:nh], in_=junk[:, 0:w].rearrange("p (h d) -> p h d", d=64),
                                              axis=AX.X, op=ALU.add, reads=[junk], writes=[ssq])
        p.op("act", "activation", out=ssq[:, 0:nh], in_=ssq[:, 0:nh], func=AF.Sqrt, bias=eps[:, 0:1], scale=1.0 / 64,
             reads=[ssq, eps], writes=[ssq])
        p.op("dve", "reciprocal", out=ssq[:, 0:nh], in_=ssq[:, 0:nh], reads=[ssq], writes=[ssq])
        p.op("dve", "tensor_tensor", out=dst[:, 0:w].rearrange("p (h d) -> p h d", d=64),
                                              in0=src_ps.rearrange("p (h d) -> p h d", d=64),
                                              in1=ssq[:, 0:nh].unsqueeze(2).to_broadcast([128, nh, 64]), op=ALU.mult,
             reads=[srcbuf[0], ssq], writes=[dst])
        p.op("pool", "tensor_tensor", out=dst[:, 0:w].rearrange("p (h d) -> p h d", d=64),
                                               in0=dst[:, 0:w].rearrange("p (h d) -> p h d", d=64),
                                               in1=gainb[:, :].unsqueeze(1).to_broadcast([128, nh, 64]), op=ALU.mult,
             reads=[dst, gainb], writes=[dst])
    srcbuf = [None]

    def rope(src, tmp, dst, nh, c_t, s_t):
        w = nh * 64
        v3 = lambda t: t[:, 0:w].rearrange("p (h d) -> p h d", d=64)
        v4 = lambda t: t[:, 0:w].rearrange("p (h a t d) -> p (h a) t d", a=2, t=2, d=16)
        s4 = s_t[:, :].rearrange("p (a t d) -> p a t d", a=2, t=2, d=16)
        p.op("dve", "tensor_tensor", out=v3(tmp), in0=v3(src), in1=c_t[:, :].unsqueeze(1).to_broadcast([128, nh, 64]),
                                              op=ALU.mult, reads=[src, c_t], writes=[tmp])
        for tt in range(2):
            p.op("pool", "tensor_tensor", out=junk[:, 0:w].rearrange("p (h a t d) -> p h a t d", a=2, t=2, d=16)[:, :, :, tt, :],
                in0=src[:, 0:w].rearrange("p (h a t d) -> p h a t d", a=2, t=2, d=16)[:, :, :, 1 - tt, :],
                in1=s4[:, :, tt, :].unsqueeze(1).to_broadcast([128, nh, 2, 16]), op=ALU.mult,
                reads=[src, s_t], writes=[junk])
        p.op("dve", "tensor_tensor", out=dst[:, 0:w], in0=tmp[:, 0:w], in1=junk[:, 0:w], op=ALU.add,
             reads=[tmp, junk], writes=[dst])

    def proc_tile(rows_ap, r, need_q, need_kv, tix, qt_buf, qcol, cos_ap, sin_ap):
        i = cnt[0] % 2
        cnt[0] += 1
        x = xt[i]
        p.dma("sp", x[:], rows_ap, writes=[x])
        if r == 0:
            p.dma("sp", cs[i][:], cos_ap, writes=[cs[i]])
            p.dma("sp", sn[i][:], sin_ap, writes=[sn[i]])
        p.op("act", "activation", out=junk[:], in_=x[:], func=AF.Square, accum_out=ss[:], reads=[x], writes=[junk, ss])
        p.op("act", "activation", out=ss[:], in_=ss[:], func=AF.Sqrt, bias=eps[:, 0:1], scale=1.0 / 1024,
             reads=[ss, eps], writes=[ss])
        p.op("dve", "reciprocal", out=ss[:], in_=ss[:], reads=[ss], writes=[ss])
        p.op("dve", "scalar_tensor_tensor", out=hn[:], in0=x[:], scalar=ss[:, 0:1], in1=Gb[r][:],
                                                     op0=ALU.mult, op1=ALU.mult, reads=[x, ss, Gb[r]], writes=[hn])
        p.op("pool", "tensor_tensor", out=hb[:], in0=hn[:], in1=Sb[r][:], op=ALU.add, reads=[hn, Sb[r]], writes=[hb])
        for k in range(8):
            p.op("pe", "transpose", pT[:, k * 128:(k + 1) * 128], hb[:, k * 128:(k + 1) * 128], ident[:],
                 reads=[hb, ident], writes=[pT])
        p.op("act", "copy", out=hT[:], in_=pT[:], reads=[pT], writes=[hT])
        if need_q:
            for c in range(2):
                for k in range(8):
                    p.op("pe", "matmul", pq[:, c * 512:(c + 1) * 512], lhsT=hT[:, k * 128:(k + 1) * 128],
                                                            rhs=wb[:, k, c * 512:(c + 1) * 512], start=(k == 0), stop=(k == 7),
                         reads=[hT, wb], writes=[pq])
            srcbuf[0] = pq
            headnorm(pq[:, :], qn, 16, qgb)
            if r == 0:
                rope(qn, t1, qr, 16, cs[i], sn[i])
            else:
                p.op("dve", "tensor_copy", out=qr[:], in_=qn[:], reads=[qn], writes=[qr])
            for j in range(8):
                p.op("pe", "transpose", pT[:, j * 128:(j + 1) * 128], qr[:, j * 128:(j + 1) * 128], ident[:],
                     reads=[qr, ident], writes=[pT])
            p.op("act", "copy", out=qt_buf[:, :, qcol:qcol + 128], in_=pT[:, :].rearrange("p (j n) -> p j n", n=128),
                 reads=[pT], writes=[qt_buf])
        if need_kv:
            for k in range(8):
                p.op("pe", "matmul", pkv[:, 0:2 * KVW], lhsT=hT[:, k * 128:(k + 1) * 128],
                                                   rhs=wb[:, k, 1024:1024 + 2 * KVW], start=(k == 0), stop=(k == 7),
                     reads=[hT, wb], writes=[pkv])
            srcbuf[0] = pkv
            headnorm(pkv[:, 0:KVW], kn, NKV, kgb)
            if r == 0:
                rope(kn, k1, kr, NKV, cs[i], sn[i])
            else:
                p.op("dve", "tensor_copy", out=kr[:], in_=kn[:], reads=[kn], writes=[kr])
            nch = KVW // 128
            for j in range(nch):
                p.op("pe", "transpose", pT[:, j * 128:(j + 1) * 128], kr[:, j * 128:(j + 1) * 128], ident[:],
                     reads=[kr, ident], writes=[pT])
            p.op("act", "copy", out=KT[:, :, tix * 128:(tix + 1) * 128],
                                         in_=pT[:, 0:nch * 128].rearrange("p (j n) -> p j n", n=128),
                 reads=[pT], writes=[KTb[tix]])
            p.op("act", "copy", out=VE[:, tix, :, 0:64], in_=pkv[:, KVW:2 * KVW].rearrange("p (g d) -> p g d", d=64),
                 reads=[pkv], writes=[VEb[tix]])
            if not dense and r == 0:
                p.op("dve", "tensor_scalar", out=VE[:, tix, :, :], in0=VE[:, tix, :, :], scalar1=kval[:, tix:tix + 1],
                                                      scalar2=None, op0=ALU.mult, reads=[VEb[tix], kval], writes=[VEb[tix]])

    for t in range(2):
        proc_tile(xc[t * 128:(t + 1) * 128, :], 1, False, True, t, None, 0, None, None)
    for t in range(NKT):
        proc_tile(xkv[t * 128:(t + 1) * 128, :], 0, False, True, 2 + t, None, 0,
                  cos_kv[t * 128:(t + 1) * 128, :], sin_kv[t * 128:(t + 1) * 128, :])

    pts = [p.buf([128, 512], BF16, f"pt{i}") for i in range(3)]
    otile = [p.buf([128, 1024], F32, f"ot{i}") for i in range(4)]
    rden = p.buf([128, 1], F32, "rden")
    outs = []
    state = {"mm": 0, "pt": 0, "acc": 0, "blk": 0}

    def attn_block(qtiles, keylists, out_aps, is_ctx):
        b = state["blk"] % 2
        state["blk"] += 1
        qt_buf = QT[b]
        for qi in range(qtiles):
            if is_ctx:
                proc_tile(xc[qi * 128:(qi + 1) * 128, :], 1, True, False, 0, qt_buf, qi * 128, None, None)
            else:
                r0 = out_aps[qi][1]
                proc_tile(xq[r0:r0 + 128, :], 0, True, False, 0, qt_buf, qi * 128,
                          cos_q[r0:r0 + 128, :], sin_q[r0:r0 + 128, :])
        same = all(keylists[qi] == keylists[0] for qi in range(qtiles))
        groups = [list(range(qtiles))] if same else [[qi] for qi in range(qtiles)]
        for h in range(16):
            half, j = h // 8, h % 8
            g = h // GRP
            ki = g % (NKV // 2)
            ps_rows = slice(half * 64, half * 64 + 64)
            pa = pacc[state["acc"] % 2]
            state["acc"] += 1
            for grp in groups:
                kl = keylists[grp[0]]
                c0, c1 = grp[0] * 128, (grp[-1] + 1) * 128
                n = c1 - c0
                for idx, (kt, mask) in enumerate(kl):
                    ps_ = pss[state["mm"] % 2]
                    state["mm"] += 1
                    pt = pts[state["pt"] % 3]
                    state["pt"] += 1
                    p.op("pe", "matmul", ps_[:, 0:n], lhsT=KT[ps_rows, ki, kt * 128:(kt + 1) * 128],
                                                                 rhs=qt_buf[ps_rows, j, c0:c1], start=True, stop=True,
                         reads=[KTb[kt], qt_buf], writes=[ps_])
                    p.op("act", "activation", out=pt[:, 0:n], in_=ps_[:, 0:n], func=AF.Exp, scale=0.125,
                         reads=[ps_], writes=[pt])
                    if mask is not None:
                        p.op("dve", "tensor_tensor", out=pt[:, 0:n], in0=pt[:, 0:n], in1=mask[:, :],
                                                                                op=ALU.mult, reads=[pt, mask], writes=[pt])
                    for qq, qi in enumerate(grp):
                        p.op("pe", "matmul", pa[:, qi * 128:qi * 128 + 65], lhsT=pt[:, qq * 128:(qq + 1) * 128], rhs=VE[:, kt, g, :],
                            start=(idx == 0 and qq == 0 and grp is groups[0]), stop=(idx == len(kl) - 1),
                            reads=[pt, VEb[kt]], writes=[pa])
            for qi in range(qtiles):
                ot = out_aps[qi][2]
                if dense:
                    p.op("dve", "reciprocal", out=rden[:], in_=pa[:, qi * 128 + 64:qi * 128 + 65],
                         reads=[pa], writes=[rden])
                else:
                    p.op("dve", "tensor_tensor", out=rden[:], in0=pa[:, qi * 128 + 64:qi * 128 + 65],
                                                                 in1=esink[:, h:h + 1], op=ALU.add,
                         reads=[pa, esink], writes=[rden])
                    p.op("dve", "reciprocal", out=rden[:], in_=rden[:], reads=[rden], writes=[rden])
                p.op("dve", "tensor_scalar", out=ot[:, h * 64:(h + 1) * 64], in0=pa[:, qi * 128:qi * 128 + 64],
                                                                    scalar1=rden[:, 0:1], scalar2=None, op0=ALU.mult,
                     reads=[pa, rden], writes=[ot])
        for qi in range(qtiles):
            dst, r0, ot = out_aps[qi]
            outs.append(p.dma("sp", dst[r0:r0 + 128, :], ot[:], reads=[ot]))

    attn_block(2, [[(0, None), (1, None)]] * 2, [(o_ctx, 0, otile[0]), (o_ctx, 128, otile[1])], True)
    if dense:
        allk = [(t, None) for t in range(NT)]
        for qb in range(4):
            attn_block(4, [allk] * 4, [(o_lat, (qb * 4 + qi) * 128, otile[qi]) for qi in range(4)], False)
    else:
        for qt in range(16):
            kl = [(0, None), (1, None), (2 + qt, mprev), (3 + qt, None), (4 + qt, mnext)]
            attn_block(1, [kl], [(o_lat, qt * 128, otile[qt % 4])], False)
    p.emit(final_waits=outs)
    return nc


def _rope_tables():
    rows = 128
    row = np.repeat(np.arange(rows, dtype=np.float32), 64)
    col = np.tile(np.arange(64, dtype=np.float32), rows)
    inv = (np.float32(10000.0) ** (-np.arange(0, 32, 2, dtype=np.float32) / np.float32(32))).astype(np.float32)
    ar = row[:, None] * inv
    ac = col[:, None] * inv
    ang = np.concatenate([ar, ar, ac, ac], -1)
    cos = np.cos(ang).astype(np.float32)
    sin = np.sin(ang).astype(np.float32)
    sgn = np.tile(np.concatenate([-np.ones(16, np.float32), np.ones(16, np.float32)]), 2)
    return cos, sin * sgn


def _perm_qkv_cols(w, nkv):
    qcols = []
    for j in range(8):
        qcols += list(range(j * 64, j * 64 + 64)) + list(range((j + 8) * 64, (j + 8) * 64 + 64))
    kcols = []
    for i in range(nkv // 2):
        for g in (i, i + nkv // 2):
            kcols += list(range(1024 + g * 64, 1024 + g * 64 + 64))
    vcols = list(range(1024 + nkv * 64, 1024 + 2 * nkv * 64))
    return np.ascontiguousarray(w[:, qcols + kcols + vcols])


def attn_inputs(dense, x, ctx, c, c_ctx, mod_w, mod_b, n1g, wqkv, qg, kg, sink):
    nkv = 4 if dense else 2
    cos, sin = _rope_tables()
    wp = _perm_qkv_cols(wqkv, nkv)
    maps = []
    for ci in range(8):
        b, qi = ci // 4, ci % 4
        r0 = qi * 2048
        cnd = np.ascontiguousarray(np.stack([c[b], c_ctx], -1).reshape(8, 128, 2).transpose(1, 0, 2))
        if dense:
            xkv = x[b]
            ckv, skv = cos, sin
            kvalid = np.ones((128, 66), np.float32)
        else:
            xkv = np.zeros((18 * 128, 1024), np.float32)
            ckv = np.zeros((18 * 128, 64), np.float32)
            skv = np.zeros((18 * 128, 64), np.float32)
            lo, hi = max(0, r0 - 128), min(8192, r0 + 2048 + 128)
            off = lo - (r0 - 128)
            xkv[off:off + hi - lo] = x[b, lo:hi]
            ckv[off:off + hi - lo] = cos[lo:hi]
            skv[off:off + hi - lo] = sin[lo:hi]
            kvalid = np.ones((128, 20), np.float32)
            if r0 - 128 < 0:
                kvalid[:, 2] = 0
            if r0 + 2048 + 128 > 8192:
                kvalid[:, 19] = 0
        maps.append(dict(
            xkv=np.ascontiguousarray(xkv), xq=np.ascontiguousarray(x[b, r0:r0 + 2048]), xc=np.ascontiguousarray(ctx[b]),
            cnd=cnd, modw=np.ascontiguousarray(mod_w[:, 0:2048]), modb=np.ascontiguousarray(mod_b[None, 0:2048]),
            n1g=np.ascontiguousarray(n1g[None, :]), wqkv=wp, qg=np.ascontiguousarray(qg[None, :]),
            kg=np.ascontiguousarray(kg[None, :]), sink=np.ascontiguousarray(sink[None, :]),
            cos_kv=np.ascontiguousarray(ckv), sin_kv=np.ascontiguousarray(skv),
            cos_q=np.ascontiguousarray(cos[r0:r0 + 2048]), sin_q=np.ascontiguousarray(sin[r0:r0 + 2048]),
            kvalid=kvalid))
    return maps


NTOK = 2304
NTILE = 18


def build_post1(ntile=NTILE, upto=99):
    nc = bass.Bass("TRN2", target_bir_lowering=False)

    def din(name, shape, dt=F32):
        return nc.dram_tensor(name, list(shape), dt, kind="ExternalInput").ap()

    def dout(name, shape, dt=F32):
        return nc.dram_tensor(name, list(shape), dt, kind="ExternalOutput").ap()
    xr = din("xr", [NTOK, 1024])
    o = din("o", [NTOK, 1024])
    cnd = din("cnd", [128, 8, 2])
    modw = din("modw", [1024, 3072])
    modb = din("modb", [1, 3072])
    n2g = din("n2g", [1, 1024])
    wo = din("wo", [1024, 1024])
    wr = din("wr", [1024, 36])
    br = din("br", [1, 36])
    x_mid = dout("x_mid", [NTOK, 1024])
    h2T = dout("h2T", [128, 8, NTOK], BF16)
    gates = dout("gates", [NTOK, 32])

    p = Prog(nc)
    idt = mk_ident(p)
    ident, identf = idt[BF16], idt[F32]
    sels = mk_sel2(p)
    pT = p.pbuf([128, 1024], BF16, "pT")
    pq = p.pbuf([128, 1024], F32, "pq")
    pkv = p.pbuf([128, 512], F32, "pkv")

    stage = p.buf([128, 4096], F32, "stage")
    g2 = p.buf([2, 1024], F32, "g2")
    p.dma("sp", g2[:], n2g[0:1, :].to_broadcast([2, 1024]), writes=[g2])
    Gvc = p.buf([2, 512], F32, "Gvc")
    G1b = [p.buf([128, 1024], F32, f"G1b{r}") for r in range(2)]
    S2b = [p.buf([128, 1024], F32, f"S2b{r}") for r in range(2)]
    G2b = [p.buf([128, 1024], F32, f"G2b{r}") for r in range(2)]

    def mod_consumer(c, Mc):
        if c < 2:
            bcast_chunk(p, sels, Mc, G1b[0], G1b[1], c * 512, pq)
        elif c < 4:
            bcast_chunk(p, sels, Mc, S2b[0], S2b[1], (c - 2) * 512, pq)
        else:
            cc = c - 4
            p.op("dve", "scalar_tensor_tensor", out=Gvc[:], in0=Mc[:], scalar=1.0, in1=g2[:, cc * 512:(cc + 1) * 512],
                 op0=ALU.add, op1=ALU.mult, reads=[Mc, g2], writes=[Gvc])
            bcast_chunk(p, sels, Gvc, G2b[0], G2b[1], cc * 512, pq)
    modulation(p, cnd, modw, modb, 3072, pkv, stage, mod_consumer)

    eps = p.buf([128, 1], F32, "eps")
    p.op("pool", "memset", eps[:], 1e-6, writes=[eps])
    wob = p.buf([128, 8, 1024], BF16, "wob")
    for k in range(8):
        p.dma("sp", stage[:, 0:1024], wo[k * 128:(k + 1) * 128, :], writes=[stage])
        p.op("pool", "tensor_copy", out=wob[:, k, :], in_=stage[:, 0:1024], reads=[stage], writes=[wob])
    wrt = p.buf([128, 8, 36], F32, "wrt")
    p.dma("sp", wrt[:], wr[:, :].rearrange("(k q) n -> q k n", q=128), writes=[wrt])
    brb = p.buf([128, 36], F32, "brb")
    p.dma("sp", brb[:], br[0:1, :].to_broadcast([128, 36]), writes=[brb])

    xt = [p.buf([128, 1024], F32, f"xt{i}") for i in range(2)]
    ot = [p.buf([128, 1024], F32, f"ot{i}") for i in range(2)]
    ob = p.buf([128, 1024], BF16, "ob")
    oT = p.buf([128, 1024], BF16, "oT")
    tmp = p.buf([128, 1024], F32, "tmp")
    xm = [p.buf([128, 1024], F32, f"xm{i}") for i in range(2)]
    junk = p.buf([128, 1024], F32, "junk")
    ss = p.buf([128, 1], F32, "ss")
    h2 = p.buf([128, 1024], F32, "h2")
    hhi = p.buf([128, 1024], BF16, "hhi")
    hlo = p.buf([128, 1024], BF16, "hlo")
    loT = p.buf([128, 1024], BF16, "loT")
    wr_hi = p.buf([128, 8, 36], BF16, "wr_hi")
    wr_lo = p.buf([128, 8, 36], BF16, "wr_lo")
    p.op("pool", "tensor_copy", out=wr_hi[:], in_=wrt[:], reads=[wrt], writes=[wr_hi])
    p.op("dve", "tensor_tensor", out=wr_lo[:], in0=wrt[:], in1=wr_hi[:], op=ALU.subtract, reads=[wrt, wr_hi], writes=[wr_lo])
    h2Tb = [p.buf([128, 1024], BF16, f"h2Tb{i}") for i in range(2)]
    lg = p.buf([128, 36], F32, "lg")
    sm = {n: p.buf([128, w], F32, n) for n, w in (("gmax", 1), ("ngmax", 1), ("goh", 4), ("gexp", 4), ("gsum", 1), ("gw", 1),
                                                   ("tmp48", 32), ("esel", 8), ("m1", 1), ("oh1", 8), ("e2", 8), ("m2", 1),
                                                   ("oh2", 8), ("d", 1), ("ed", 1), ("den", 1), ("w1", 1), ("w2", 1),
                                                   ("ga", 8), ("gsel", 8))}
    gt = [p.buf([128, 32], F32, f"gt{i}") for i in range(2)]
    outs = []

    def S(n):
        return sm[n]

    for t in range(ntile):
        r = 1 if t < 2 else 0
        i = t % 2
        rows = slice(t * 128, (t + 1) * 128)
        p.dma("sp", ot[i][:], o[rows, :], writes=[ot[i]])
        p.dma("sp", xt[i][:], xr[rows, :], writes=[xt[i]])
        p.op("pool", "tensor_copy", out=ob[:], in_=ot[i][:], reads=[ot[i]], writes=[ob])
        for k in range(8):
            p.op("pe", "transpose", pT[:, k * 128:(k + 1) * 128], ob[:, k * 128:(k + 1) * 128], ident[:],
                 reads=[ob, ident], writes=[pT])
        p.op("act", "copy", out=oT[:], in_=pT[:], reads=[pT], writes=[oT])
        for c in range(2):
            for k in range(8):
                p.op("pe", "matmul", pq[:, c * 512:(c + 1) * 512], lhsT=oT[:, k * 128:(k + 1) * 128],
                     rhs=wob[:, k, c * 512:(c + 1) * 512], start=(k == 0), stop=(k == 7), reads=[oT, wob], writes=[pq])
        p.op("dve", "tensor_tensor", out=tmp[:], in0=pq[:, :], in1=G1b[r][:], op=ALU.mult, reads=[pq, G1b[r]], writes=[tmp])
        p.op("pool", "tensor_tensor", out=xm[i][:], in0=tmp[:], in1=xt[i][:], op=ALU.add, reads=[tmp, xt[i]], writes=[xm[i]])
        outs.append(p.dma("sp", x_mid[rows, :], xm[i][:], reads=[xm[i]]))
        if upto < 1:
            continue
        p.op("act", "activation", out=junk[:], in_=xm[i][:], func=AF.Square, accum_out=ss[:], reads=[xm[i]], writes=[junk, ss])
        p.op("act", "activation", out=ss[:], in_=ss[:], func=AF.Sqrt, bias=eps[:, 0:1], scale=1.0 / 1024,
             reads=[ss, eps], writes=[ss])
        p.op("dve", "reciprocal", out=ss[:], in_=ss[:], reads=[ss], writes=[ss])
        p.op("dve", "scalar_tensor_tensor", out=tmp[:], in0=xm[i][:], scalar=ss[:, 0:1], in1=G2b[r][:], op0=ALU.mult, op1=ALU.mult,
             reads=[xm[i], ss, G2b[r]], writes=[tmp])
        p.op("pool", "tensor_tensor", out=h2[:], in0=tmp[:], in1=S2b[r][:], op=ALU.add, reads=[tmp, S2b[r]], writes=[h2])
        if upto < 2:
            continue
        p.op("pool", "tensor_copy", out=hhi[:], in_=h2[:], reads=[h2], writes=[hhi])
        p.op("dve", "tensor_tensor", out=hlo[:], in0=h2[:], in1=hhi[:], op=ALU.subtract, reads=[h2, hhi], writes=[hlo])
        for k in range(8):
            p.op("pe", "transpose", pT[:, k * 128:(k + 1) * 128], hhi[:, k * 128:(k + 1) * 128], ident[:],
                 reads=[hhi, ident], writes=[pT])
        p.op("act", "copy", out=h2Tb[i][:], in_=pT[:], reads=[pT], writes=[h2Tb[i]])
        for k in range(8):
            p.op("pe", "transpose", pT[:, k * 128:(k + 1) * 128], hlo[:, k * 128:(k + 1) * 128], ident[:],
                 reads=[hlo, ident], writes=[pT])
        p.op("act", "copy", out=loT[:], in_=pT[:], reads=[pT], writes=[loT])
        if upto >= 2.5:
            outs.append(p.dma("sp", h2T[:, :, t * 128:(t + 1) * 128], h2Tb[i][:, :].rearrange("p (k n) -> p k n", n=128),
                              reads=[h2Tb[i]]))
        if upto < 3:
            continue
        n_mm = 0
        for (lt, wt) in ((h2Tb[i], wr_hi), (h2Tb[i], wr_lo), (loT, wr_hi)):
            for k in range(8):
                p.op("pe", "matmul", pkv[:, 0:36], lhsT=lt[:, k * 128:(k + 1) * 128], rhs=wt[:, k, :],
                     start=(n_mm == 0), stop=(n_mm == 23), reads=[lt, wt], writes=[pkv])
                n_mm += 1
        p.op("dve", "tensor_tensor", out=lg[:], in0=pkv[:, 0:36], in1=brb[:], op=ALU.add, reads=[pkv, brb], writes=[lg])
        if upto < 4:
            continue
        gl = lg[:, 0:4]
        el = lg[:, 4:36].rearrange("p (g e) -> p g e", e=8)
        p.op("dve", "tensor_reduce", out=S("gmax")[:], in_=gl, axis=AX.X, op=ALU.max, reads=[lg], writes=[S("gmax")])
        p.op("dve", "tensor_scalar", out=S("goh")[:], in0=gl, scalar1=S("gmax")[:, 0:1], scalar2=None, op0=ALU.is_equal,
             reads=[lg, S("gmax")], writes=[S("goh")])
        p.op("dve", "tensor_scalar", out=S("ngmax")[:], in0=S("gmax")[:], scalar1=-1.0, scalar2=None, op0=ALU.mult,
             reads=[S("gmax")], writes=[S("ngmax")])
        p.op("act", "activation", out=S("gexp")[:], in_=gl, func=AF.Exp, bias=S("ngmax")[:, 0:1], scale=1.0,
             accum_out=S("gsum")[:], reads=[lg, S("ngmax")], writes=[S("gexp"), S("gsum")])
        p.op("dve", "reciprocal", out=S("gw")[:], in_=S("gsum")[:], reads=[S("gsum")], writes=[S("gw")])
        p.op("dve", "tensor_tensor", out=S("tmp48")[:, :].rearrange("p (g e) -> p g e", e=8), in0=el,
             in1=S("goh")[:, :].unsqueeze(2).to_broadcast([128, 4, 8]), op=ALU.mult, reads=[lg, S("goh")], writes=[S("tmp48")])
        p.op("dve", "tensor_reduce", out=S("esel")[:], in_=S("tmp48")[:, :].rearrange("p (g e) -> p e g", e=8), axis=AX.X,
             op=ALU.add, reads=[S("tmp48")], writes=[S("esel")])
        p.op("dve", "tensor_reduce", out=S("m1")[:], in_=S("esel")[:], axis=AX.X, op=ALU.max, reads=[S("esel")], writes=[S("m1")])
        p.op("dve", "tensor_scalar", out=S("oh1")[:], in0=S("esel")[:], scalar1=S("m1")[:, 0:1], scalar2=None, op0=ALU.is_equal,
             reads=[S("esel"), S("m1")], writes=[S("oh1")])
        p.op("dve", "scalar_tensor_tensor", out=S("e2")[:], in0=S("oh1")[:], scalar=-1e30, in1=S("esel")[:], op0=ALU.mult,
             op1=ALU.add, reads=[S("oh1"), S("esel")], writes=[S("e2")])
        p.op("dve", "tensor_reduce", out=S("m2")[:], in_=S("e2")[:], axis=AX.X, op=ALU.max, reads=[S("e2")], writes=[S("m2")])
        p.op("dve", "tensor_scalar", out=S("oh2")[:], in0=S("e2")[:], scalar1=S("m2")[:, 0:1], scalar2=None, op0=ALU.is_equal,
             reads=[S("e2"), S("m2")], writes=[S("oh2")])
        p.op("dve", "tensor_tensor", out=S("d")[:], in0=S("m2")[:], in1=S("m1")[:], op=ALU.subtract,
             reads=[S("m2"), S("m1")], writes=[S("d")])
        p.op("act", "activation", out=S("ed")[:], in_=S("d")[:], func=AF.Exp, reads=[S("d")], writes=[S("ed")])
        p.op("dve", "tensor_scalar", out=S("den")[:], in0=S("ed")[:], scalar1=1.0, scalar2=None, op0=ALU.add,
             reads=[S("ed")], writes=[S("den")])
        p.op("dve", "reciprocal", out=S("w1")[:], in_=S("den")[:], reads=[S("den")], writes=[S("w1")])
        p.op("dve", "tensor_tensor", out=S("w1")[:], in0=S("w1")[:], in1=S("gw")[:], op=ALU.mult,
             reads=[S("w1"), S("gw")], writes=[S("w1")])
        p.op("dve", "tensor_tensor", out=S("w2")[:], in0=S("w1")[:], in1=S("ed")[:], op=ALU.mult,
             reads=[S("w1"), S("ed")], writes=[S("w2")])
        p.op("dve", "tensor_scalar", out=S("ga")[:], in0=S("oh1")[:], scalar1=S("w1")[:, 0:1], scalar2=None, op0=ALU.mult,
             reads=[S("oh1"), S("w1")], writes=[S("ga")])
        p.op("dve", "scalar_tensor_tensor", out=S("gsel")[:], in0=S("oh2")[:], scalar=S("w2")[:, 0:1], in1=S("ga")[:],
             op0=ALU.mult, op1=ALU.add, reads=[S("oh2"), S("w2"), S("ga")], writes=[S("gsel")])
        p.op("dve", "tensor_tensor", out=gt[i][:, :].rearrange("p (g e) -> p g e", e=8),
             in0=S("goh")[:, :].unsqueeze(2).to_broadcast([128, 4, 8]),
             in1=S("gsel")[:, :].unsqueeze(1).to_broadcast([128, 4, 8]), op=ALU.mult,
             reads=[S("goh"), S("gsel")], writes=[gt[i]])
        outs.append(p.dma("sp", gates[rows, :], gt[i][:], reads=[gt[i]]))
    p.emit(final_waits=outs)
    return nc


def build_post2():
    nc = bass.Bass("TRN2", target_bir_lowering=False)

    def din(name, shape, dt=F32):
        return nc.dram_tensor(name, list(shape), dt, kind="ExternalInput").ap()

    def dout(name, shape, dt=F32):
        return nc.dram_tensor(name, list(shape), dt, kind="ExternalOutput").ap()
    x_mid = din("x_mid", [NTOK, 1024])
    h2T = din("h2T", [128, 8, NTOK], BF16)
    gates = din("gates", [NTOK, 32])
    cnd = din("cnd", [128, 8, 2])
    modw = din("modw", [1024, 1024])
    modb = din("modb", [1, 1024])
    fg = din("fg", [1, 1024])
    w1 = din("w1", [32, 1024, 768])
    w3 = din("w3", [32, 1024, 768])
    w2 = din("w2", [32, 768, 1024])
    x_out = dout("x_out", [NTOK, 1024])
    y_fin = dout("y_fin", [NTOK, 1024])

    p = Prog(nc)
    sels = mk_sel2(p)
    ph1 = [p.pbuf([128, 512], F32, f"ph1{i}") for i in range(2)]
    ph3 = [p.pbuf([128, 512], F32, f"ph3{i}") for i in range(2)]
    py = [p.pbuf([128, 512], F32, f"py{i}") for i in range(2)]
    pm = p.pbuf([128, 512], F32, "pm")
    pm2 = p.pbuf([128, 512], F32, "pm2")

    stg = [p.buf([128, 2048], F32, f"stg{i}") for i in range(2)]
    GTb = [p.buf([128, 1024], F32, f"GTb{r}") for r in range(2)]

    def mod_consumer(c, Mc):
        bcast_chunk(p, sels, Mc, GTb[0], GTb[1], c * 512, pm2)
    modulation(p, cnd, modw, modb, 1024, pm, stg, mod_consumer)
    fgb = p.buf([128, 1024], F32, "fgb")
    p.dma("sp", fgb[:], fg[0:1, :].to_broadcast([128, 1024]), writes=[fgb])
    eps = p.buf([128, 1], F32, "eps")
    p.op("pool", "memset", eps[:], 1e-6, writes=[eps])

    hT = p.buf([128, 8, NTOK], BF16, "hT")
    p.dma("sp", hT[:], h2T[:, :, :], writes=[hT])
    gts = p.buf([128, NTILE, 32], F32, "gts")
    p.dma("sp", gts[:], gates[:, :].rearrange("(t q) e -> q t e", q=128), writes=[gts])
    HT = NTILE // 2
    accs = [p.buf([128, 1024], F32, f"acc{t}") for t in range(HT)]
    w1b = [p.buf([128, 8, 768], BF16, f"w1b{i}") for i in range(2)]
    w3b = [p.buf([128, 8, 768], BF16, f"w3b{i}") for i in range(2)]
    w2b = [p.buf([128, 6, 1024], BF16, f"w2b{i}") for i in range(2)]
    hid = [p.buf([128, 6, 384], BF16, f"hid{i}") for i in range(2)]
    s1 = [p.buf([128, 384], F32, f"s1{i}") for i in range(2)]
    xo = [p.buf([128, 1024], F32, f"xo{i}") for i in range(2)]
    yo = [p.buf([128, 1024], F32, f"yo{i}") for i in range(2)]
    ss = p.buf([128, 1], F32, "ss")
    outs = []
    cnts = {"stg": 0, "h": 0, "y": 0, "g": 0}

    def load_w(e, par):
        for kk in range(4):
            for (src, dst) in ((w1, w1b[par]), (w3, w3b[par])):
                s = stg[cnts["stg"] % 2]
                cnts["stg"] += 1
                p.dma("sp", s[:, 0:1536].rearrange("p (k n) -> p k n", n=768),
                      src[e, kk * 256:(kk + 1) * 256, :].rearrange("(k q) n -> q k n", q=128), writes=[s])
                p.op("pool", "tensor_copy", out=dst[:, 2 * kk:2 * kk + 2, :], in_=s[:, 0:1536].rearrange("p (k n) -> p k n", n=768),
                     reads=[s], writes=[dst])
        for kk in range(3):
            s = stg[cnts["stg"] % 2]
            cnts["stg"] += 1
            p.dma("sp", s[:, :].rearrange("p (k n) -> p k n", n=1024),
                  w2[e, kk * 256:(kk + 1) * 256, :].rearrange("(k q) n -> q k n", q=128), writes=[s])
            p.op("pool", "tensor_copy", out=w2b[par][:, 2 * kk:2 * kk + 2, :], in_=s[:, :].rearrange("p (k n) -> p k n", n=1024),
                 reads=[s], writes=[w2b[par]])

    for half in range(2):
        t0 = half * HT
        for e in range(32):
            par = e % 2
            load_w(e, par)
            for gi in range(HT // 3):
                tok0 = (t0 + gi * 3) * 128
                hd = hid[cnts["g"] % 2]
                cnts["g"] += 1
                for f in range(6):
                    j = cnts["h"] % 2
                    cnts["h"] += 1
                    for k in range(8):
                        p.op("pe", "matmul", ph1[j][:, 0:384], lhsT=w1b[par][:, k, f * 128:(f + 1) * 128],
                             rhs=hT[:, k, tok0:tok0 + 384], start=(k == 0), stop=(k == 7), reads=[w1b[par], hT], writes=[ph1[j]])
                    for k in range(8):
                        p.op("pe", "matmul", ph3[j][:, 0:384], lhsT=w3b[par][:, k, f * 128:(f + 1) * 128],
                             rhs=hT[:, k, tok0:tok0 + 384], start=(k == 0), stop=(k == 7), reads=[w3b[par], hT], writes=[ph3[j]])
                    p.op("act", "activation", out=s1[j][:], in_=ph1[j][:, 0:384], func=AF.Silu, reads=[ph1[j]], writes=[s1[j]])
                    p.op("dve", "tensor_tensor", out=hd[:, f, :], in0=s1[j][:], in1=ph3[j][:, 0:384], op=ALU.mult,
                         reads=[s1[j], ph3[j]], writes=[hd])
                for tt in range(3):
                    tl = gi * 3 + tt
                    tg = t0 + tl
                    for c in range(2):
                        j = cnts["y"] % 2
                        cnts["y"] += 1
                        for f in range(6):
                            p.op("pe", "matmul", py[j][:, 0:512], lhsT=hd[:, f, tt * 128:(tt + 1) * 128],
                                 rhs=w2b[par][:, f, c * 512:(c + 1) * 512], start=(f == 0), stop=(f == 5),
                                 reads=[hd, w2b[par]], writes=[py[j]])
                        if e == 0:
                            p.op("dve", "tensor_scalar", out=accs[tl][:, c * 512:(c + 1) * 512], in0=py[j][:, 0:512],
                                 scalar1=gts[:, tg, e:e + 1], scalar2=None, op0=ALU.mult,
                                 reads=[py[j], gts], writes=[accs[tl]])
                        else:
                            p.op("dve", "scalar_tensor_tensor", out=accs[tl][:, c * 512:(c + 1) * 512], in0=py[j][:, 0:512],
                                 scalar=gts[:, tg, e:e + 1], in1=accs[tl][:, c * 512:(c + 1) * 512], op0=ALU.mult, op1=ALU.add,
                                 reads=[py[j], gts, accs[tl]], writes=[accs[tl]])
        for tl in range(HT):
            tg = t0 + tl
            r = 1 if tg < 2 else 0
            i = tl % 2
            rows = slice(tg * 128, (tg + 1) * 128)
            p.dma("sp", xo[i][:], x_mid[rows, :], writes=[xo[i]])
            p.op("pool", "tensor_tensor", out=accs[tl][:], in0=accs[tl][:], in1=GTb[r][:], op=ALU.mult,
                 reads=[accs[tl], GTb[r]], writes=[accs[tl]])
            p.op("pool", "tensor_tensor", out=xo[i][:], in0=xo[i][:], in1=accs[tl][:], op=ALU.add,
                 reads=[xo[i], accs[tl]], writes=[xo[i]])
            outs.append(p.dma("sp", x_out[rows, :], xo[i][:], reads=[xo[i]]))
            p.op("act", "activation", out=yo[i][:], in_=xo[i][:], func=AF.Square, accum_out=ss[:], reads=[xo[i]], writes=[yo[i], ss])
            p.op("act", "activation", out=ss[:], in_=ss[:], func=AF.Sqrt, bias=eps[:, 0:1], scale=1.0 / 1024,
                 reads=[ss, eps], writes=[ss])
            p.op("dve", "reciprocal", out=ss[:], in_=ss[:], reads=[ss], writes=[ss])
            p.op("dve", "scalar_tensor_tensor", out=yo[i][:], in0=xo[i][:], scalar=ss[:, 0:1], in1=fgb[:], op0=ALU.mult, op1=ALU.mult,
                 reads=[xo[i], ss, fgb], writes=[yo[i]])
            outs.append(p.dma("sp", y_fin[rows, :], yo[i][:], reads=[yo[i]]))
    p.emit(final_waits=outs)
    return nc


def _cnd(c_b, c_ctx):
    return np.ascontiguousarray(np.stack([c_b, c_ctx], -1).reshape(8, 128, 2).transpose(1, 0, 2))


def _rows(x_lat, x_ctx, ci):
    b, qi = ci // 4, ci % 4
    return np.ascontiguousarray(np.concatenate([x_ctx[b], x_lat[b, qi * 2048:(qi + 1) * 2048]], 0))


def post1_inputs(x_lat, x_ctx, o_lat, o_ctx, c, c_ctx, mod_w, mod_b, n2g, wo, wg, bg, we, be):
    wr = np.ascontiguousarray(np.concatenate([wg, we], 1))
    br = np.ascontiguousarray(np.concatenate([bg, be])[None, :])
    maps = []
    for ci in range(8):
        b = ci // 4
        maps.append(dict(xr=_rows(x_lat, x_ctx, ci), o=_rows(o_lat, o_ctx, ci), cnd=_cnd(c[b], c_ctx),
                         modw=np.ascontiguousarray(mod_w[:, 2048:5120]), modb=np.ascontiguousarray(mod_b[None, 2048:5120]),
                         n2g=np.ascontiguousarray(n2g[None, :]), wo=np.ascontiguousarray(wo), wr=wr, br=br))
    return maps


def post2_inputs(res1, c, c_ctx, mod_w, mod_b, fg, w1, w3, w2):
    maps = []
    for ci in range(8):
        b = ci // 4
        r = res1[ci]
        maps.append(dict(x_mid=r["x_mid"], h2T=r["h2T"], gates=r["gates"], cnd=_cnd(c[b], c_ctx),
                         modw=np.ascontiguousarray(mod_w[:, 5120:6144]), modb=np.ascontiguousarray(mod_b[None, 5120:6144]),
                         fg=np.ascontiguousarray(fg[None, :]), w1=w1, w3=w3, w2=w2))
    return maps


def _unrows(res, key):
    lat = np.zeros((2, 8192, 1024), np.float32)
    ctx = np.zeros((2, 256, 1024), np.float32)
    for ci in range(8):
        b, qi = ci // 4, ci % 4
        a = res[ci][key]
        lat[b, qi * 2048:(qi + 1) * 2048] = a[256:]
        if qi == 0:
            ctx[b] = a[:256]
    return lat, ctx


RT_TOK = 8448
RT_TILES = 66
NDEC = -0.6065306597126334


def build_rwkv(ntiles=RT_TILES, phase_c=True):
    nc = bass.Bass("TRN2", target_bir_lowering=False)

    def din(name, shape, dt=F32):
        return nc.dram_tensor(name, list(shape), dt, kind="ExternalInput").ap()

    def dscr(name, shape, dt=F32, kind="Internal"):
        return nc.dram_tensor(name, list(shape), dt, kind=kind).ap()
    xa = din("xa", [RT_TOK, 1024])
    xp = din("xp", [RT_TOK, 1024])
    xn = din("xn", [RT_TOK, 1024])
    mpn = din("mpn", [RT_TOK, 2])
    cnd = din("cnd", [128, 8, 2])
    modw = din("modw", [1024, 2048])
    modb = din("modb", [1, 2048])
    n1g = din("n1g", [1, 1024])
    mu = din("mu", [6, 1024])
    w_r = din("w_r", [1024, 256])
    w_k = din("w_k", [1024, 256])
    w_v = din("w_v", [1024, 256])
    g1 = din("g1", [1024, 128])
    g2 = din("g2", [128, 256])
    w1 = din("w1", [1024, 128])
    w2 = din("w2", [64, 512])
    a1 = din("a1", [1024, 128])
    a2 = din("a2", [64, 512])
    vecs = din("vecs", [9, 256])
    o_out = nc.dram_tensor("o_out", [RT_TOK, 256], F32, kind="ExternalOutput").ap()
    FK = "ExternalOutput" if not phase_c else "Internal"
    f_r = dscr("f_r", [RT_TOK, 256], kind=FK)
    f_v = dscr("f_v", [RT_TOK, 256], kind=FK)
    f_kk = dscr("f_kk", [RT_TOK, 256], kind=FK)
    f_g = dscr("f_g", [RT_TOK, 256], kind=FK)
    f_bon = dscr("f_bon", [RT_TOK, 256], kind=FK)
    f_kd = [dscr(f"f_kd{d}", [RT_TOK, 256], kind=FK) for d in range(2)]
    f_b = [dscr(f"f_b{d}", [RT_TOK, 256], kind=FK) for d in range(2)]
    f_lw = [dscr(f"f_lw{d}", [RT_TOK, 256], kind=FK) for d in range(2)]
    f_y = dscr("f_y", [RT_TOK, 256])

    p = Prog(nc)
    idt = mk_ident(p)
    ident, identf = idt[BF16], idt[F32]
    sels = mk_sel2(p)
    pT = p.pbuf([128, 1024], BF16, "pT")
    Q = [p.pbuf([128, 512], F32, f"q{i}") for i in range(7)]

    stage = p.buf([128, 4096], F32, "stage")
    g2n = p.buf([2, 1024], F32, "g2n")
    p.dma("sp", g2n[:], n1g[0:1, :].to_broadcast([2, 1024]), writes=[g2n])
    Gvc = p.buf([2, 512], F32, "Gvc")
    Gb = [p.buf([128, 1024], F32, f"Gb{r}") for r in range(2)]
    Sb = [p.buf([128, 1024], F32, f"Sb{r}") for r in range(2)]

    def mod_consumer(c, Mc):
        if c < 2:
            bcast_chunk(p, sels, Mc, Sb[0], Sb[1], c * 512, Q[1])
        else:
            cc = c - 2
            p.op("dve", "scalar_tensor_tensor", out=Gvc[:], in0=Mc[:], scalar=1.0, in1=g2n[:, cc * 512:(cc + 1) * 512],
                 op0=ALU.add, op1=ALU.mult, reads=[Mc, g2n], writes=[Gvc])
            bcast_chunk(p, sels, Gvc, Gb[0], Gb[1], cc * 512, Q[1])
    modulation(p, cnd, modw, modb, 2048, Q[0], stage, mod_consumer)

    eps = p.buf([128, 1], F32, "eps")
    p.op("pool", "memset", eps[:], 1e-6, writes=[eps])
    eps_ln = p.buf([128, 1], F32, "eps_ln")
    p.op("pool", "memset", eps_ln[:], 64e-5, writes=[eps_ln])
    MUb = [p.buf([128, 1024], F32, f"MUb{i}") for i in range(6)]
    for i in range(6):
        p.dma("sp", MUb[i][:], mu[i:i + 1, :].to_broadcast([128, 1024]), writes=[MUb[i]])
    VC = [p.buf([128, 256], F32, f"vc{i}") for i in range(9)]
    for i in range(9):
        p.dma("sp", VC[i][:], vecs[i:i + 1, :].to_broadcast([128, 256]), writes=[VC[i]])
    w0b, a0b, kkb, kab, rkb, lnwb, lnbb = VC[0:2], VC[2:4], VC[4], VC[5], VC[6], VC[7], VC[8]

    def load_bf(src_ap, shape, name, view):
        dst = p.buf(shape, BF16, name)
        n = shape[-1]
        p.dma("sp", stage[:, 0:8 * n].rearrange("p (k n) -> p k n", n=n), src_ap.rearrange("(k q) n -> q k n", q=128), writes=[stage])
        p.op("pool", "tensor_copy", out=dst[:], in_=stage[:, 0:8 * n].rearrange("p (k n) -> p k n", n=n), reads=[stage], writes=[dst])
        return dst
    wrb = load_bf(w_r[:, :], [128, 8, 256], "wrb", None)
    wkb = load_bf(w_k[:, :], [128, 8, 256], "wkb", None)
    wvb = load_bf(w_v[:, :], [128, 8, 256], "wvb", None)
    g1b = load_bf(g1[:, :], [128, 8, 128], "g1b", None)
    w1b = load_bf(w1[:, :], [128, 8, 128], "w1b", None)
    a1b = load_bf(a1[:, :], [128, 8, 128], "a1b", None)

    def load_small_bf(src_ap, shape, name):
        dst = p.buf(shape, BF16, name)
        p.dma("sp", stage[0:shape[0], 0:shape[1]], src_ap, writes=[stage])
        p.op("pool", "tensor_copy", out=dst[:], in_=stage[0:shape[0], 0:shape[1]], reads=[stage], writes=[dst])
        return dst
    g2b = load_small_bf(g2[:, :], [128, 256], "g2b")
    w2b = load_small_bf(w2[:, :], [64, 512], "w2b")
    a2b = load_small_bf(a2[:, :], [64, 512], "a2b")

    xt3 = [p.buf([128, 1024], F32, f"x3_{i}") for i in range(3)]
    h3 = [p.buf([128, 1024], F32, f"h3_{i}") for i in range(3)]
    msk = p.buf([128, 2], F32, "msk")
    junk = p.buf([128, 1024], F32, "junk")
    ss3 = [p.buf([128, 1], F32, f"ss3_{i}") for i in range(3)]
    xx = p.buf([128, 1024], F32, "xx")
    tmpx = [p.buf([128, 1024], F32, f"tmpx{i}") for i in range(2)]
    xib = [p.buf([128, 1024], BF16, f"xib{i}") for i in range(2)]
    xT = [p.buf([128, 1024], BF16, f"xT{i}") for i in range(6)]
    sgT = p.buf([128, 128], BF16, "sgT")
    lorT = [p.buf([64, 128], BF16, f"lorT{i}") for i in range(4)]
    names = ["r32", "k32", "v32", "g32", "kkr", "kk32", "bon", "t0", "t1", "ks"] + \
            [f"{n}{d}" for n in ("wl", "a32", "lw", "kd", "bb") for d in range(2)]
    T_ = {n: p.buf([128, 256], F32, n) for n in names}
    sm4 = [p.buf([128, 4], F32, f"sm4_{i}") for i in range(3)]
    ew_i = [0]

    def ew():
        ew_i[0] += 1
        return "dve" if ew_i[0] % 2 else "pool"

    def v4(t):
        return t[:, :].rearrange("p (h d) -> p h d", d=64)

    def b4(s):
        return s[:, 0:4].unsqueeze(2).to_broadcast([128, 4, 64])

    feat_out = []
    for t in range(ntiles):
        rr = 1 if t < 2 else 0
        rows = slice(t * 128, (t + 1) * 128)
        for i, src in enumerate((xa, xp, xn)):
            p.dma("sp", xt3[i][:], src[rows, :], writes=[xt3[i]])
        p.dma("sp", msk[:], mpn[rows, :], writes=[msk])
        for i in range(3):
            p.op("act", "activation", out=junk[:], in_=xt3[i][:], func=AF.Square, accum_out=ss3[i][:], reads=[xt3[i]], writes=[junk, ss3[i]])
            p.op("act", "activation", out=ss3[i][:], in_=ss3[i][:], func=AF.Sqrt, bias=eps[:, 0:1], scale=1.0 / 1024,
                 reads=[ss3[i], eps], writes=[ss3[i]])
            p.op("dve", "reciprocal", out=ss3[i][:], in_=ss3[i][:], reads=[ss3[i]], writes=[ss3[i]])
            p.op("dve", "scalar_tensor_tensor", out=h3[i][:], in0=xt3[i][:], scalar=ss3[i][:, 0:1], in1=Gb[rr][:], op0=ALU.mult,
                 op1=ALU.mult, reads=[xt3[i], ss3[i], Gb[rr]], writes=[h3[i]])
            p.op("pool", "tensor_tensor", out=h3[i][:], in0=h3[i][:], in1=Sb[rr][:], op=ALU.add, reads=[h3[i], Sb[rr]], writes=[h3[i]])
        h = h3[0]
        p.op("dve", "tensor_scalar", out=xx[:], in0=h3[1][:], scalar1=msk[:, 0:1], scalar2=None, op0=ALU.mult,
             reads=[h3[1], msk], writes=[xx])
        p.op("dve", "scalar_tensor_tensor", out=xx[:], in0=h3[2][:], scalar=msk[:, 1:2], in1=xx[:], op0=ALU.mult, op1=ALU.add,
             reads=[h3[2], msk, xx], writes=[xx])
        p.op("dve", "scalar_tensor_tensor", out=xx[:], in0=xx[:], scalar=0.5, in1=h[:], op0=ALU.mult, op1=ALU.subtract,
             reads=[xx, h], writes=[xx])
        for i in range(6):
            tm, xb_ = tmpx[i % 2], xib[i % 2]
            p.op("pool", "tensor_tensor", out=tm[:], in0=xx[:], in1=MUb[i][:], op=ALU.mult, reads=[xx, MUb[i]], writes=[tm])
            p.op("dve", "tensor_tensor", out=xb_[:], in0=tm[:], in1=h[:], op=ALU.add, reads=[tm, h], writes=[xb_])
            for k in range(8):
                p.op("pe", "transpose", pT[:, k * 128:(k + 1) * 128], xb_[:, k * 128:(k + 1) * 128], ident[:],
                     reads=[xb_, ident], writes=[pT])
            p.op("act", "copy", out=xT[i][:], in_=pT[:], reads=[pT], writes=[xT[i]])
        for (qi, c0, src, wt) in ((0, 0, xT[0], wrb), (0, 256, xT[2], wkb), (1, 0, xT[3], wvb)):
            for k in range(8):
                p.op("pe", "matmul", Q[qi][:, c0:c0 + 256], lhsT=src[:, k * 128:(k + 1) * 128], rhs=wt[:, k, :],
                     start=(k == 0 and c0 == 0), stop=(k == 7), reads=[src, wt], writes=[Q[qi]])
        for k in range(8):
            p.op("pe", "matmul", Q[2][:, 0:128], lhsT=g1b[:, k, :], rhs=xT[5][:, k * 128:(k + 1) * 128],
                 start=(k == 0), stop=(k == 7), reads=[g1b, xT[5]], writes=[Q[2]])
        p.op("act", "activation", out=sgT[:], in_=Q[2][:, 0:128], func=AF.Sigmoid, reads=[Q[2]], writes=[sgT])
        p.op("pe", "matmul", Q[1][:, 256:512], lhsT=sgT[:, :], rhs=g2b[:, :], start=False, stop=True,
             reads=[sgT, g2b], writes=[Q[1]])
        for li, (src, wa, wb_, fn) in enumerate(((xT[1], w1b, w2b, AF.Tanh), (xT[4], a1b, a2b, AF.Copy))):
            for d in range(2):
                for k in range(8):
                    p.op("pe", "matmul", Q[3][0:64, 0:128], lhsT=wa[:, k, d * 64:(d + 1) * 64], rhs=src[:, k * 128:(k + 1) * 128],
                         start=(k == 0), stop=(k == 7), reads=[wa, src], writes=[Q[3]])
                lt = lorT[li * 2 + d]
                p.op("act", "activation", out=lt[:], in_=Q[3][0:64, 0:128], func=fn, reads=[Q[3]], writes=[lt])
                p.op("pe", "matmul", Q[4 + li][:, d * 256:(d + 1) * 256], lhsT=lt[:, :], rhs=wb_[:, d * 256:(d + 1) * 256],
                     start=(d == 0), stop=True, reads=[lt, wb_], writes=[Q[4 + li]])
        p.op("act", "copy", out=T_["r32"][:], in_=Q[0][:, 0:256], reads=[Q[0]], writes=[T_["r32"]])
        p.op("act", "copy", out=T_["k32"][:], in_=Q[0][:, 256:512], reads=[Q[0]], writes=[T_["k32"]])
        p.op("act", "copy", out=T_["v32"][:], in_=Q[1][:, 0:256], reads=[Q[1]], writes=[T_["v32"]])
        p.op("act", "copy", out=T_["g32"][:], in_=Q[1][:, 256:512], reads=[Q[1]], writes=[T_["g32"]])
        for d in range(2):
            wl, a32, lw = T_[f"wl{d}"], T_[f"a32{d}"], T_[f"lw{d}"]
            p.op("dve", "tensor_tensor", out=wl[:], in0=Q[4][:, d * 256:(d + 1) * 256], in1=w0b[d][:], op=ALU.add,
                 reads=[Q[4], w0b[d]], writes=[wl])
            p.op("act", "activation", out=wl[:], in_=wl[:], func=AF.Sigmoid, reads=[wl], writes=[wl])
            p.op("pool", "tensor_scalar", out=lw[:], in0=wl[:], scalar1=NDEC, scalar2=None, op0=ALU.mult, reads=[wl], writes=[lw])
            p.op("dve", "tensor_tensor", out=a32[:], in0=Q[5][:, d * 256:(d + 1) * 256], in1=a0b[d][:], op=ALU.add,
                 reads=[Q[5], a0b[d]], writes=[a32])
            p.op("act", "activation", out=a32[:], in_=a32[:], func=AF.Sigmoid, reads=[a32], writes=[a32])
        p.op("pool", "tensor_tensor", out=T_["kkr"][:], in0=T_["k32"][:], in1=kkb[:], op=ALU.mult, reads=[T_["k32"], kkb], writes=[T_["kkr"]])
        p.op("act", "activation", out=T_["t0"][:], in_=T_["kkr"][:], func=AF.Square, reads=[T_["kkr"]], writes=[T_["t0"]])
        p.op("dve", "tensor_reduce", out=sm4[0][:], in_=v4(T_["t0"]), axis=AX.X, op=ALU.add, reads=[T_["t0"]], writes=[sm4[0]])
        p.op("act", "activation", out=sm4[0][:], in_=sm4[0][:], func=AF.Sqrt, reads=[sm4[0]], writes=[sm4[0]])
        p.op("dve", "tensor_scalar", out=sm4[0][:], in0=sm4[0][:], scalar1=1e-12, scalar2=None, op0=ALU.max, reads=[sm4[0]], writes=[sm4[0]])
        p.op("dve", "reciprocal", out=sm4[0][:], in_=sm4[0][:], reads=[sm4[0]], writes=[sm4[0]])
        p.op("dve", "tensor_tensor", out=v4(T_["kk32"]), in0=v4(T_["kkr"]), in1=b4(sm4[0]), op=ALU.mult,
             reads=[T_["kkr"], sm4[0]], writes=[T_["kk32"]])
        for d in range(2):
            a32, kd, bb = T_[f"a32{d}"], T_[f"kd{d}"], T_[f"bb{d}"]
            e1 = ew()
            p.op(e1, "scalar_tensor_tensor", out=T_["t1"][:], in0=a32[:], scalar=-1.0, in1=kab[:], op0=ALU.add, op1=ALU.mult,
                 reads=[a32, kab], writes=[T_["t1"]])
            p.op(e1, "scalar_tensor_tensor", out=kd[:], in0=T_["t1"][:], scalar=1.0, in1=T_["k32"][:], op0=ALU.add, op1=ALU.mult,
                 reads=[T_["t1"], T_["k32"]], writes=[kd])
            p.op(ew(), "tensor_tensor", out=bb[:], in0=T_["kk32"][:], in1=a32[:], op=ALU.mult, reads=[T_["kk32"], a32], writes=[bb])
        p.op("pool", "tensor_tensor", out=T_["ks"][:], in0=T_["kd0"][:], in1=T_["kd1"][:], op=ALU.add,
             reads=[T_["kd0"], T_["kd1"]], writes=[T_["ks"]])
        p.op("pool", "tensor_tensor", out=T_["ks"][:], in0=T_["ks"][:], in1=T_["r32"][:], op=ALU.mult, reads=[T_["ks"], T_["r32"]], writes=[T_["ks"]])
        p.op("pool", "tensor_tensor", out=T_["ks"][:], in0=T_["ks"][:], in1=rkb[:], op=ALU.mult, reads=[T_["ks"], rkb], writes=[T_["ks"]])
        p.op("dve", "tensor_reduce", out=sm4[1][:], in_=v4(T_["ks"]), axis=AX.X, op=ALU.add, reads=[T_["ks"]], writes=[sm4[1]])
        p.op("dve", "tensor_tensor", out=v4(T_["bon"]), in0=v4(T_["v32"]), in1=b4(sm4[1]), op=ALU.mult,
             reads=[T_["v32"], sm4[1]], writes=[T_["bon"]])
        for (dst, srcn) in ((f_r, "r32"), (f_v, "v32"), (f_kk, "kk32"), (f_g, "g32"), (f_bon, "bon"),
                            (f_kd[0], "kd0"), (f_kd[1], "kd1"), (f_b[0], "bb0"), (f_b[1], "bb1"),
                            (f_lw[0], "lw0"), (f_lw[1], "lw1")):
            feat_out.append(p.dma("sp", dst[rows, :], T_[srcn][:], reads=[T_[srcn]]))

    if not phase_c:
        p.emit(final_waits=feat_out)
        return nc
    rwkv_phase_c(p, nc, locals())
    return nc


def rwkv_phase_c(p, nc, L):
    ntiles = L["ntiles"]
    ident, identf, pT, Q = L["ident"], L["identf"], L["pT"], L["Q"]
    f_r, f_v, f_kk, f_g, f_bon, f_kd, f_b, f_lw, f_y, o_out = (L[k] for k in
        ("f_r", "f_v", "f_kk", "f_g", "f_bon", "f_kd", "f_b", "f_lw", "f_y", "o_out"))
    lnwb, lnbb, eps_ln, feat_out = L["lnwb"], L["lnbb"], L["eps_ln"], L["feat_out"]

    fence = Buf(None, "fence")
    for d in feat_out:
        fence.r.append(d)
    fdum = p.buf([1, 8], F32, "fdum")
    fd2 = p.buf([1, 8], F32, "fd2")
    fd3 = p.buf([1, 8], F32, "fd3")
    p.op("pool", "memset", fdum[:], 0.0, writes=[fence, fdum])
    p.op("dve", "tensor_copy", out=fd2[:], in_=fdum[:], reads=[fdum], writes=[fd2])
    p.op("act", "copy", out=fd3[:], in_=fdum[:], reads=[fdum], writes=[fd3])
    arena_src = [b.ap for b in (L["MUb"] + L["xt3"] + L["h3"] + L["tmpx"] + [L["xx"], L["junk"]])]
    ar = {"i": 0, "off": 0}

    def abuf(shape, dt, name):
        nparts = shape[0]
        ncols = 1
        for v_ in shape[1:]:
            ncols *= v_
        nf = ncols if dt == F32 else (ncols + 1) // 2
        if ar["off"] + nf > 1024:
            ar["i"] += 1
            ar["off"] = 0
        t = arena_src[ar["i"]]
        ap = t[0:nparts, ar["off"]:ar["off"] + nf]
        ar["off"] += nf
        if dt != F32:
            ap = ap.bitcast(dt)
        if len(shape) == 3:
            ap = ap.rearrange("p (j n) -> p j n", n=shape[2])
        return Buf(ap, name)

    def tri_mask(name, sg, strict, transpose=False):
        mf = abuf([128, 128], F32, name + "f")
        p.op("pool", "memset", mf[:], 1.0, writes=[mf])
        s_ = -sg if transpose else sg
        p.op("pool", "affine_select", out=mf[:], in_=mf[:], pattern=[[s_, 128]], compare_op=ALU.is_ge, fill=0.0,
             base=(-1 if strict else 0), channel_multiplier=-s_, reads=[mf], writes=[mf])
        p.op("pool", "memset", mf[0:64, 64:128], 0.0, reads=[mf], writes=[mf])
        p.op("pool", "memset", mf[64:128, 0:64], 0.0, reads=[mf], writes=[mf])
        return mf
    masks = []
    for d in range(2):
        sg = 1 if d == 0 else -1
        ms = tri_mask(f"ms{d}", sg, True)
        mi = tri_mask(f"mi{d}", sg, False)
        mst = tri_mask(f"mst{d}", sg, True, transpose=True)
        m2 = abuf([128, 256], F32, f"m2_{d}")
        p.op("dve", "tensor_copy", out=m2[:, 0:128], in_=ms[:], reads=[ms], writes=[m2])
        p.op("dve", "tensor_copy", out=m2[:, 128:256], in_=mi[:], reads=[mi], writes=[m2])
        msb = abuf([128, 128], BF16, f"msb{d}")
        mib = abuf([128, 128], BF16, f"mib{d}")
        p.op("dve", "tensor_copy", out=msb[:], in_=ms[:], reads=[ms], writes=[msb])
        p.op("dve", "tensor_copy", out=mib[:], in_=mi[:], reads=[mi], writes=[mib])
        masks.append(dict(m2=m2, mst=mst, msb=msb, mib=mib))
    cind = abuf([128, 2], BF16, "cind")
    p.op("pool", "memset", cind[:], 0.0, writes=[cind])
    p.op("pool", "memset", cind[0:64, 0:1], 1.0, reads=[cind], writes=[cind])
    p.op("pool", "memset", cind[64:128, 1:2], 1.0, reads=[cind], writes=[cind])

    NH = 4
    ld = {n: [abuf([128, 256], F32, f"ld_{n}{i}") for i in range(2)] for n in ("r", "v", "kk", "kd", "b", "lw")}
    lwh = abuf([128, 256], BF16, "lwh")
    lwl = abuf([128, 256], BF16, "lwl")
    Pm = {n: abuf([128, 256], F32, "P" + n) for n in ("p", "inv", "ex")}
    rcb = abuf([128, 256], BF16, "rcb")
    khb = abuf([128, 256], BF16, "khb")
    nbb = abuf([128, 256], BF16, "nbb")
    vb = abuf([128, 256], BF16, "vb")
    Wall = [[abuf([128, 128], BF16, f"W{h}_{i}") for i in range(2)] for h in range(NH)]
    FT = [abuf([64, 4, 128], BF16, f"FT{h}") for h in range(NH)]
    YN = [abuf([128, 256], BF16, f"YN{h}") for h in range(NH)]
    AG = [abuf([128, 256], BF16, f"AG{h}") for h in range(NH)]
    Xm = [[abuf([128, 128], BF16, f"X{h}_{i}") for i in range(2)] for h in range(NH)]
    Ym = [[abuf([128, 128], BF16, f"Y{h}_{i}") for i in range(2)] for h in range(NH)]
    Y0f = [abuf([128, 64], F32, f"Y0f{h}") for h in range(NH)]
    M2b = [abuf([128, 64], BF16, f"M2b{h}") for h in range(NH)]
    M2T = [abuf([64, 128], BF16, f"M2T{h}") for h in range(NH)]
    M3T = [[abuf([64, 64], BF16, f"M3T{h}_{c}") for c in range(2)] for h in range(NH)]
    Z0P = [[abuf([64, 64], F32, f"Z0P{h}_{c}") for c in range(2)] for h in range(NH)]
    Pc = abuf([64, 8], F32, "Pc")
    H32 = [[abuf([64, 64], F32, f"H32_{d}_{h}") for h in range(NH)] for d in range(2)]
    Hhi = [[abuf([64, 64], BF16, f"Hhi_{d}_{h}") for h in range(NH)] for d in range(2)]
    Hlo = [[abuf([64, 64], BF16, f"Hlo_{d}_{h}") for h in range(NH)] for d in range(2)]
    for d in range(2):
        for h in range(NH):
            p.op("pool", "memset", H32[d][h][:], 0.0, writes=[H32[d][h]])
            p.op("pool", "memset", Hhi[d][h][:], 0.0, writes=[Hhi[d][h]])
            p.op("pool", "memset", Hlo[d][h][:], 0.0, writes=[Hlo[d][h]])
    ytile = [abuf([128, 256], F32, f"ytile{i}") for i in range(2)]
    yf = abuf([128, 256], F32, "yf")
    og = {n: abuf([128, 256], F32, "og_" + n) for n in ("g", "bon", "yc", "sq")}
    st4 = [abuf([128, 4], F32, f"st4_{i}") for i in range(2)]
    outs = []
    ycnt = [0]
    pcum, pA, pB, pS0, pS1, pYc, pHc = Q
    psm = [pS0, pS1]
    smi = [0]

    def nps():
        smi[0] += 1
        return psm[smi[0] % 2]

    def v4(t):
        return t[:, :].rearrange("p (h d) -> p h d", d=64)

    def b4(s):
        return s[:, 0:4].unsqueeze(2).to_broadcast([128, 4, 64])

    tcount = [0]

    def do_tile(d, t):
        mk = masks[d]
        rows = slice(t * 128, (t + 1) * 128)
        i = tcount[0] % 2
        tcount[0] += 1
        srcs = (("r", f_r), ("v", f_v), ("kk", f_kk), ("kd", f_kd[d]), ("b", f_b[d]), ("lw", f_lw[d]))
        for n, src in srcs:
            p.dma("sp", ld[n][i][:], src[rows, :], reads=[fence], writes=[ld[n][i]])
        lw = ld["lw"][i]
        p.op("pool", "tensor_copy", out=lwh[:], in_=lw[:], reads=[lw], writes=[lwh])
        p.op("dve", "tensor_tensor", out=lwl[:], in0=lw[:], in1=lwh[:], op=ALU.subtract, reads=[lw, lwh], writes=[lwl])
        for (c0, mm) in ((0, mk["mib"]), (256, mk["msb"])):
            p.op("pe", "matmul", pcum[:, c0:c0 + 256], lhsT=mm[:, :], rhs=lwh[:, :], start=(c0 == 0), stop=False,
                 reads=[mm, lwh], writes=[pcum])
            p.op("pe", "matmul", pcum[:, c0:c0 + 256], lhsT=mm[:, :], rhs=lwl[:, :], start=False, stop=True,
                 reads=[mm, lwl], writes=[pcum])
        p.op("act", "activation", out=Pm["p"][:], in_=pcum[:, 0:256], func=AF.Exp, reads=[pcum], writes=[Pm["p"]])
        p.op("act", "activation", out=Pm["inv"][:], in_=pcum[:, 0:256], func=AF.Exp, scale=-1.0, reads=[pcum], writes=[Pm["inv"]])
        p.op("act", "activation", out=Pm["ex"][:], in_=pcum[:, 256:512], func=AF.Exp, reads=[pcum], writes=[Pm["ex"]])
        for h in range(NH):
            for j, lt in enumerate((lwh, lwl)):
                p.op("pe", "matmul", pHc[0:64, 64 + 2 * h:64 + 2 * h + 2], lhsT=lt[:, h * 64:(h + 1) * 64], rhs=cind[:, :],
                     start=(h == 0 and j == 0), stop=(j == 1), reads=[lt, cind], writes=[pHc])
        p.op("act", "activation", out=Pc[:], in_=pHc[0:64, 64:72], func=AF.Exp, reads=[pHc], writes=[Pc])
        p.op("dve", "tensor_tensor", out=rcb[:], in0=ld["r"][i][:], in1=Pm["p"][:], op=ALU.mult, reads=[ld["r"][i], Pm["p"]], writes=[rcb])
        p.op("pool", "tensor_tensor", out=khb[:], in0=ld["kd"][i][:], in1=Pm["inv"][:], op=ALU.mult,
             reads=[ld["kd"][i], Pm["inv"]], writes=[khb])
        p.op("dve", "scalar_tensor_tensor", out=nbb[:], in0=ld["b"][i][:], scalar=-1.0, in1=Pm["inv"][:], op0=ALU.mult, op1=ALU.mult,
             reads=[ld["b"][i], Pm["inv"]], writes=[nbb])
        p.op("pool", "tensor_copy", out=vb[:], in_=ld["v"][i][:], reads=[ld["v"][i]], writes=[vb])
        W = [Wall[h][0] for h in range(NH)]
        for h in range(NH):
            hs = slice(h * 64, (h + 1) * 64)
            p.op("dve" if h % 2 else "pool", "tensor_tensor", out=W[h][:, 64:128], in0=ld["kk"][i][:, hs], in1=Pm["ex"][:, hs],
                 op=ALU.mult, reads=[ld["kk"][i], Pm["ex"]], writes=[W[h]])
        for h in range(NH):
            hs = slice(h * 64, (h + 1) * 64)
            for j, (src, sl) in enumerate(((W[h], slice(64, 128)), (rcb, hs), (khb, hs), (nbb, hs))):
                p.op("pe", "transpose", pT[0:64, j * 128:(j + 1) * 128], src[:, sl], ident[:], reads=[src, ident], writes=[pT])
            p.op("act", "copy", out=FT[h][:, :, :], in_=pT[0:64, 0:512].rearrange("p (j n) -> p j n", n=128), reads=[pT], writes=[FT[h]])
        X = [Xm[h][0] for h in range(NH)]
        Y = [None] * NH
        for h in range(NH):
            hs = slice(h * 64, (h + 1) * 64)
            rhs2 = FT[h][:, 0:2, :].rearrange("p j n -> p (j n)")
            p.op("pe", "matmul", pA[:, 0:256], lhsT=FT[h][:, 3, :], rhs=rhs2, start=True, stop=True, reads=[FT[h]], writes=[pA])
            p.op("dve", "tensor_tensor", out=YN[h][:], in0=pA[:, 0:256], in1=mk["m2"][:], op=ALU.mult, reads=[pA, mk["m2"]], writes=[YN[h]])
            p.op("pe", "matmul", pB[:, 0:256], lhsT=FT[h][:, 2, :], rhs=rhs2, start=True, stop=True, reads=[FT[h]], writes=[pB])
            p.op("dve", "tensor_tensor", out=AG[h][:], in0=pB[:, 0:256], in1=mk["m2"][:], op=ALU.mult, reads=[pB, mk["m2"]], writes=[AG[h]])
            ps = nps()
            p.op("pe", "matmul", ps[:, 0:128], lhsT=FT[h][:, 0, :], rhs=FT[h][:, 3, :], start=True, stop=True, reads=[FT[h]], writes=[ps])
            p.op("dve", "tensor_tensor", out=X[h][:], in0=ps[:, 0:128], in1=mk["mst"][:], op=ALU.mult, reads=[ps, mk["mst"]], writes=[X[h]])
            ps = nps()
            p.op("pe", "matmul", ps[:, 0:64], lhsT=AG[h][:, 0:128], rhs=vb[:, hs], start=True, stop=True, reads=[AG[h], vb], writes=[ps])
            p.op("act", "copy", out=W[h][:, 0:64], in_=ps[:, 0:64], reads=[ps], writes=[W[h]])
        Ycur = [(YN[h], slice(0, 128)) for h in range(NH)]
        for j in range(6):
            for h in range(NH):
                yb, ysl = Ycur[h]
                wi, wo_ = Wall[h][j % 2], Wall[h][(j + 1) % 2]
                ps = nps()
                p.op("pe", "matmul", ps[:, 0:128], lhsT=yb[:, ysl], rhs=wi[:, :], start=True, stop=False, reads=[yb, wi], writes=[ps])
                p.op("pe", "matmul", ps[:, 0:128], lhsT=ident[:, :], rhs=wi[:, :], start=False, stop=True, reads=[ident, wi], writes=[ps])
                p.op("act" if h % 2 else "dve", "copy" if h % 2 else "tensor_copy", out=wo_[:], in_=ps[:, 0:128], reads=[ps], writes=[wo_])
            if j < 5:
                for h in range(NH):
                    yb, ysl = Ycur[h]
                    xi = Xm[h][j % 2]
                    xo, yo = Xm[h][(j + 1) % 2], Ym[h][(j + 1) % 2]
                    ps = nps()
                    p.op("pe", "matmul", ps[:, 0:128], lhsT=xi[:, :], rhs=yb[:, ysl], start=True, stop=True, reads=[xi, yb], writes=[ps])
                    p.op("act", "copy", out=yo[:], in_=ps[:, 0:128], reads=[ps], writes=[yo])
                    ps = nps()
                    p.op("pe", "matmul", ps[:, 0:128], lhsT=yb[:, ysl], rhs=xi[:, :], start=True, stop=True, reads=[xi, yb], writes=[ps])
                    p.op("dve", "tensor_copy", out=xo[:], in_=ps[:, 0:128], reads=[ps], writes=[xo])
                    Ycur[h] = (yo, slice(0, 128))
        TW = [Wall[h][0] for h in range(NH)]
        for h in range(NH):
            hs = slice(h * 64, (h + 1) * 64)
            ps = nps()
            p.op("pe", "matmul", ps[:, 0:128], lhsT=YN[h][:, 128:256], rhs=TW[h][:, :], start=True, stop=False, reads=[YN[h], TW[h]], writes=[ps])
            p.op("pe", "matmul", ps[:, 0:64], lhsT=AG[h][:, 128:256], rhs=vb[:, hs], start=False, stop=False, reads=[AG[h], vb], writes=[ps])
            p.op("pe", "matmul", ps[:, 64:128], lhsT=ident[:, :], rhs=rcb[:, hs], start=False, stop=True, reads=[ident, rcb], writes=[ps])
            p.op("act", "copy", out=Y0f[h][:], in_=ps[:, 0:64], reads=[ps], writes=[Y0f[h]])
            p.op("dve", "tensor_copy", out=M2b[h][:], in_=ps[:, 64:128], reads=[ps], writes=[M2b[h]])
            p.op("pe", "transpose", pT[0:64, 0:128], M2b[h][:, :], ident[:], reads=[M2b[h], ident], writes=[pT])
            p.op("act", "copy", out=M2T[h][:], in_=pT[0:64, 0:128], reads=[pT], writes=[M2T[h]])
            for c in range(2):
                cr = slice(c * 64, (c + 1) * 64)
                ps = nps()
                p.op("pe", "matmul", ps[0:64, 0:64], lhsT=TW[h][cr, 64:128], rhs=nbb[cr, hs], start=True, stop=True,
                     reads=[TW[h], nbb], writes=[ps])
                p.op("dve", "tensor_tensor", out=M3T[h][c][:], in0=ps[0:64, 0:64], in1=identf[0:64, 0:64], op=ALU.add,
                     reads=[ps, identf], writes=[M3T[h][c]])
                ps = nps()
                p.op("pe", "matmul", ps[0:64, 0:64], lhsT=khb[cr, hs], rhs=vb[cr, hs], start=True, stop=False, reads=[khb, vb], writes=[ps])
                p.op("pe", "matmul", ps[0:64, 0:64], lhsT=nbb[cr, hs], rhs=TW[h][cr, 0:64], start=False, stop=True,
                     reads=[nbb, TW[h]], writes=[ps])
                p.op("dve", "tensor_scalar", out=Z0P[h][c][:], in0=ps[0:64, 0:64], scalar1=Pc[:, 2 * h + c:2 * h + c + 1], scalar2=None,
                     op0=ALU.mult, reads=[ps, Pc], writes=[Z0P[h][c]])
        yt = ytile[ycnt[0] % 2]
        ycnt[0] += 1
        for c in ((0, 1) if d == 0 else (1, 0)):
            cr = slice(c * 64, (c + 1) * 64)
            for h in range(NH):
                hs = slice(h * 64, (h + 1) * 64)
                hh, hl, h32 = Hhi[d][h], Hlo[d][h], H32[d][h]
                p.op("pe", "matmul", pYc[cr, hs], lhsT=M2T[h][:, cr], rhs=hh[:, :], start=True, stop=False, reads=[M2T[h], hh], writes=[pYc])
                p.op("pe", "matmul", pYc[cr, hs], lhsT=M2T[h][:, cr], rhs=hl[:, :], start=False, stop=True, reads=[M2T[h], hl], writes=[pYc])
                p.op("pe", "matmul", pHc[0:64, 0:64], lhsT=M3T[h][c][:, :], rhs=hh[:, :], start=True, stop=False, reads=[M3T[h][c], hh], writes=[pHc])
                p.op("pe", "matmul", pHc[0:64, 0:64], lhsT=M3T[h][c][:, :], rhs=hl[:, :], start=False, stop=True, reads=[M3T[h][c], hl], writes=[pHc])
                p.op("dve", "tensor_tensor", out=yt[cr, hs], in0=pYc[cr, hs], in1=Y0f[h][cr, :], op=ALU.add, reads=[pYc, Y0f[h]], writes=[yt])
                p.op("dve", "scalar_tensor_tensor", out=h32[:], in0=pHc[0:64, 0:64], scalar=Pc[:, 2 * h + c:2 * h + c + 1], in1=Z0P[h][c][:],
                     op0=ALU.mult, op1=ALU.add, reads=[pHc, Pc, Z0P[h][c]], writes=[h32])
                p.op("act", "copy", out=hh[:], in_=h32[:], reads=[h32], writes=[hh])
                p.op("dve", "tensor_tensor", out=hl[:], in0=h32[:], in1=hh[:], op=ALU.subtract, reads=[h32, hh], writes=[hl])
        if d == 0:
            fy = p.dma("sp", f_y[rows, :], yt[:], reads=[yt])
            fence_y.r.append(fy)
        else:
            p.dma("sp", yf[:], f_y[rows, :], reads=[fence_y], writes=[yf])
            p.dma("sp", og["g"][:], f_g[rows, :], reads=[fence], writes=[og["g"]])
            p.dma("sp", og["bon"][:], f_bon[rows, :], reads=[fence], writes=[og["bon"]])
            p.op("pool", "tensor_tensor", out=yt[:], in0=yt[:], in1=yf[:], op=ALU.add, reads=[yt, yf], writes=[yt])
            p.op("dve", "tensor_reduce", out=st4[0][:], in_=v4(yt), axis=AX.X, op=ALU.add, reads=[yt], writes=[st4[0]])
            p.op("dve", "tensor_scalar", out=st4[0][:], in0=st4[0][:], scalar1=1.0 / 64, scalar2=None, op0=ALU.mult, reads=[st4[0]], writes=[st4[0]])
            p.op("dve", "tensor_tensor", out=v4(og["yc"]), in0=v4(yt), in1=b4(st4[0]), op=ALU.subtract, reads=[yt, st4[0]], writes=[og["yc"]])
            p.op("act", "activation", out=og["sq"][:], in_=og["yc"][:], func=AF.Square, reads=[og["yc"]], writes=[og["sq"]])
            p.op("dve", "tensor_reduce", out=st4[1][:], in_=v4(og["sq"]), axis=AX.X, op=ALU.add, reads=[og["sq"]], writes=[st4[1]])
            p.op("act", "activation", out=st4[1][:], in_=st4[1][:], func=AF.Sqrt, bias=eps_ln[:, 0:1], scale=1.0 / 64,
                 reads=[st4[1], eps_ln], writes=[st4[1]])
            p.op("dve", "reciprocal", out=st4[1][:], in_=st4[1][:], reads=[st4[1]], writes=[st4[1]])
            p.op("dve", "tensor_tensor", out=v4(og["yc"]), in0=v4(og["yc"]), in1=b4(st4[1]), op=ALU.mult, reads=[og["yc"], st4[1]], writes=[og["yc"]])
            p.op("pool", "tensor_tensor", out=og["yc"][:], in0=og["yc"][:], in1=lnwb[:], op=ALU.mult, reads=[og["yc"], lnwb], writes=[og["yc"]])
            p.op("pool", "tensor_tensor", out=og["yc"][:], in0=og["yc"][:], in1=lnbb[:], op=ALU.add, reads=[og["yc"], lnbb], writes=[og["yc"]])
            p.op("pool", "tensor_tensor", out=og["yc"][:], in0=og["yc"][:], in1=og["bon"][:], op=ALU.add, reads=[og["yc"], og["bon"]], writes=[og["yc"]])
            p.op("pool", "tensor_tensor", out=og["sq"][:], in0=og["yc"][:], in1=og["g"][:], op=ALU.mult, reads=[og["yc"], og["g"]], writes=[og["sq"]])
            outs.append(p.dma("sp", o_out[rows, :], og["sq"][:], reads=[og["sq"]]))

    fence_y = Buf(None, "fence_y")
    nlat = ntiles - 2
    order_f = [0, 1] + [2 + i for i in range(nlat)]
    order_b = [1, 0] + [2 + i for i in range(nlat - 1, -1, -1)]
    for t in order_f:
        do_tile(0, t)
    p.op("pool", "memset", fdum[:], 0.0, writes=[fence_y, fdum])
    for t in order_b:
        do_tile(1, t)
    p.emit(final_waits=outs)


def _shift_rows(a, k):
    out = np.zeros_like(a)
    if k == 1:
        out[1:] = a[:-1]
    else:
        out[:-1] = a[1:]
    return out


def rwkv_inputs(x_lat, x_ctx, c, c_ctx, mod_w, mod_b, n1g, P):
    maps = []
    for ci in range(8):
        b, hg = ci // 4, ci % 4
        cs = slice(hg * 256, (hg + 1) * 256)
        xa = np.concatenate([x_ctx[b], x_lat[b]], 0)
        xp = np.concatenate([_shift_rows(x_ctx[b], 1), _shift_rows(x_lat[b], 1)], 0)
        xn = np.concatenate([_shift_rows(x_ctx[b], -1), _shift_rows(x_lat[b], -1)], 0)
        mpn = np.ones((RT_TOK, 2), np.float32)
        mpn[0, 0] = 0
        mpn[256, 0] = 0
        mpn[255, 1] = 0
        mpn[RT_TOK - 1, 1] = 0
        vecs = np.stack([P["w0"][0][cs], P["w0"][1][cs], P["a0"][0][cs], P["a0"][1][cs], P["k_k"][cs], P["k_a"][cs],
                         P["r_k"].reshape(-1)[cs], P["ln_w"][cs], P["ln_b"][cs]], 0)
        maps.append(dict(
            xa=np.ascontiguousarray(xa), xp=np.ascontiguousarray(xp), xn=np.ascontiguousarray(xn), mpn=mpn,
            cnd=_cnd(c[b], c_ctx), modw=np.ascontiguousarray(mod_w[:, 0:2048]), modb=np.ascontiguousarray(mod_b[None, 0:2048]),
            n1g=np.ascontiguousarray(n1g[None, :]), mu=np.ascontiguousarray(P["mu"]),
            w_r=np.ascontiguousarray(P["w_r"][:, cs]), w_k=np.ascontiguousarray(P["w_k"][:, cs]),
            w_v=np.ascontiguousarray(P["w_v"][:, cs]), g1=np.ascontiguousarray(P["g1"]),
            g2=np.ascontiguousarray(P["g2"][:, cs]),
            w1=np.ascontiguousarray(np.concatenate([P["w1"][0], P["w1"][1]], 1)),
            w2=np.ascontiguousarray(np.concatenate([P["w2"][0][:, cs], P["w2"][1][:, cs]], 1)),
            a1=np.ascontiguousarray(np.concatenate([P["a1"][0], P["a1"][1]], 1)),
            a2=np.ascontiguousarray(np.concatenate([P["a2"][0][:, cs], P["a2"][1][:, cs]], 1)),
            vecs=np.ascontiguousarray(vecs.astype(np.float32))))
    return maps


_PROGS = {}


def _prog(key, fn):
    if key not in _PROGS:
        _PROGS[key] = fn()
    return _PROGS[key]


def _run(nc, maps):
    return run_bass_kernel_spmd(nc, maps, core_ids=list(range(8))).results


def kernel(x, c, ctx, c_ctx, mod_w, mod_b, norm1_g, norm2_g, a_w_qkv, a_w_o, a_q_gain, a_k_gain,
           b_mu, b_w_r, b_w_k, b_w_v, b_w_o, b_decay_w0, b_decay_w1, b_decay_w2, b_iclr_a0, b_iclr_a1,
           b_iclr_a2, b_gate_g1, b_gate_g2, b_k_k, b_k_a, b_r_k, b_ln_w, b_ln_b, c_w_qkv, c_w_o, c_sink,
           moe_w_group, moe_b_group, moe_w_expert, moe_b_expert, moe_w1, moe_w3, moe_w2, final_g):
    f32 = lambda a: np.ascontiguousarray(np.asarray(a, dtype=np.float32))
    x_lat, x_ctx = f32(x), f32(ctx)
    c, c_ctx = f32(c), f32(c_ctx)
    mod_w, mod_b = f32(mod_w), f32(mod_b)
    y_last = None
    for l in range(4):
        kind, j = l % 3, l // 3
        mw, mb = mod_w[l], mod_b[l]
        n1g, n2g = f32(norm1_g[l]), f32(norm2_g[l])
        if kind == 0:
            nc = _prog("attn_d", lambda: build_attn(True))
            maps = attn_inputs(True, x_lat, x_ctx, c, c_ctx, mw, mb, n1g, f32(a_w_qkv[j]), f32(a_q_gain[j]),
                               f32(a_k_gain[j]), np.zeros(16, np.float32))
            res = _run(nc, maps)
            wo = f32(a_w_o[j])
        elif kind == 2:
            nc = _prog("attn_w", lambda: build_attn(False))
            ones = np.ones(64, np.float32)
            maps = attn_inputs(False, x_lat, x_ctx, c, c_ctx, mw, mb, n1g, f32(c_w_qkv[j]), ones, ones, f32(c_sink[j]))
            res = _run(nc, maps)
            wo = f32(c_w_o[j])
        else:
            nc = _prog("rwkv", lambda: build_rwkv())
            P = dict(mu=f32(b_mu[j]), w_r=f32(b_w_r[j]), w_k=f32(b_w_k[j]), w_v=f32(b_w_v[j]), w0=f32(b_decay_w0[j]),
                     w1=f32(b_decay_w1[j]), w2=f32(b_decay_w2[j]), a0=f32(b_iclr_a0[j]), a1=f32(b_iclr_a1[j]),
                     a2=f32(b_iclr_a2[j]), g1=f32(b_gate_g1[j]), g2=f32(b_gate_g2[j]), k_k=f32(b_k_k[j]), k_a=f32(b_k_a[j]),
                     r_k=f32(b_r_k[j]), ln_w=f32(b_ln_w[j]), ln_b=f32(b_ln_b[j]))
            maps = rwkv_inputs(x_lat, x_ctx, c, c_ctx, mw, mb, n1g, P)
            res = _run(nc, maps)
            wo = f32(b_w_o[j])
        o_lat = np.zeros((2, 8192, 1024), np.float32)
        o_ctx = np.zeros((2, 256, 1024), np.float32)
        if kind == 1:
            for ci in range(8):
                b, hg = ci // 4, ci % 4
                o = res[ci]["o_out"]
                o_ctx[b][:, hg * 256:(hg + 1) * 256] = o[:256]
                o_lat[b][:, hg * 256:(hg + 1) * 256] = o[256:]
        else:
            for ci in range(8):
                b, qi = ci // 4, ci % 4
                o_lat[b, qi * 2048:(qi + 1) * 2048] = res[ci]["o_lat"]
                if qi == 0:
                    o_ctx[b] = res[ci]["o_ctx"]
        del res
        nc1 = _prog("post1", build_post1)
        res1 = _run(nc1, post1_inputs(x_lat, x_ctx, o_lat, o_ctx, c, c_ctx, mw, mb, n2g, wo, f32(moe_w_group[l]),
                                      f32(moe_b_group[l]), f32(moe_w_expert[l]), f32(moe_b_expert[l])))
        nc2 = _prog("post2", build_post2)
        res2 = _run(nc2, post2_inputs(res1, c, c_ctx, mw, mb, f32(final_g), f32(moe_w1[l]), f32(moe_w3[l]), f32(moe_w2[l])))
        x_lat, x_ctx = _unrows(res2, "x_out")
        if l == 3:
            y_last, _ = _unrows(res2, "y_fin")
    return y_last
```

```python
import contextlib
import numpy as np
import concourse.bass as bass
import concourse.mybir as mybir
from concourse.bass_utils import run_bass_kernel_spmd

F32 = mybir.dt.float32
BF16 = mybir.dt.bfloat16
I32 = mybir.dt.int32
AF = mybir.ActivationFunctionType
ALU = mybir.AluOpType
AX = mybir.AxisListType


class Buf:
    __slots__ = ("ap", "w", "r", "name", "psum")

    def __init__(self, ap, name="", psum=False):
        self.psum = psum
        self.ap = ap
        self.w = None
        self.r = []
        self.name = name

    def __getitem__(self, idx):
        return self.ap[idx]


class Ins:
    __slots__ = ("eng", "fn", "deps", "is_dma", "idx", "slot", "target", "need", "cnt", "prev_slot")

    def __init__(self, eng, fn, is_dma):
        self.eng = eng
        self.fn = fn
        self.deps = {}
        self.is_dma = is_dma
        self.need = False
        self.cnt = 0
        self.slot = None
        self.target = 0
        self.prev_slot = None


COMPUTE = ("pe", "dve", "act", "pool")
_DBG = {}
NSLOT = 8


class Prog:
    def __init__(self, nc):
        self.nc = nc
        self.es = contextlib.ExitStack()
        self.q = {e: [] for e in ("pe", "dve", "act", "pool", "sp")}
        self.dma_count = {e: 0 for e in self.q}
        self.slot_last = {}
        self.n_sb = 0

    def sb(self, shape, dt, name=None):
        self.n_sb += 1
        t = self.es.enter_context(self.nc.sbuf_tensor(name or f"sb{self.n_sb}", list(shape), dt))
        return t

    def ps(self, shape, dt, name=None):
        self.n_sb += 1
        t = self.es.enter_context(self.nc.psum_tensor(name or f"ps{self.n_sb}", list(shape), dt))
        return t

    def buf(self, shape, dt, name=None):
        return Buf(self.sb(shape, dt, name), name or "")

    def pbuf(self, shape, dt, name=None):
        return Buf(self.ps(shape, dt, name), name or "", psum=True)

    def _deps(self, ins, reads, writes):
        cand = []
        for b in reads:
            if b.w is not None:
                cand.append(b.w)
            if b.psum:
                cand.extend(x for x in b.r if x.eng != ins.eng)
        for b in writes:
            if b.w is not None:
                cand.append(b.w)
            cand.extend(b.r)
        for d in cand:
            if d is ins:
                continue
            if d.is_dma:
                ins.deps[("dma", id(d))] = d
            else:
                if d.eng == "pe" and ins.eng == "pe" and not ins.is_dma:
                    continue
                k = ("c", d.eng)
                if k not in ins.deps or ins.deps[k].idx < d.idx:
                    ins.deps[k] = d
        for b in reads:
            b.r.append(ins)
        for b in writes:
            b.w = ins
            b.r = []

    def op(self, eng, name, *args, reads=(), writes=(), **kw):
        fn = (lambda e: getattr(e, name)(*args, **kw))
        ins = Ins(eng, fn, False)
        ins.idx = len(self.q[eng])
        self._deps(ins, reads, writes)
        self.q[eng].append(ins)
        return ins

    def dma(self, eng, out, in_, reads=(), writes=(), **kw):
        ins = Ins(eng, lambda e: e.dma_start(out=out, in_=in_, **kw), True)
        ins.idx = len(self.q[eng])
        k = self.dma_count[eng]
        self.dma_count[eng] += 1
        ins.slot = (eng, k % NSLOT)
        ins.target = 16 * (k // NSLOT + 1)
        ins.prev_slot = self.slot_last.get(ins.slot)
        self.slot_last[ins.slot] = ins
        self._deps(ins, reads, writes)
        self.q[eng].append(ins)
        return ins

    def emit(self, final_waits=()):
        nc = self.nc
        es = self.es
        sem = {e: es.enter_context(nc.semaphore(f"s_{e}")) for e in COMPUTE}
        dsem = {}
        for e in ("sp", "pool", "act"):
            if self.dma_count[e]:
                for s in range(NSLOT):
                    dsem[(e, s)] = es.enter_context(nc.semaphore(f"d_{e}{s}"))
        for e, lst in self.q.items():
            for ins in lst:
                for k, d in ins.deps.items():
                    if k[0] == "c":
                        d.need = True
        for e in COMPUTE:
            c = 0
            for ins in self.q[e]:
                if ins.is_dma:
                    continue
                if ins.need:
                    c += 1
                    ins.cnt = c
        engobj = {"pe": "tensor", "dve": "vector", "act": "scalar", "pool": "gpsimd", "sp": "sync"}
        final = list(final_waits)
        block = es.enter_context(nc.Block())

        def run(e):
            def body(eng):
                waited = {}
                def wait(s, key, v):
                    if waited.get(key, 0) >= v:
                        return
                    waited[key] = v
                    eng.wait_ge(s, v)
                for ins in self.q[e]:
                    if ins.is_dma and ins.prev_slot is not None:
                        wait(dsem[ins.slot], ("d",) + ins.slot, ins.prev_slot.target)
                    for k, d in ins.deps.items():
                        if k[0] == "c":
                            wait(sem[d.eng], ("c", d.eng), d.cnt)
                        else:
                            wait(dsem[d.slot], ("d",) + d.slot, d.target)
                    bi = ins.fn(eng)
                    if ins.is_dma:
                        bi.then_inc(dsem[ins.slot], 16)
                    elif ins.need:
                        bi.then_inc(sem[e], 1)
                if e == "sp":
                    for d in final:
                        wait(dsem[d.slot], ("d",) + d.slot, d.target)
            return body
        for e in ("sp", "pe", "dve", "act", "pool"):
            if not self.q[e] and e != "sp":
                continue
            getattr(block, engobj[e])(run(e))
        es.close()


def mk_ident(p, dt_list=(BF16,)):
    identf = p.buf([128, 128], F32, "identf")
    p.op("pool", "memset", identf[:], 0.0, writes=[identf])
    p.op("pool", "affine_select", out=identf[:], in_=identf[:], pattern=[[-1, 128]],
                                           compare_op=ALU.not_equal, fill=1.0, base=0, channel_multiplier=1,
         reads=[identf], writes=[identf])
    outs = {F32: identf}
    for d in dt_list:
        if d == F32:
            continue
        t = p.buf([128, 128], d, "identb")
        p.op("dve", "tensor_copy", out=t[:], in_=identf[:], reads=[identf], writes=[t])
        outs[d] = t
    return outs


def mk_sel2(p):
    sels = []
    for r in range(2):
        s = p.buf([2, 128], F32, f"sel{r}")
        p.op("pool", "memset", s[:], 1.0, writes=[s])
        p.op("pool", "affine_select", out=s[:], in_=s[:], pattern=[[0, 128]],
                                                         compare_op=ALU.is_equal, fill=0.0, base=-r, channel_multiplier=1,
             reads=[s], writes=[s])
        sels.append(s)
    return sels


def modulation(p, cnd, modw, modb, ncol, ps_small, stage, consumer):
    sc = p.buf([128, 8, 2], F32, "sc")
    p.dma("sp", sc[:], cnd[:, :, :], writes=[sc])
    p.op("act", "activation", out=sc[:], in_=sc[:], func=AF.Silu, reads=[sc], writes=[sc])
    Mc = p.buf([2, 512], F32, "Mc")
    mb = p.buf([2, 512], F32, "mb")
    for c in range(ncol // 512):
        p.dma("sp", mb[:], modb[0:1, c * 512:(c + 1) * 512].to_broadcast([2, 512]), writes=[mb])
        stl = stage if isinstance(stage, list) else [stage]
        nper = 8 // len(stl)
        for si, st in enumerate(stl):
            p.dma("sp", st[:, 0:nper * 512].rearrange("p (k n) -> p k n", n=512),
                  modw[si * nper * 128:(si + 1) * nper * 128, c * 512:(c + 1) * 512].rearrange("(k q) n -> q k n", q=128),
                  writes=[st])
        for k in range(8):
            st = stl[k // nper]
            p.op("pe", "matmul", ps_small[0:2, 0:512], lhsT=sc[:, k, :], rhs=st[:, (k % nper) * 512:(k % nper + 1) * 512],
                 start=(k == 0), stop=(k == 7), reads=[sc, st], writes=[ps_small])
        p.op("dve", "tensor_tensor", out=Mc[:], in0=ps_small[0:2, 0:512], in1=mb[:], op=ALU.add,
             reads=[ps_small, mb], writes=[Mc])
        consumer(c, Mc)


def bcast_chunk(p, sels, src, dst_lat, dst_ctx, col0, ps):
    for r, dst in ((0, dst_lat), (1, dst_ctx)):
        if dst is None:
            continue
        p.op("pe", "matmul", ps[:, 0:512], lhsT=sels[r][:, :], rhs=src[:, :], start=True, stop=True,
             reads=[sels[r], src], writes=[ps])
        p.op("act", "copy", out=dst[:, col0:col0 + 512], in_=ps[:, 0:512], reads=[ps], writes=[dst])


def bcast_rows(p, sels, src, col0, dst_lat, dst_ctx, ps):
    for r, dst in ((0, dst_lat), (1, dst_ctx)):
        if dst is None:
            continue
        for c in range(2):
            p.op("pe", "matmul", ps[:, 0:512], lhsT=sels[r][:, :],
                                                    rhs=src[:, col0 + c * 512: col0 + (c + 1) * 512],
                                                    start=True, stop=True,
                 reads=[sels[r], src], writes=[ps])
            p.op("act", "copy", out=dst[:, c * 512:(c + 1) * 512], in_=ps[:, 0:512],
                 reads=[ps], writes=[dst])


def build_attn(dense):
    NKV = 4 if dense else 2
    KVW = NKV * 64
    QKVC = 1024 + 2 * KVW
    NKT = 64 if dense else 18
    NT = 2 + NKT
    GRP = 16 // NKV
    nc = bass.Bass("TRN2", target_bir_lowering=False)

    def din(name, shape, dt=F32):
        return nc.dram_tensor(name, list(shape), dt, kind="ExternalInput").ap()
    xkv = din("xkv", [NKT * 128, 1024])
    xq = din("xq", [2048, 1024])
    xc = din("xc", [256, 1024])
    cnd = din("cnd", [128, 8, 2])
    modw = din("modw", [1024, 2048])
    modb = din("modb", [1, 2048])
    n1g = din("n1g", [1, 1024])
    wqkv = din("wqkv", [1024, QKVC])
    qg = din("qg", [1, 64])
    kg = din("kg", [1, 64])
    sink = din("sink", [1, 16])
    cos_kv = din("cos_kv", [NKT * 128, 64])
    sin_kv = din("sin_kv", [NKT * 128, 64])
    cos_q = din("cos_q", [2048, 64])
    sin_q = din("sin_q", [2048, 64])
    kvalid = din("kvalid", [128, NT])
    o_lat = nc.dram_tensor("o_lat", [2048, 1024], F32, kind="ExternalOutput").ap()
    o_ctx = nc.dram_tensor("o_ctx", [256, 1024], F32, kind="ExternalOutput").ap()

    p = Prog(nc)
    ident = mk_ident(p)[BF16]
    sels = mk_sel2(p)
    pT = p.pbuf([128, 1024], BF16, "pT")
    pq = p.pbuf([128, 1024], F32, "pq")
    pkv = p.pbuf([128, 512], F32, "pkv")
    pss = [p.pbuf([128, 512], F32, f"pss{i}") for i in range(2)]
    pacc = [p.pbuf([128, 512], F32, f"pacc{i}") for i in range(2)]

    stage = p.buf([128, 4096], F32, "stage")
    g2 = p.buf([2, 1024], F32, "g2")
    p.dma("sp", g2[:], n1g[0:1, :].to_broadcast([2, 1024]), writes=[g2])
    Gvc = p.buf([2, 512], F32, "Gvc")
    Gb = [p.buf([128, 1024], F32, f"Gb{r}") for r in range(2)]
    Sb = [p.buf([128, 1024], F32, f"Sb{r}") for r in range(2)]

    def mod_consumer(c, Mc):
        if c < 2:
            bcast_chunk(p, sels, Mc, Sb[0], Sb[1], c * 512, pq)
        else:
            cc = c - 2
            p.op("dve", "scalar_tensor_tensor", out=Gvc[:], in0=Mc[:], scalar=1.0, in1=g2[:, cc * 512:(cc + 1) * 512],
                 op0=ALU.add, op1=ALU.mult, reads=[Mc, g2], writes=[Gvc])
            bcast_chunk(p, sels, Gvc, Gb[0], Gb[1], cc * 512, pq)
    modulation(p, cnd, modw, modb, 2048, pkv, stage, mod_consumer)

    eps = p.buf([128, 1], F32, "eps")
    p.op("pool", "memset", eps[:], 1e-6, writes=[eps])
    qgb = p.buf([128, 64], F32, "qgb")
    kgb = p.buf([128, 64], F32, "kgb")
    p.dma("sp", qgb[:], qg[0:1, :].to_broadcast([128, 64]), writes=[qgb])
    p.dma("sp", kgb[:], kg[0:1, :].to_broadcast([128, 64]), writes=[kgb])
    esink = p.buf([128, 16], F32, "esink")
    p.dma("sp", esink[:], sink[0:1, :].to_broadcast([128, 16]), writes=[esink])
    p.op("act", "activation", out=esink[:], in_=esink[:], func=AF.Exp, reads=[esink], writes=[esink])
    kval = p.buf([128, NT], F32, "kval")
    p.dma("sp", kval[:], kvalid[:, :], writes=[kval])

    wb = p.buf([128, 8, QKVC], BF16, "wb")
    for k in range(8):
        p.dma("sp", stage[:, 0:QKVC], wqkv[k * 128:(k + 1) * 128, :], writes=[stage])
        p.op("pool", "tensor_copy", out=wb[:, k, :], in_=stage[:, 0:QKVC], reads=[stage], writes=[wb])

    mprev = p.buf([128, 128], BF16, "mprev")
    mnext = p.buf([128, 128], BF16, "mnext")
    if not dense:
        mf = p.buf([128, 128], F32, "mf")
        for (dst, sg) in ((mprev, 1), (mnext, -1)):
            p.op("pool", "memset", mf[:], 1.0, writes=[mf])
            p.op("pool", "affine_select", out=mf[:], in_=mf[:], pattern=[[-sg, 128]], compare_op=ALU.is_ge,
                                                            fill=0.0, base=0, channel_multiplier=sg,
                 reads=[mf], writes=[mf])
            p.op("dve", "tensor_copy", out=dst[:], in_=mf[:], reads=[mf], writes=[dst])

    KT = p.sb([128, NKV // 2, NT * 128], BF16, "KT")
    VE = p.sb([128, NT, NKV, 65], BF16, "VE")
    KTb = [Buf(KT, f"KT{t}") for t in range(NT)]
    VEb = [Buf(VE, f"VE{t}") for t in range(NT)]
    veall = Buf(VE, "VEall")
    ins0 = p.op("pool", "memset", VE[:, :, :, 64:65], 1.0, writes=[veall] + VEb)
    QT = [p.buf([128, 8, 512], BF16, f"QT{i}") for i in range(2)]

    xt = [p.buf([128, 1024], F32, f"xt{i}") for i in range(2)]
    qn = p.buf([128, 1024], F32, "qn")
    t1 = p.buf([128, 1024], F32, "t1")
    qr = p.buf([128, 1024], BF16, "qr")
    cs = [p.buf([128, 64], F32, f"cs{i}") for i in range(2)]
    sn = [p.buf([128, 64], F32, f"sn{i}") for i in range(2)]
    WS = []
    for wi in range(2):
        WS.append(dict(junk=p.buf([128, 1024], F32, f"junk{wi}"), ss=p.buf([128, 1], F32, f"ss{wi}"),
                       hn=p.buf([128, 1024], F32, f"hn{wi}"), hb=p.buf([128, 1024], BF16, f"hb{wi}"),
                       hT=p.buf([128, 1024], BF16, f"hT{wi}"), ssq=p.buf([128, 16], F32, f"ssq{wi}"),
                       kn=p.buf([128, KVW], F32, f"kn{wi}"), k1=p.buf([128, KVW], F32, f"k1{wi}"),
                       kr=p.buf([128, KVW], BF16, f"kr{wi}")))
    W_ = dict(WS[0])
    cnt = [0]

    def headnorm(src_ps, dst, nh, gainb):
        w = nh * 64
        if not dense:
            p.op("act", "copy", out=dst[:, 0:w], in_=src_ps, reads=[srcbuf[0]], writes=[dst])
            return
        p.op("act", "activation", out=W_["junk"][:, 0:w], in_=src_ps, func=AF.Square, reads=[srcbuf[0]], writes=[W_["junk"]])
        p.op("dve", "tensor_reduce", out=W_["ssq"][:, 0:nh], in_=W_["junk"][:, 0:w].rearrange("p (h d) -> p h d", d=64),
                                              axis=AX.X, op=ALU.add, reads=[W_["junk"]], writes=[W_["ssq"]])
        p.op("act", "activation", out=W_["ssq"][:, 0:nh], in_=W_["ssq"][:, 0:nh], func=AF.Sqrt, bias=eps[:, 0:1], scale=1.0 / 64,
             reads=[W_["ssq"], eps], writes=[W_["ssq"]])
        p.op("dve", "reciprocal", out=W_["ssq"][:, 0:nh], in_=W_["ssq"][:, 0:nh], reads=[W_["ssq"]], writes=[W_["ssq"]])
        p.op("dve", "tensor_tensor", out=dst[:, 0:w].rearrange("p (h d) -> p h d", d=64),
                                              in0=src_ps.rearrange("p (h d) -> p h d", d=64),
                                              in1=W_["ssq"][:, 0:nh].unsqueeze(2).to_broadcast([128, nh, 64]), op=ALU.mult,
             reads=[srcbuf[0], W_["ssq"]], writes=[dst])
        p.op("pool", "tensor_tensor", out=dst[:, 0:w].rearrange("p (h d) -> p h d", d=64),
                                               in0=dst[:, 0:w].rearrange("p (h d) -> p h d", d=64),
                                               in1=gainb[:, :].unsqueeze(1).to_broadcast([128, nh, 64]), op=ALU.mult,
             reads=[dst, gainb], writes=[dst])
    srcbuf = [None]

    def rope(src, tmp, dst, nh, c_t, s_t):
        w = nh * 64
        v3 = lambda t: t[:, 0:w].rearrange("p (h d) -> p h d", d=64)
        v4 = lambda t: t[:, 0:w].rearrange("p (h a t d) -> p (h a) t d", a=2, t=2, d=16)
        s4 = s_t[:, :].rearrange("p (a t d) -> p a t d", a=2, t=2, d=16)
        p.op("dve", "tensor_tensor", out=v3(tmp), in0=v3(src), in1=c_t[:, :].unsqueeze(1).to_broadcast([128, nh, 64]),
                                              op=ALU.mult, reads=[src, c_t], writes=[tmp])
        for tt in range(2):
            p.op("pool", "tensor_tensor", out=W_["junk"][:, 0:w].rearrange("p (h a t d) -> p h a t d", a=2, t=2, d=16)[:, :, :, tt, :],
                in0=src[:, 0:w].rearrange("p (h a t d) -> p h a t d", a=2, t=2, d=16)[:, :, :, 1 - tt, :],
                in1=s4[:, :, tt, :].unsqueeze(1).to_broadcast([128, nh, 2, 16]), op=ALU.mult,
                reads=[src, s_t], writes=[W_["junk"]])
        p.op("dve", "tensor_tensor", out=dst[:, 0:w], in0=tmp[:, 0:w], in1=W_["junk"][:, 0:w], op=ALU.add,
             reads=[tmp, W_["junk"]], writes=[dst])

    def proc_tile(rows_ap, r, need_q, need_kv, tix, qt_buf, qcol, cos_ap, sin_ap):
        i = cnt[0] % 2
        cnt[0] += 1
        W_.update(WS[i])
        x = xt[i]
        p.dma("sp", x[:], rows_ap, writes=[x])
        if r == 0:
            p.dma("sp", cs[i][:], cos_ap, writes=[cs[i]])
            p.dma("sp", sn[i][:], sin_ap, writes=[sn[i]])
        p.op("act", "activation", out=W_["junk"][:], in_=x[:], func=AF.Square, accum_out=W_["ss"][:], reads=[x], writes=[W_["junk"], W_["ss"]])
        p.op("act", "activation", out=W_["ss"][:], in_=W_["ss"][:], func=AF.Sqrt, bias=eps[:, 0:1], scale=1.0 / 1024,
             reads=[W_["ss"], eps], writes=[W_["ss"]])
        p.op("dve", "reciprocal", out=W_["ss"][:], in_=W_["ss"][:], reads=[W_["ss"]], writes=[W_["ss"]])
        p.op("dve", "scalar_tensor_tensor", out=W_["hn"][:], in0=x[:], scalar=W_["ss"][:, 0:1], in1=Gb[r][:],
                                                     op0=ALU.mult, op1=ALU.mult, reads=[x, W_["ss"], Gb[r]], writes=[W_["hn"]])
        p.op("pool", "tensor_tensor", out=W_["hb"][:], in0=W_["hn"][:], in1=Sb[r][:], op=ALU.add, reads=[W_["hn"], Sb[r]], writes=[W_["hb"]])
        for k in range(8):
            p.op("pe", "transpose", pT[:, k * 128:(k + 1) * 128], W_["hb"][:, k * 128:(k + 1) * 128], ident[:],
                 reads=[W_["hb"], ident], writes=[pT])
        p.op("act", "copy", out=W_["hT"][:], in_=pT[:], reads=[pT], writes=[W_["hT"]])
        if need_q:
            for c in range(2):
                for k in range(8):
                    p.op("pe", "matmul", pq[:, c * 512:(c + 1) * 512], lhsT=W_["hT"][:, k * 128:(k + 1) * 128],
                                                            rhs=wb[:, k, c * 512:(c + 1) * 512], start=(k == 0), stop=(k == 7),
                         reads=[W_["hT"], wb], writes=[pq])
            srcbuf[0] = pq
            headnorm(pq[:, :], qn, 16, qgb)
            if r == 0:
                rope(qn, t1, qr, 16, cs[i], sn[i])
            else:
                p.op("dve", "tensor_copy", out=qr[:], in_=qn[:], reads=[qn], writes=[qr])
            for j in range(8):
                p.op("pe", "transpose", pT[:, j * 128:(j + 1) * 128], qr[:, j * 128:(j + 1) * 128], ident[:],
                     reads=[qr, ident], writes=[pT])
            p.op("act", "copy", out=qt_buf[:, :, qcol:qcol + 128], in_=pT[:, :].rearrange("p (j n) -> p j n", n=128),
                 reads=[pT], writes=[qt_buf])
        if need_kv:
            for k in range(8):
                p.op("pe", "matmul", pkv[:, 0:2 * KVW], lhsT=W_["hT"][:, k * 128:(k + 1) * 128],
                                                   rhs=wb[:, k, 1024:1024 + 2 * KVW], start=(k == 0), stop=(k == 7),
                     reads=[W_["hT"], wb], writes=[pkv])
            srcbuf[0] = pkv
            headnorm(pkv[:, 0:KVW], W_["kn"], NKV, kgb)
            if r == 0:
                rope(W_["kn"], W_["k1"], W_["kr"], NKV, cs[i], sn[i])
            else:
                p.op("dve", "tensor_copy", out=W_["kr"][:], in_=W_["kn"][:], reads=[W_["kn"]], writes=[W_["kr"]])
            nch = KVW // 128
            for j in range(nch):
                p.op("pe", "transpose", pT[:, j * 128:(j + 1) * 128], W_["kr"][:, j * 128:(j + 1) * 128], ident[:],
                     reads=[W_["kr"], ident], writes=[pT])
            p.op("act", "copy", out=KT[:, :, tix * 128:(tix + 1) * 128],
                                         in_=pT[:, 0:nch * 128].rearrange("p (j n) -> p j n", n=128),
                 reads=[pT], writes=[KTb[tix]])
            p.op("act", "copy", out=VE[:, tix, :, 0:64], in_=pkv[:, KVW:2 * KVW].rearrange("p (g d) -> p g d", d=64),
                 reads=[pkv], writes=[VEb[tix]])
            if not dense and r == 0:
                p.op("dve", "tensor_scalar", out=VE[:, tix, :, :], in0=VE[:, tix, :, :], scalar1=kval[:, tix:tix + 1],
                                                      scalar2=None, op0=ALU.mult, reads=[VEb[tix], kval], writes=[VEb[tix]])

    for t in range(2):
        proc_tile(xc[t * 128:(t + 1) * 128, :], 1, False, True, t, None, 0, None, None)
    for t in range(NKT):
        proc_tile(xkv[t * 128:(t + 1) * 128, :], 0, False, True, 2 + t, None, 0,
                  cos_kv[t * 128:(t + 1) * 128, :], sin_kv[t * 128:(t + 1) * 128, :])

    pts = [p.buf([128, 512], BF16, f"pt{i}") for i in range(3)]
    otile = [Buf(stage.ap[:, i * 1024:(i + 1) * 1024], f"ot{i}") for i in range(4)]
    rden = p.buf([128, 1], F32, "rden")
    outs = []
    state = {"mm": 0, "pt": 0, "acc": 0, "blk": 0}

    def attn_block(qtiles, keylists, out_aps, is_ctx):
        b = state["blk"] % 2
        state["blk"] += 1
        qt_buf = QT[b]
        for qi in range(qtiles):
            if is_ctx:
                proc_tile(xc[qi * 128:(qi + 1) * 128, :], 1, True, False, 0, qt_buf, qi * 128, None, None)
            else:
                r0 = out_aps[qi][1]
                proc_tile(xq[r0:r0 + 128, :], 0, True, False, 0, qt_buf, qi * 128,
                          cos_q[r0:r0 + 128, :], sin_q[r0:r0 + 128, :])
        same = all(keylists[qi] == keylists[0] for qi in range(qtiles))
        groups = [list(range(qtiles))] if same else [[qi] for qi in range(qtiles)]
        for h in range(16):
            half, j = h // 8, h % 8
            g = h // GRP
            ki = g % (NKV // 2)
            ps_rows = slice(half * 64, half * 64 + 64)
            pa = pacc[state["acc"] % 2]
            state["acc"] += 1
            for grp in groups:
                kl = keylists[grp[0]]
                c0, c1 = grp[0] * 128, (grp[-1] + 1) * 128
                n = c1 - c0
                def emit_mm2(idx, kt, pt):
                    for qq, qi in enumerate(grp):
                        p.op("pe", "matmul", pa[:, qi * 128:qi * 128 + 65], lhsT=pt[:, qq * 128:(qq + 1) * 128], rhs=VE[:, kt, g, :],
                             start=(idx == 0 and qq == 0 and grp is groups[0]), stop=(idx == len(kl) - 1),
                             reads=[pt, VEb[kt]], writes=[pa])
                pend = None
                for idx, (kt, mask) in enumerate(kl):
                    ps_ = pss[state["mm"] % 2]
                    state["mm"] += 1
                    pt = pts[state["pt"] % 3]
                    state["pt"] += 1
                    p.op("pe", "matmul", ps_[:, 0:n], lhsT=KT[ps_rows, ki, kt * 128:(kt + 1) * 128],
                         rhs=qt_buf[ps_rows, j, c0:c1], start=True, stop=True, reads=[KTb[kt], qt_buf], writes=[ps_])
                    p.op("act", "activation", out=pt[:, 0:n], in_=ps_[:, 0:n], func=AF.Exp, scale=0.125, reads=[ps_], writes=[pt])
                    if mask is not None:
                        p.op("dve", "tensor_tensor", out=pt[:, 0:n], in0=pt[:, 0:n], in1=mask[:, :], op=ALU.mult,
                             reads=[pt, mask], writes=[pt])
                    if pend is not None:
                        emit_mm2(*pend)
                    pend = (idx, kt, pt)
                emit_mm2(*pend)
            for qi in range(qtiles):
                ot = out_aps[qi][2]
                if dense:
                    p.op("dve", "reciprocal", out=rden[:], in_=pa[:, qi * 128 + 64:qi * 128 + 65],
                         reads=[pa], writes=[rden])
                else:
                    p.op("dve", "tensor_tensor", out=rden[:], in0=pa[:, qi * 128 + 64:qi * 128 + 65],
                                                                 in1=esink[:, h:h + 1], op=ALU.add,
                         reads=[pa, esink], writes=[rden])
                    p.op("dve", "reciprocal", out=rden[:], in_=rden[:], reads=[rden], writes=[rden])
                p.op("dve", "tensor_scalar", out=ot[:, h * 64:(h + 1) * 64], in0=pa[:, qi * 128:qi * 128 + 64],
                                                                    scalar1=rden[:, 0:1], scalar2=None, op0=ALU.mult,
                     reads=[pa, rden], writes=[ot])
        for qi in range(qtiles):
            dst, r0, ot = out_aps[qi]
            outs.append(p.dma("sp", dst[r0:r0 + 128, :], ot[:], reads=[ot]))

    if _DBG.get('p1only'):
        p.emit(final_waits=outs)
        return nc
    attn_block(2, [[(0, None), (1, None)]] * 2, [(o_ctx, 0, otile[0]), (o_ctx, 128, otile[1])], True)
    if dense:
        allk = [(t, None) for t in range(NT)]
        for qb in range(4):
            attn_block(4, [allk] * 4, [(o_lat, (qb * 4 + qi) * 128, otile[qi]) for qi in range(4)], False)
    else:
        for qt in range(16):
            kl = [(0, None), (1, None), (2 + qt, mprev), (3 + qt, None), (4 + qt, mnext)]
            attn_block(1, [kl], [(o_lat, qt * 128, otile[qt % 4])], False)
    p.emit(final_waits=outs)
    return nc


def _rope_tables():
    rows = 128
    row = np.repeat(np.arange(rows, dtype=np.float32), 64)
    col = np.tile(np.arange(64, dtype=np.float32), rows)
    inv = (np.float32(10000.0) ** (-np.arange(0, 32, 2, dtype=np.float32) / np.float32(32))).astype(np.float32)
    ar = row[:, None] * inv
    ac = col[:, None] * inv
    ang = np.concatenate([ar, ar, ac, ac], -1)
    cos = np.cos(ang).astype(np.float32)
    sin = np.sin(ang).astype(np.float32)
    sgn = np.tile(np.concatenate([-np.ones(16, np.float32), np.ones(16, np.float32)]), 2)
    return cos, sin * sgn


def _perm_qkv_cols(w, nkv):
    qcols = []
    for j in range(8):
        qcols += list(range(j * 64, j * 64 + 64)) + list(range((j + 8) * 64, (j + 8) * 64 + 64))
    kcols = []
    for i in range(nkv // 2):
        for g in (i, i + nkv // 2):
            kcols += list(range(1024 + g * 64, 1024 + g * 64 + 64))
    vcols = list(range(1024 + nkv * 64, 1024 + 2 * nkv * 64))
    return np.ascontiguousarray(w[:, qcols + kcols + vcols])


def attn_inputs(dense, x, ctx, c, c_ctx, mod_w, mod_b, n1g, wqkv, qg, kg, sink):
    nkv = 4 if dense else 2
    cos, sin = _rope_tables()
    wp = _perm_qkv_cols(wqkv, nkv)
    maps = []
    for ci in range(8):
        b, qi = ci // 4, ci % 4
        r0 = qi * 2048
        cnd = np.ascontiguousarray(np.stack([c[b], c_ctx], -1).reshape(8, 128, 2).transpose(1, 0, 2))
        if dense:
            xkv = x[b]
            ckv, skv = cos, sin
            kvalid = np.ones((128, 66), np.float32)
        else:
            xkv = np.zeros((18 * 128, 1024), np.float32)
            ckv = np.zeros((18 * 128, 64), np.float32)
            skv = np.zeros((18 * 128, 64), np.float32)
            lo, hi = max(0, r0 - 128), min(8192, r0 + 2048 + 128)
            off = lo - (r0 - 128)
            xkv[off:off + hi - lo] = x[b, lo:hi]
            ckv[off:off + hi - lo] = cos[lo:hi]
            skv[off:off + hi - lo] = sin[lo:hi]
            kvalid = np.ones((128, 20), np.float32)
            if r0 - 128 < 0:
                kvalid[:, 2] = 0
            if r0 + 2048 + 128 > 8192:
                kvalid[:, 19] = 0
        maps.append(dict(
            xkv=np.ascontiguousarray(xkv), xq=np.ascontiguousarray(x[b, r0:r0 + 2048]), xc=np.ascontiguousarray(ctx[b]),
            cnd=cnd, modw=np.ascontiguousarray(mod_w[:, 0:2048]), modb=np.ascontiguousarray(mod_b[None, 0:2048]),
            n1g=np.ascontiguousarray(n1g[None, :]), wqkv=wp, qg=np.ascontiguousarray(qg[None, :]),
            kg=np.ascontiguousarray(kg[None, :]), sink=np.ascontiguousarray(sink[None, :]),
            cos_kv=np.ascontiguousarray(ckv), sin_kv=np.ascontiguousarray(skv),
            cos_q=np.ascontiguousarray(cos[r0:r0 + 2048]), sin_q=np.ascontiguousarray(sin[r0:r0 + 2048]),
            kvalid=kvalid))
    return maps


NTOK = 2304
NTILE = 18


def build_post1(ntile=NTILE, upto=99):
    nc = bass.Bass("TRN2", target_bir_lowering=False)

    def din(name, shape, dt=F32):
        return nc.dram_tensor(name, list(shape), dt, kind="ExternalInput").ap()

    def dout(name, shape, dt=F32):
        return nc.dram_tensor(name, list(shape), dt, kind="ExternalOutput").ap()
    xr = din("xr", [NTOK, 1024])
    o = din("o", [NTOK, 1024])
    cnd = din("cnd", [128, 8, 2])
    modw = din("modw", [1024, 3072])
    modb = din("modb", [1, 3072])
    n2g = din("n2g", [1, 1024])
    wo = din("wo", [1024, 1024])
    wr = din("wr", [1024, 36])
    br = din("br", [1, 36])
    x_mid = dout("x_mid", [NTOK, 1024])
    h2T = dout("h2T", [128, 8, NTOK], BF16)
    gates = dout("gates", [NTOK, 32])

    p = Prog(nc)
    idt = mk_ident(p)
    ident, identf = idt[BF16], idt[F32]
    sels = mk_sel2(p)
    pT = p.pbuf([128, 1024], BF16, "pT")
    pq = p.pbuf([128, 1024], F32, "pq")
    pkv = p.pbuf([128, 512], F32, "pkv")

    stage = p.buf([128, 4096], F32, "stage")
    g2 = p.buf([2, 1024], F32, "g2")
    p.dma("sp", g2[:], n2g[0:1, :].to_broadcast([2, 1024]), writes=[g2])
    Gvc = p.buf([2, 512], F32, "Gvc")
    G1b = [p.buf([128, 1024], F32, f"G1b{r}") for r in range(2)]
    S2b = [p.buf([128, 1024], F32, f"S2b{r}") for r in range(2)]
    G2b = [p.buf([128, 1024], F32, f"G2b{r}") for r in range(2)]

    def mod_consumer(c, Mc):
        if c < 2:
            bcast_chunk(p, sels, Mc, G1b[0], G1b[1], c * 512, pq)
        elif c < 4:
            bcast_chunk(p, sels, Mc, S2b[0], S2b[1], (c - 2) * 512, pq)
        else:
            cc = c - 4
            p.op("dve", "scalar_tensor_tensor", out=Gvc[:], in0=Mc[:], scalar=1.0, in1=g2[:, cc * 512:(cc + 1) * 512],
                 op0=ALU.add, op1=ALU.mult, reads=[Mc, g2], writes=[Gvc])
            bcast_chunk(p, sels, Gvc, G2b[0], G2b[1], cc * 512, pq)
    modulation(p, cnd, modw, modb, 3072, pkv, stage, mod_consumer)

    eps = p.buf([128, 1], F32, "eps")
    p.op("pool", "memset", eps[:], 1e-6, writes=[eps])
    wob = p.buf([128, 8, 1024], BF16, "wob")
    for k in range(8):
        p.dma("sp", stage[:, 0:1024], wo[k * 128:(k + 1) * 128, :], writes=[stage])
        p.op("pool", "tensor_copy", out=wob[:, k, :], in_=stage[:, 0:1024], reads=[stage], writes=[wob])
    wrt = p.buf([128, 8, 36], F32, "wrt")
    p.dma("sp", wrt[:], wr[:, :].rearrange("(k q) n -> q k n", q=128), writes=[wrt])
    brb = p.buf([128, 36], F32, "brb")
    p.dma("sp", brb[:], br[0:1, :].to_broadcast([128, 36]), writes=[brb])

    xt = [p.buf([128, 1024], F32, f"xt{i}") for i in range(2)]
    ot = [p.buf([128, 1024], F32, f"ot{i}") for i in range(2)]
    ob = p.buf([128, 1024], BF16, "ob")
    oT = p.buf([128, 1024], BF16, "oT")
    tmp = p.buf([128, 1024], F32, "tmp")
    xm = [p.buf([128, 1024], F32, f"xm{i}") for i in range(2)]
    junk = p.buf([128, 1024], F32, "junk")
    ss = p.buf([128, 1], F32, "ss")
    h2 = p.buf([128, 1024], F32, "h2")
    hhi = p.buf([128, 1024], BF16, "hhi")
    hlo = p.buf([128, 1024], BF16, "hlo")
    loT = p.buf([128, 1024], BF16, "loT")
    wr_hi = p.buf([128, 8, 36], BF16, "wr_hi")
    wr_lo = p.buf([128, 8, 36], BF16, "wr_lo")
    p.op("pool", "tensor_copy", out=wr_hi[:], in_=wrt[:], reads=[wrt], writes=[wr_hi])
    p.op("dve", "tensor_tensor", out=wr_lo[:], in0=wrt[:], in1=wr_hi[:], op=ALU.subtract, reads=[wrt, wr_hi], writes=[wr_lo])
    h2Tb = [p.buf([128, 1024], BF16, f"h2Tb{i}") for i in range(2)]
    lg = p.buf([128, 36], F32, "lg")
    sm = {n: p.buf([128, w], F32, n) for n, w in (("gmax", 1), ("ngmax", 1), ("goh", 4), ("gexp", 4), ("gsum", 1), ("gw", 1),
                                                   ("tmp48", 32), ("esel", 8), ("m1", 1), ("oh1", 8), ("e2", 8), ("m2", 1),
                                                   ("oh2", 8), ("d", 1), ("ed", 1), ("den", 1), ("w1", 1), ("w2", 1),
                                                   ("ga", 8), ("gsel", 8))}
    gt = [p.buf([128, 32], F32, f"gt{i}") for i in range(2)]
    outs = []

    def S(n):
        return sm[n]

    for t in range(ntile):
        r = 1 if t < 2 else 0
        i = t % 2
        rows = slice(t * 128, (t + 1) * 128)
        p.dma("sp", ot[i][:], o[rows, :], writes=[ot[i]])
        p.dma("sp", xt[i][:], xr[rows, :], writes=[xt[i]])
        p.op("pool", "tensor_copy", out=ob[:], in_=ot[i][:], reads=[ot[i]], writes=[ob])
        for k in range(8):
            p.op("pe", "transpose", pT[:, k * 128:(k + 1) * 128], ob[:, k * 128:(k + 1) * 128], ident[:],
                 reads=[ob, ident], writes=[pT])
        p.op("act", "copy", out=oT[:], in_=pT[:], reads=[pT], writes=[oT])
        for c in range(2):
            for k in range(8):
                p.op("pe", "matmul", pq[:, c * 512:(c + 1) * 512], lhsT=oT[:, k * 128:(k + 1) * 128],
                     rhs=wob[:, k, c * 512:(c + 1) * 512], start=(k == 0), stop=(k == 7), reads=[oT, wob], writes=[pq])
        p.op("dve", "tensor_tensor", out=tmp[:], in0=pq[:, :], in1=G1b[r][:], op=ALU.mult, reads=[pq, G1b[r]], writes=[tmp])
        p.op("pool", "tensor_tensor", out=xm[i][:], in0=tmp[:], in1=xt[i][:], op=ALU.add, reads=[tmp, xt[i]], writes=[xm[i]])
        outs.append(p.dma("sp", x_mid[rows, :], xm[i][:], reads=[xm[i]]))
        if upto < 1:
            continue
        p.op("act", "activation", out=junk[:], in_=xm[i][:], func=AF.Square, accum_out=ss[:], reads=[xm[i]], writes=[junk, ss])
        p.op("act", "activation", out=ss[:], in_=ss[:], func=AF.Sqrt, bias=eps[:, 0:1], scale=1.0 / 1024,
             reads=[ss, eps], writes=[ss])
        p.op("dve", "reciprocal", out=ss[:], in_=ss[:], reads=[ss], writes=[ss])
        p.op("dve", "scalar_tensor_tensor", out=tmp[:], in0=xm[i][:], scalar=ss[:, 0:1], in1=G2b[r][:], op0=ALU.mult, op1=ALU.mult,
             reads=[xm[i], ss, G2b[r]], writes=[tmp])
        p.op("pool", "tensor_tensor", out=h2[:], in0=tmp[:], in1=S2b[r][:], op=ALU.add, reads=[tmp, S2b[r]], writes=[h2])
        if upto < 2:
            continue
        p.op("pool", "tensor_copy", out=hhi[:], in_=h2[:], reads=[h2], writes=[hhi])
        p.op("dve", "tensor_tensor", out=hlo[:], in0=h2[:], in1=hhi[:], op=ALU.subtract, reads=[h2, hhi], writes=[hlo])
        for k in range(8):
            p.op("pe", "transpose", pT[:, k * 128:(k + 1) * 128], hhi[:, k * 128:(k + 1) * 128], ident[:],
                 reads=[hhi, ident], writes=[pT])
        p.op("act", "copy", out=h2Tb[i][:], in_=pT[:], reads=[pT], writes=[h2Tb[i]])
        for k in range(8):
            p.op("pe", "transpose", pT[:, k * 128:(k + 1) * 128], hlo[:, k * 128:(k + 1) * 128], ident[:],
                 reads=[hlo, ident], writes=[pT])
        p.op("act", "copy", out=loT[:], in_=pT[:], reads=[pT], writes=[loT])
        if upto >= 2.5:
            outs.append(p.dma("sp", h2T[:, :, t * 128:(t + 1) * 128], h2Tb[i][:, :].rearrange("p (k n) -> p k n", n=128),
                              reads=[h2Tb[i]]))
        if upto < 3:
            continue
        n_mm = 0
        for (lt, wt) in ((h2Tb[i], wr_hi), (h2Tb[i], wr_lo), (loT, wr_hi)):
            for k in range(8):
                p.op("pe", "matmul", pkv[:, 0:36], lhsT=lt[:, k * 128:(k + 1) * 128], rhs=wt[:, k, :],
                     start=(n_mm == 0), stop=(n_mm == 23), reads=[lt, wt], writes=[pkv])
                n_mm += 1
        p.op("dve", "tensor_tensor", out=lg[:], in0=pkv[:, 0:36], in1=brb[:], op=ALU.add, reads=[pkv, brb], writes=[lg])
        if upto < 4:
            continue
        gl = lg[:, 0:4]
        el = lg[:, 4:36].rearrange("p (g e) -> p g e", e=8)
        p.op("dve", "tensor_reduce", out=S("gmax")[:], in_=gl, axis=AX.X, op=ALU.max, reads=[lg], writes=[S("gmax")])
        p.op("dve", "tensor_scalar", out=S("goh")[:], in0=gl, scalar1=S("gmax")[:, 0:1], scalar2=None, op0=ALU.is_equal,
             reads=[lg, S("gmax")], writes=[S("goh")])
        p.op("dve", "tensor_scalar", out=S("ngmax")[:], in0=S("gmax")[:], scalar1=-1.0, scalar2=None, op0=ALU.mult,
             reads=[S("gmax")], writes=[S("ngmax")])
        p.op("act", "activation", out=S("gexp")[:], in_=gl, func=AF.Exp, bias=S("ngmax")[:, 0:1], scale=1.0,
             accum_out=S("gsum")[:], reads=[lg, S("ngmax")], writes=[S("gexp"), S("gsum")])
        p.op("dve", "reciprocal", out=S("gw")[:], in_=S("gsum")[:], reads=[S("gsum")], writes=[S("gw")])
        p.op("dve", "tensor_tensor", out=S("tmp48")[:, :].rearrange("p (g e) -> p g e", e=8), in0=el,
             in1=S("goh")[:, :].unsqueeze(2).to_broadcast([128, 4, 8]), op=ALU.mult, reads=[lg, S("goh")], writes=[S("tmp48")])
        p.op("dve", "tensor_reduce", out=S("esel")[:], in_=S("tmp48")[:, :].rearrange("p (g e) -> p e g", e=8), axis=AX.X,
             op=ALU.add, reads=[S("tmp48")], writes=[S("esel")])
        p.op("dve", "tensor_reduce", out=S("m1")[:], in_=S("esel")[:], axis=AX.X, op=ALU.max, reads=[S("esel")], writes=[S("m1")])
        p.op("dve", "tensor_scalar", out=S("oh1")[:], in0=S("esel")[:], scalar1=S("m1")[:, 0:1], scalar2=None, op0=ALU.is_equal,
             reads=[S("esel"), S("m1")], writes=[S("oh1")])
        p.op("dve", "scalar_tensor_tensor", out=S("e2")[:], in0=S("oh1")[:], scalar=-1e30, in1=S("esel")[:], op0=ALU.mult,
             op1=ALU.add, reads=[S("oh1"), S("esel")], writes=[S("e2")])
        p.op("dve", "tensor_reduce", out=S("m2")[:], in_=S("e2")[:], axis=AX.X, op=ALU.max, reads=[S("e2")], writes=[S("m2")])
        p.op("dve", "tensor_scalar", out=S("oh2")[:], in0=S("e2")[:], scalar1=S("m2")[:, 0:1], scalar2=None, op0=ALU.is_equal,
             reads=[S("e2"), S("m2")], writes=[S("oh2")])
        p.op("dve", "tensor_tensor", out=S("d")[:], in0=S("m2")[:], in1=S("m1")[:], op=ALU.subtract,
             reads=[S("m2"), S("m1")], writes=[S("d")])
        p.op("act", "activation", out=S("ed")[:], in_=S("d")[:], func=AF.Exp, reads=[S("d")], writes=[S("ed")])
        p.op("dve", "tensor_scalar", out=S("den")[:], in0=S("ed")[:], scalar1=1.0, scalar2=None, op0=ALU.add,
             reads=[S("ed")], writes=[S("den")])
        p.op("dve", "reciprocal", out=S("w1")[:], in_=S("den")[:], reads=[S("den")], writes=[S("w1")])
        p.op("dve", "tensor_tensor", out=S("w1")[:], in0=S("w1")[:], in1=S("gw")[:], op=ALU.mult,
             reads=[S("w1"), S("gw")], writes=[S("w1")])
        p.op("dve", "tensor_tensor", out=S("w2")[:], in0=S("w1")[:], in1=S("ed")[:], op=ALU.mult,
             reads=[S("w1"), S("ed")], writes=[S("w2")])
        p.op("dve", "tensor_scalar", out=S("ga")[:], in0=S("oh1")[:], scalar1=S("w1")[:, 0:1], scalar2=None, op0=ALU.mult,
             reads=[S("oh1"), S("w1")], writes=[S("ga")])
        p.op("dve", "scalar_tensor_tensor", out=S("gsel")[:], in0=S("oh2")[:], scalar=S("w2")[:, 0:1], in1=S("ga")[:],
             op0=ALU.mult, op1=ALU.add, reads=[S("oh2"), S("w2"), S("ga")], writes=[S("gsel")])
        p.op("dve", "tensor_tensor", out=gt[i][:, :].rearrange("p (g e) -> p g e", e=8),
             in0=S("goh")[:, :].unsqueeze(2).to_broadcast([128, 4, 8]),
             in1=S("gsel")[:, :].unsqueeze(1).to_broadcast([128, 4, 8]), op=ALU.mult,
             reads=[S("goh"), S("gsel")], writes=[gt[i]])
        outs.append(p.dma("sp", gates[rows, :], gt[i][:], reads=[gt[i]]))
    p.emit(final_waits=outs)
    return nc


def build_post2():
    nc = bass.Bass("TRN2", target_bir_lowering=False)

    def din(name, shape, dt=F32):
        return nc.dram_tensor(name, list(shape), dt, kind="ExternalInput").ap()

    def dout(name, shape, dt=F32):
        return nc.dram_tensor(name, list(shape), dt, kind="ExternalOutput").ap()
    x_mid = din("x_mid", [NTOK, 1024])
    h2T = din("h2T", [128, 8, NTOK], BF16)
    gates = din("gates", [NTOK, 32])
    cnd = din("cnd", [128, 8, 2])
    modw = din("modw", [1024, 1024])
    modb = din("modb", [1, 1024])
    fg = din("fg", [1, 1024])
    w1 = din("w1", [32, 1024, 768])
    w3 = din("w3", [32, 1024, 768])
    w2 = din("w2", [32, 768, 1024])
    x_out = dout("x_out", [NTOK, 1024])
    y_fin = dout("y_fin", [NTOK, 1024])

    p = Prog(nc)
    sels = mk_sel2(p)
    ph1 = [p.pbuf([128, 512], F32, f"ph1{i}") for i in range(2)]
    ph3 = [p.pbuf([128, 512], F32, f"ph3{i}") for i in range(2)]
    py = [p.pbuf([128, 512], F32, f"py{i}") for i in range(2)]
    pm = p.pbuf([128, 512], F32, "pm")
    pm2 = p.pbuf([128, 512], F32, "pm2")

    stg = [p.buf([128, 2048], F32, f"stg{i}") for i in range(2)]
    GTb = [p.buf([128, 1024], F32, f"GTb{r}") for r in range(2)]

    def mod_consumer(c, Mc):
        bcast_chunk(p, sels, Mc, GTb[0], GTb[1], c * 512, pm2)
    modulation(p, cnd, modw, modb, 1024, pm, stg, mod_consumer)
    fgb = p.buf([128, 1024], F32, "fgb")
    p.dma("sp", fgb[:], fg[0:1, :].to_broadcast([128, 1024]), writes=[fgb])
    eps = p.buf([128, 1], F32, "eps")
    p.op("pool", "memset", eps[:], 1e-6, writes=[eps])

    hT = p.buf([128, 8, NTOK], BF16, "hT")
    p.dma("sp", hT[:], h2T[:, :, :], writes=[hT])
    gts = p.buf([128, NTILE, 32], F32, "gts")
    p.dma("sp", gts[:], gates[:, :].rearrange("(t q) e -> q t e", q=128), writes=[gts])
    HT = NTILE // 2
    accs = [p.buf([128, 1024], F32, f"acc{t}") for t in range(HT)]
    w1b = [p.buf([128, 8, 768], BF16, f"w1b{i}") for i in range(2)]
    w3b = [p.buf([128, 8, 768], BF16, f"w3b{i}") for i in range(2)]
    w2b = [p.buf([128, 6, 1024], BF16, f"w2b{i}") for i in range(2)]
    hid = [p.buf([128, 6, 384], BF16, f"hid{i}") for i in range(2)]
    s1 = [p.buf([128, 384], F32, f"s1{i}") for i in range(2)]
    xo = [p.buf([128, 1024], F32, f"xo{i}") for i in range(2)]
    yo = [p.buf([128, 1024], F32, f"yo{i}") for i in range(2)]
    ss = p.buf([128, 1], F32, "ss")
    outs = []
    cnts = {"stg": 0, "h": 0, "y": 0, "g": 0}

    def load_w(e, par):
        for kk in range(4):
            for (src, dst) in ((w1, w1b[par]), (w3, w3b[par])):
                s = stg[cnts["stg"] % 2]
                cnts["stg"] += 1
                p.dma("sp", s[:, 0:1536].rearrange("p (k n) -> p k n", n=768),
                      src[e, kk * 256:(kk + 1) * 256, :].rearrange("(k q) n -> q k n", q=128), writes=[s])
                p.op("pool", "tensor_copy", out=dst[:, 2 * kk:2 * kk + 2, :], in_=s[:, 0:1536].rearrange("p (k n) -> p k n", n=768),
                     reads=[s], writes=[dst])
        for kk in range(3):
            s = stg[cnts["stg"] % 2]
            cnts["stg"] += 1
            p.dma("sp", s[:, :].rearrange("p (k n) -> p k n", n=1024),
                  w2[e, kk * 256:(kk + 1) * 256, :].rearrange("(k q) n -> q k n", q=128), writes=[s])
            p.op("pool", "tensor_copy", out=w2b[par][:, 2 * kk:2 * kk + 2, :], in_=s[:, :].rearrange("p (k n) -> p k n", n=1024),
                 reads=[s], writes=[w2b[par]])

    pend_y = [None]

    def emit_y(e, par, gi, hd, t0):
        for tt in range(3):
            tl = gi * 3 + tt
            tg = t0 + tl
            for c in range(2):
                j = cnts["y"] % 2
                cnts["y"] += 1
                for f in range(6):
                    p.op("pe", "matmul", py[j][:, 0:512], lhsT=hd[:, f, tt * 128:(tt + 1) * 128],
                         rhs=w2b[par][:, f, c * 512:(c + 1) * 512], start=(f == 0), stop=(f == 5),
                         reads=[hd, w2b[par]], writes=[py[j]])
                if e == 0:
                    p.op("dve", "tensor_scalar", out=accs[tl][:, c * 512:(c + 1) * 512], in0=py[j][:, 0:512],
                         scalar1=gts[:, tg, e:e + 1], scalar2=None, op0=ALU.mult,
                         reads=[py[j], gts], writes=[accs[tl]])
                else:
                    p.op("dve", "scalar_tensor_tensor", out=accs[tl][:, c * 512:(c + 1) * 512], in0=py[j][:, 0:512],
                         scalar=gts[:, tg, e:e + 1], in1=accs[tl][:, c * 512:(c + 1) * 512], op0=ALU.mult, op1=ALU.add,
                         reads=[py[j], gts, accs[tl]], writes=[accs[tl]])

    for half in range(2):
        t0 = half * HT
        for e in range(32):
            par = e % 2
            load_w(e, par)
            for gi in range(HT // 3):
                tok0 = (t0 + gi * 3) * 128
                hd = hid[cnts["g"] % 2]
                cnts["g"] += 1
                for f in range(6):
                    j = cnts["h"] % 2
                    cnts["h"] += 1
                    for k in range(8):
                        p.op("pe", "matmul", ph1[j][:, 0:384], lhsT=w1b[par][:, k, f * 128:(f + 1) * 128],
                             rhs=hT[:, k, tok0:tok0 + 384], start=(k == 0), stop=(k == 7), reads=[w1b[par], hT], writes=[ph1[j]])
                    for k in range(8):
                        p.op("pe", "matmul", ph3[j][:, 0:384], lhsT=w3b[par][:, k, f * 128:(f + 1) * 128],
                             rhs=hT[:, k, tok0:tok0 + 384], start=(k == 0), stop=(k == 7), reads=[w3b[par], hT], writes=[ph3[j]])
                    p.op("act", "activation", out=s1[j][:], in_=ph1[j][:, 0:384], func=AF.Silu, reads=[ph1[j]], writes=[s1[j]])
                    p.op("dve", "tensor_tensor", out=hd[:, f, :], in0=s1[j][:], in1=ph3[j][:, 0:384], op=ALU.mult,
                         reads=[s1[j], ph3[j]], writes=[hd])
                if pend_y[0] is not None:
                    emit_y(*pend_y[0])
                pend_y[0] = (e, par, gi, hd, t0)
        emit_y(*pend_y[0])
        pend_y[0] = None
        for tl in range(HT):
            tg = t0 + tl
            r = 1 if tg < 2 else 0
            i = tl % 2
            rows = slice(tg * 128, (tg + 1) * 128)
            p.dma("sp", xo[i][:], x_mid[rows, :], writes=[xo[i]])
            p.op("pool", "tensor_tensor", out=accs[tl][:], in0=accs[tl][:], in1=GTb[r][:], op=ALU.mult,
                 reads=[accs[tl], GTb[r]], writes=[accs[tl]])
            p.op("pool", "tensor_tensor", out=xo[i][:], in0=xo[i][:], in1=accs[tl][:], op=ALU.add,
                 reads=[xo[i], accs[tl]], writes=[xo[i]])
            outs.append(p.dma("sp", x_out[rows, :], xo[i][:], reads=[xo[i]]))
            p.op("act", "activation", out=yo[i][:], in_=xo[i][:], func=AF.Square, accum_out=ss[:], reads=[xo[i]], writes=[yo[i], ss])
            p.op("act", "activation", out=ss[:], in_=ss[:], func=AF.Sqrt, bias=eps[:, 0:1], scale=1.0 / 1024,
                 reads=[ss, eps], writes=[ss])
            p.op("dve", "reciprocal", out=ss[:], in_=ss[:], reads=[ss], writes=[ss])
            p.op("dve", "scalar_tensor_tensor", out=yo[i][:], in0=xo[i][:], scalar=ss[:, 0:1], in1=fgb[:], op0=ALU.mult, op1=ALU.mult,
                 reads=[xo[i], ss, fgb], writes=[yo[i]])
            outs.append(p.dma("sp", y_fin[rows, :], yo[i][:], reads=[yo[i]]))
    p.emit(final_waits=outs)
    return nc


def _cnd(c_b, c_ctx):
    return np.ascontiguousarray(np.stack([c_b, c_ctx], -1).reshape(8, 128, 2).transpose(1, 0, 2))


def _rows(x_lat, x_ctx, ci):
    b, qi = ci // 4, ci % 4
    return np.ascontiguousarray(np.concatenate([x_ctx[b], x_lat[b, qi * 2048:(qi + 1) * 2048]], 0))


def post1_inputs(x_lat, x_ctx, o_lat, o_ctx, c, c_ctx, mod_w, mod_b, n2g, wo, wg, bg, we, be):
    wr = np.ascontiguousarray(np.concatenate([wg, we], 1))
    br = np.ascontiguousarray(np.concatenate([bg, be])[None, :])
    maps = []
    for ci in range(8):
        b = ci // 4
        maps.append(dict(xr=_rows(x_lat, x_ctx, ci), o=_rows(o_lat, o_ctx, ci), cnd=_cnd(c[b], c_ctx),
                         modw=np.ascontiguousarray(mod_w[:, 2048:5120]), modb=np.ascontiguousarray(mod_b[None, 2048:5120]),
                         n2g=np.ascontiguousarray(n2g[None, :]), wo=np.ascontiguousarray(wo), wr=wr, br=br))
    return maps


def post2_inputs(res1, c, c_ctx, mod_w, mod_b, fg, w1, w3, w2):
    maps = []
    for ci in range(8):
        b = ci // 4
        r = res1[ci]
        maps.append(dict(x_mid=r["x_mid"], h2T=r["h2T"], gates=r["gates"], cnd=_cnd(c[b], c_ctx),
                         modw=np.ascontiguousarray(mod_w[:, 5120:6144]), modb=np.ascontiguousarray(mod_b[None, 5120:6144]),
                         fg=np.ascontiguousarray(fg[None, :]), w1=w1, w3=w3, w2=w2))
    return maps


def _unrows(res, key):
    lat = np.zeros((2, 8192, 1024), np.float32)
    ctx = np.zeros((2, 256, 1024), np.float32)
    for ci in range(8):
        b, qi = ci // 4, ci % 4
        a = res[ci][key]
        lat[b, qi * 2048:(qi + 1) * 2048] = a[256:]
        if qi == 0:
            ctx[b] = a[:256]
    return lat, ctx


RT_TOK = 8448
RT_TILES = 66
NDEC = -0.6065306597126334


def build_rwkv(ntiles=RT_TILES, phase_c=True):
    nc = bass.Bass("TRN2", target_bir_lowering=False)

    def din(name, shape, dt=F32):
        return nc.dram_tensor(name, list(shape), dt, kind="ExternalInput").ap()

    def dscr(name, shape, dt=F32, kind="Internal"):
        return nc.dram_tensor(name, list(shape), dt, kind=kind).ap()
    xa = din("xa", [RT_TOK, 1024])
    xp = din("xp", [RT_TOK, 1024])
    xn = din("xn", [RT_TOK, 1024])
    mpn = din("mpn", [RT_TOK, 2])
    cnd = din("cnd", [128, 8, 2])
    modw = din("modw", [1024, 2048])
    modb = din("modb", [1, 2048])
    n1g = din("n1g", [1, 1024])
    mu = din("mu", [6, 1024])
    w_r = din("w_r", [1024, 256])
    w_k = din("w_k", [1024, 256])
    w_v = din("w_v", [1024, 256])
    g1 = din("g1", [1024, 128])
    g2 = din("g2", [128, 256])
    w1 = din("w1", [1024, 128])
    w2 = din("w2", [64, 512])
    a1 = din("a1", [1024, 128])
    a2 = din("a2", [64, 512])
    vecs = din("vecs", [9, 256])
    o_out = nc.dram_tensor("o_out", [RT_TOK, 256], F32, kind="ExternalOutput").ap()
    FK = "ExternalOutput" if not phase_c else "Internal"
    f_r = dscr("f_r", [RT_TOK, 256], kind=FK)
    f_v = dscr("f_v", [RT_TOK, 256], kind=FK)
    f_kk = dscr("f_kk", [RT_TOK, 256], kind=FK)
    f_g = dscr("f_g", [RT_TOK, 256], kind=FK)
    f_bon = dscr("f_bon", [RT_TOK, 256], kind=FK)
    f_kd = [dscr(f"f_kd{d}", [RT_TOK, 256], kind=FK) for d in range(2)]
    f_b = [dscr(f"f_b{d}", [RT_TOK, 256], kind=FK) for d in range(2)]
    f_lw = [dscr(f"f_lw{d}", [RT_TOK, 256], kind=FK) for d in range(2)]
    f_y = dscr("f_y", [RT_TOK, 256])

    p = Prog(nc)
    idt = mk_ident(p)
    ident, identf = idt[BF16], idt[F32]
    sels = mk_sel2(p)
    pT = p.pbuf([128, 1024], BF16, "pT")
    Q = [p.pbuf([128, 512], F32, f"q{i}") for i in range(7)]

    stage = p.buf([128, 4096], F32, "stage")
    g2n = p.buf([2, 1024], F32, "g2n")
    p.dma("sp", g2n[:], n1g[0:1, :].to_broadcast([2, 1024]), writes=[g2n])
    Gvc = p.buf([2, 512], F32, "Gvc")
    Gb = [p.buf([128, 1024], F32, f"Gb{r}") for r in range(2)]
    Sb = [p.buf([128, 1024], F32, f"Sb{r}") for r in range(2)]

    def mod_consumer(c, Mc):
        if c < 2:
            bcast_chunk(p, sels, Mc, Sb[0], Sb[1], c * 512, Q[1])
        else:
            cc = c - 2
            p.op("dve", "scalar_tensor_tensor", out=Gvc[:], in0=Mc[:], scalar=1.0, in1=g2n[:, cc * 512:(cc + 1) * 512],
                 op0=ALU.add, op1=ALU.mult, reads=[Mc, g2n], writes=[Gvc])
            bcast_chunk(p, sels, Gvc, Gb[0], Gb[1], cc * 512, Q[1])
    modulation(p, cnd, modw, modb, 2048, Q[0], stage, mod_consumer)

    eps = p.buf([128, 1], F32, "eps")
    p.op("pool", "memset", eps[:], 1e-6, writes=[eps])
    eps_ln = p.buf([128, 1], F32, "eps_ln")
    p.op("pool", "memset", eps_ln[:], 64e-5, writes=[eps_ln])
    MUb = [p.buf([128, 1024], F32, f"MUb{i}") for i in range(6)]
    for i in range(6):
        p.dma("sp", MUb[i][:], mu[i:i + 1, :].to_broadcast([128, 1024]), writes=[MUb[i]])
    VC = [p.buf([128, 256], F32, f"vc{i}") for i in range(9)]
    for i in range(9):
        p.dma("sp", VC[i][:], vecs[i:i + 1, :].to_broadcast([128, 256]), writes=[VC[i]])
    w0b, a0b, kkb, kab, rkb, lnwb, lnbb = VC[0:2], VC[2:4], VC[4], VC[5], VC[6], VC[7], VC[8]

    def load_bf(src_ap, shape, name, view):
        dst = p.buf(shape, BF16, name)
        n = shape[-1]
        p.dma("sp", stage[:, 0:8 * n].rearrange("p (k n) -> p k n", n=n), src_ap.rearrange("(k q) n -> q k n", q=128), writes=[stage])
        p.op("pool", "tensor_copy", out=dst[:], in_=stage[:, 0:8 * n].rearrange("p (k n) -> p k n", n=n), reads=[stage], writes=[dst])
        return dst
    wrb = load_bf(w_r[:, :], [128, 8, 256], "wrb", None)
    wkb = load_bf(w_k[:, :], [128, 8, 256], "wkb", None)
    wvb = load_bf(w_v[:, :], [128, 8, 256], "wvb", None)
    g1b = load_bf(g1[:, :], [128, 8, 128], "g1b", None)
    w1b = load_bf(w1[:, :], [128, 8, 128], "w1b", None)
    a1b = load_bf(a1[:, :], [128, 8, 128], "a1b", None)

    def load_small_bf(src_ap, shape, name):
        dst = p.buf(shape, BF16, name)
        p.dma("sp", stage[0:shape[0], 0:shape[1]], src_ap, writes=[stage])
        p.op("pool", "tensor_copy", out=dst[:], in_=stage[0:shape[0], 0:shape[1]], reads=[stage], writes=[dst])
        return dst
    g2b = load_small_bf(g2[:, :], [128, 256], "g2b")
    w2b = load_small_bf(w2[:, :], [64, 512], "w2b")
    a2b = load_small_bf(a2[:, :], [64, 512], "a2b")

    xt3 = [p.buf([128, 1024], F32, f"x3_{i}") for i in range(3)]
    h3 = [p.buf([128, 1024], F32, f"h3_{i}") for i in range(3)]
    msk = p.buf([128, 2], F32, "msk")
    junk = p.buf([128, 1024], F32, "junk")
    ss3 = [p.buf([128, 1], F32, f"ss3_{i}") for i in range(3)]
    xx = p.buf([128, 1024], F32, "xx")
    tmpx = [p.buf([128, 1024], F32, f"tmpx{i}") for i in range(2)]
    xib = [p.buf([128, 1024], BF16, f"xib{i}") for i in range(2)]
    xT = [p.buf([128, 1024], BF16, f"xT{i}") for i in range(6)]
    sgT = p.buf([128, 128], BF16, "sgT")
    lorT = [p.buf([64, 128], BF16, f"lorT{i}") for i in range(4)]
    names = ["r32", "k32", "v32", "g32", "kkr", "kk32", "bon", "t0", "t1", "ks"] + \
            [f"{n}{d}" for n in ("wl", "a32", "lw", "kd", "bb") for d in range(2)]
    T_ = {n: p.buf([128, 256], F32, n) for n in names}
    sm4 = [p.buf([128, 4], F32, f"sm4_{i}") for i in range(3)]
    ew_i = [0]

    def ew():
        ew_i[0] += 1
        return "dve" if ew_i[0] % 2 else "pool"

    def v4(t):
        return t[:, :].rearrange("p (h d) -> p h d", d=64)

    def b4(s):
        return s[:, 0:4].unsqueeze(2).to_broadcast([128, 4, 64])

    feat_out = []
    for t in range(ntiles):
        rr = 1 if t < 2 else 0
        rows = slice(t * 128, (t + 1) * 128)
        for i, src in enumerate((xa, xp, xn)):
            p.dma("sp", xt3[i][:], src[rows, :], writes=[xt3[i]])
        p.dma("sp", msk[:], mpn[rows, :], writes=[msk])
        for i in range(3):
            p.op("act", "activation", out=junk[:], in_=xt3[i][:], func=AF.Square, accum_out=ss3[i][:], reads=[xt3[i]], writes=[junk, ss3[i]])
            p.op("act", "activation", out=ss3[i][:], in_=ss3[i][:], func=AF.Sqrt, bias=eps[:, 0:1], scale=1.0 / 1024,
                 reads=[ss3[i], eps], writes=[ss3[i]])
            p.op("dve", "reciprocal", out=ss3[i][:], in_=ss3[i][:], reads=[ss3[i]], writes=[ss3[i]])
            p.op("dve", "scalar_tensor_tensor", out=h3[i][:], in0=xt3[i][:], scalar=ss3[i][:, 0:1], in1=Gb[rr][:], op0=ALU.mult,
                 op1=ALU.mult, reads=[xt3[i], ss3[i], Gb[rr]], writes=[h3[i]])
            p.op("pool", "tensor_tensor", out=h3[i][:], in0=h3[i][:], in1=Sb[rr][:], op=ALU.add, reads=[h3[i], Sb[rr]], writes=[h3[i]])
        h = h3[0]
        p.op("dve", "tensor_scalar", out=xx[:], in0=h3[1][:], scalar1=msk[:, 0:1], scalar2=None, op0=ALU.mult,
             reads=[h3[1], msk], writes=[xx])
        p.op("dve", "scalar_tensor_tensor", out=xx[:], in0=h3[2][:], scalar=msk[:, 1:2], in1=xx[:], op0=ALU.mult, op1=ALU.add,
             reads=[h3[2], msk, xx], writes=[xx])
        p.op("dve", "scalar_tensor_tensor", out=xx[:], in0=xx[:], scalar=0.5, in1=h[:], op0=ALU.mult, op1=ALU.subtract,
             reads=[xx, h], writes=[xx])
        for i in range(6):
            tm, xb_ = tmpx[i % 2], xib[i % 2]
            p.op("pool", "tensor_tensor", out=tm[:], in0=xx[:], in1=MUb[i][:], op=ALU.mult, reads=[xx, MUb[i]], writes=[tm])
            p.op("dve", "tensor_tensor", out=xb_[:], in0=tm[:], in1=h[:], op=ALU.add, reads=[tm, h], writes=[xb_])
            for k in range(8):
                p.op("pe", "transpose", pT[:, k * 128:(k + 1) * 128], xb_[:, k * 128:(k + 1) * 128], ident[:],
                     reads=[xb_, ident], writes=[pT])
            p.op("act", "copy", out=xT[i][:], in_=pT[:], reads=[pT], writes=[xT[i]])
        for (qi, c0, src, wt) in ((0, 0, xT[0], wrb), (0, 256, xT[2], wkb), (1, 0, xT[3], wvb)):
            for k in range(8):
                p.op("pe", "matmul", Q[qi][:, c0:c0 + 256], lhsT=src[:, k * 128:(k + 1) * 128], rhs=wt[:, k, :],
                     start=(k == 0 and c0 == 0), stop=(k == 7), reads=[src, wt], writes=[Q[qi]])
        for k in range(8):
            p.op("pe", "matmul", Q[2][:, 0:128], lhsT=g1b[:, k, :], rhs=xT[5][:, k * 128:(k + 1) * 128],
                 start=(k == 0), stop=(k == 7), reads=[g1b, xT[5]], writes=[Q[2]])
        p.op("act", "activation", out=sgT[:], in_=Q[2][:, 0:128], func=AF.Sigmoid, reads=[Q[2]], writes=[sgT])
        p.op("pe", "matmul", Q[1][:, 256:512], lhsT=sgT[:, :], rhs=g2b[:, :], start=False, stop=True,
             reads=[sgT, g2b], writes=[Q[1]])
        for li, (src, wa, wb_, fn) in enumerate(((xT[1], w1b, w2b, AF.Tanh), (xT[4], a1b, a2b, AF.Copy))):
            for d in range(2):
                for k in range(8):
                    p.op("pe", "matmul", Q[3][0:64, 0:128], lhsT=wa[:, k, d * 64:(d + 1) * 64], rhs=src[:, k * 128:(k + 1) * 128],
                         start=(k == 0), stop=(k == 7), reads=[wa, src], writes=[Q[3]])
                lt = lorT[li * 2 + d]
                p.op("act", "activation", out=lt[:], in_=Q[3][0:64, 0:128], func=fn, reads=[Q[3]], writes=[lt])
                p.op("pe", "matmul", Q[4 + li][:, d * 256:(d + 1) * 256], lhsT=lt[:, :], rhs=wb_[:, d * 256:(d + 1) * 256],
                     start=(d == 0), stop=True, reads=[lt, wb_], writes=[Q[4 + li]])
        p.op("act", "copy", out=T_["r32"][:], in_=Q[0][:, 0:256], reads=[Q[0]], writes=[T_["r32"]])
        p.op("act", "copy", out=T_["k32"][:], in_=Q[0][:, 256:512], reads=[Q[0]], writes=[T_["k32"]])
        p.op("act", "copy", out=T_["v32"][:], in_=Q[1][:, 0:256], reads=[Q[1]], writes=[T_["v32"]])
        p.op("act", "copy", out=T_["g32"][:], in_=Q[1][:, 256:512], reads=[Q[1]], writes=[T_["g32"]])
        for d in range(2):
            wl, a32, lw = T_[f"wl{d}"], T_[f"a32{d}"], T_[f"lw{d}"]
            p.op("dve", "tensor_tensor", out=wl[:], in0=Q[4][:, d * 256:(d + 1) * 256], in1=w0b[d][:], op=ALU.add,
                 reads=[Q[4], w0b[d]], writes=[wl])
            p.op("act", "activation", out=wl[:], in_=wl[:], func=AF.Sigmoid, reads=[wl], writes=[wl])
            p.op("pool", "tensor_scalar", out=lw[:], in0=wl[:], scalar1=NDEC, scalar2=None, op0=ALU.mult, reads=[wl], writes=[lw])
            p.op("dve", "tensor_tensor", out=a32[:], in0=Q[5][:, d * 256:(d + 1) * 256], in1=a0b[d][:], op=ALU.add,
                 reads=[Q[5], a0b[d]], writes=[a32])
            p.op("act", "activation", out=a32[:], in_=a32[:], func=AF.Sigmoid, reads=[a32], writes=[a32])
        p.op("pool", "tensor_tensor", out=T_["kkr"][:], in0=T_["k32"][:], in1=kkb[:], op=ALU.mult, reads=[T_["k32"], kkb], writes=[T_["kkr"]])
        p.op("act", "activation", out=T_["t0"][:], in_=T_["kkr"][:], func=AF.Square, reads=[T_["kkr"]], writes=[T_["t0"]])
        p.op("dve", "tensor_reduce", out=sm4[0][:], in_=v4(T_["t0"]), axis=AX.X, op=ALU.add, reads=[T_["t0"]], writes=[sm4[0]])
        p.op("act", "activation", out=sm4[0][:], in_=sm4[0][:], func=AF.Sqrt, reads=[sm4[0]], writes=[sm4[0]])
        p.op("dve", "tensor_scalar", out=sm4[0][:], in0=sm4[0][:], scalar1=1e-12, scalar2=None, op0=ALU.max, reads=[sm4[0]], writes=[sm4[0]])
        p.op("dve", "reciprocal", out=sm4[0][:], in_=sm4[0][:], reads=[sm4[0]], writes=[sm4[0]])
        p.op("dve", "tensor_tensor", out=v4(T_["kk32"]), in0=v4(T_["kkr"]), in1=b4(sm4[0]), op=ALU.mult,
             reads=[T_["kkr"], sm4[0]], writes=[T_["kk32"]])
        for d in range(2):
            a32, kd, bb = T_[f"a32{d}"], T_[f"kd{d}"], T_[f"bb{d}"]
            e1 = ew()
            p.op(e1, "scalar_tensor_tensor", out=T_["t1"][:], in0=a32[:], scalar=-1.0, in1=kab[:], op0=ALU.add, op1=ALU.mult,
                 reads=[a32, kab], writes=[T_["t1"]])
            p.op(e1, "scalar_tensor_tensor", out=kd[:], in0=T_["t1"][:], scalar=1.0, in1=T_["k32"][:], op0=ALU.add, op1=ALU.mult,
                 reads=[T_["t1"], T_["k32"]], writes=[kd])
            p.op(ew(), "tensor_tensor", out=bb[:], in0=T_["kk32"][:], in1=a32[:], op=ALU.mult, reads=[T_["kk32"], a32], writes=[bb])
        p.op("pool", "tensor_tensor", out=T_["ks"][:], in0=T_["kd0"][:], in1=T_["kd1"][:], op=ALU.add,
             reads=[T_["kd0"], T_["kd1"]], writes=[T_["ks"]])
        p.op("pool", "tensor_tensor", out=T_["ks"][:], in0=T_["ks"][:], in1=T_["r32"][:], op=ALU.mult, reads=[T_["ks"], T_["r32"]], writes=[T_["ks"]])
        p.op("pool", "tensor_tensor", out=T_["ks"][:], in0=T_["ks"][:], in1=rkb[:], op=ALU.mult, reads=[T_["ks"], rkb], writes=[T_["ks"]])
        p.op("dve", "tensor_reduce", out=sm4[1][:], in_=v4(T_["ks"]), axis=AX.X, op=ALU.add, reads=[T_["ks"]], writes=[sm4[1]])
        p.op("dve", "tensor_tensor", out=v4(T_["bon"]), in0=v4(T_["v32"]), in1=b4(sm4[1]), op=ALU.mult,
             reads=[T_["v32"], sm4[1]], writes=[T_["bon"]])
        for (dst, srcn) in ((f_r, "r32"), (f_v, "v32"), (f_kk, "kk32"), (f_g, "g32"), (f_bon, "bon"),
                            (f_kd[0], "kd0"), (f_kd[1], "kd1"), (f_b[0], "bb0"), (f_b[1], "bb1"),
                            (f_lw[0], "lw0"), (f_lw[1], "lw1")):
            feat_out.append(p.dma("sp", dst[rows, :], T_[srcn][:], reads=[T_[srcn]]))

    if not phase_c:
        p.emit(final_waits=feat_out)
        return nc
    rwkv_phase_c(p, nc, locals())
    return nc


def rwkv_phase_c(p, nc, L):
    ntiles = L["ntiles"]
    ident, identf, pT, Q = L["ident"], L["identf"], L["pT"], L["Q"]
    f_r, f_v, f_kk, f_g, f_bon, f_kd, f_b, f_lw, f_y, o_out = (L[k] for k in
        ("f_r", "f_v", "f_kk", "f_g", "f_bon", "f_kd", "f_b", "f_lw", "f_y", "o_out"))
    lnwb, lnbb, eps_ln, feat_out = L["lnwb"], L["lnbb"], L["eps_ln"], L["feat_out"]

    fence = Buf(None, "fence")
    for d in feat_out:
        fence.r.append(d)
    fdum = p.buf([1, 8], F32, "fdum")
    fd2 = p.buf([1, 8], F32, "fd2")
    fd3 = p.buf([1, 8], F32, "fd3")
    p.op("pool", "memset", fdum[:], 0.0, writes=[fence, fdum])
    p.op("dve", "tensor_copy", out=fd2[:], in_=fdum[:], reads=[fdum], writes=[fd2])
    p.op("act", "copy", out=fd3[:], in_=fdum[:], reads=[fdum], writes=[fd3])
    arena_src = [b.ap for b in (L["MUb"] + L["xt3"] + L["h3"] + L["tmpx"] + [L["xx"], L["junk"]])]
    ar = {"i": 0, "off": 0}

    def abuf(shape, dt, name):
        nparts = shape[0]
        ncols = 1
        for v_ in shape[1:]:
            ncols *= v_
        nf = ncols if dt == F32 else (ncols + 1) // 2
        if ar["off"] + nf > 1024:
            ar["i"] += 1
            ar["off"] = 0
        t = arena_src[ar["i"]]
        ap = t[0:nparts, ar["off"]:ar["off"] + nf]
        ar["off"] += nf
        if dt != F32:
            ap = ap.bitcast(dt)
        if len(shape) == 3:
            ap = ap.rearrange("p (j n) -> p j n", n=shape[2])
        return Buf(ap, name)

    def tri_mask(name, sg, strict, transpose=False):
        mf = abuf([128, 128], F32, name + "f")
        p.op("pool", "memset", mf[:], 1.0, writes=[mf])
        s_ = -sg if transpose else sg
        p.op("pool", "affine_select", out=mf[:], in_=mf[:], pattern=[[s_, 128]], compare_op=ALU.is_ge, fill=0.0,
             base=(-1 if strict else 0), channel_multiplier=-s_, reads=[mf], writes=[mf])
        p.op("pool", "memset", mf[0:64, 64:128], 0.0, reads=[mf], writes=[mf])
        p.op("pool", "memset", mf[64:128, 0:64], 0.0, reads=[mf], writes=[mf])
        return mf
    masks = []
    for d in range(2):
        sg = 1 if d == 0 else -1
        ms = tri_mask(f"ms{d}", sg, True)
        mi = tri_mask(f"mi{d}", sg, False)
        mst = tri_mask(f"mst{d}", sg, True, transpose=True)
        m2 = abuf([128, 256], F32, f"m2_{d}")
        p.op("dve", "tensor_copy", out=m2[:, 0:128], in_=ms[:], reads=[ms], writes=[m2])
        p.op("dve", "tensor_copy", out=m2[:, 128:256], in_=mi[:], reads=[mi], writes=[m2])
        msb = abuf([128, 128], BF16, f"msb{d}")
        mib = abuf([128, 128], BF16, f"mib{d}")
        p.op("dve", "tensor_copy", out=msb[:], in_=ms[:], reads=[ms], writes=[msb])
        p.op("dve", "tensor_copy", out=mib[:], in_=mi[:], reads=[mi], writes=[mib])
        masks.append(dict(m2=m2, mst=mst, msb=msb, mib=mib))
    cind = abuf([128, 2], BF16, "cind")
    p.op("pool", "memset", cind[:], 0.0, writes=[cind])
    p.op("pool", "memset", cind[0:64, 0:1], 1.0, reads=[cind], writes=[cind])
    p.op("pool", "memset", cind[64:128, 1:2], 1.0, reads=[cind], writes=[cind])

    NH = 4
    ld = {n: [abuf([128, 256], F32, f"ld_{n}{i}") for i in range(2)] for n in ("r", "v", "kk", "kd", "b", "lw")}
    lwh = abuf([128, 256], BF16, "lwh")
    lwl = abuf([128, 256], BF16, "lwl")
    Pm = {n: abuf([128, 256], F32, "P" + n) for n in ("p", "inv", "ex")}
    rcb = abuf([128, 256], BF16, "rcb")
    khb = abuf([128, 256], BF16, "khb")
    nbb = abuf([128, 256], BF16, "nbb")
    vb = abuf([128, 256], BF16, "vb")
    Wall = [[abuf([128, 128], BF16, f"W{h}_{i}") for i in range(2)] for h in range(NH)]
    FT = [abuf([64, 4, 128], BF16, f"FT{h}") for h in range(NH)]
    YN = [abuf([128, 256], BF16, f"YN{h}") for h in range(NH)]
    AG = [abuf([128, 256], BF16, f"AG{h}") for h in range(NH)]
    Xm = [[abuf([128, 128], BF16, f"X{h}_{i}") for i in range(2)] for h in range(NH)]
    Ym = [[abuf([128, 128], BF16, f"Y{h}_{i}") for i in range(2)] for h in range(NH)]
    Y0f = [abuf([128, 64], F32, f"Y0f{h}") for h in range(NH)]
    M2b = [abuf([128, 64], BF16, f"M2b{h}") for h in range(NH)]
    M2T = [abuf([64, 128], BF16, f"M2T{h}") for h in range(NH)]
    M3T = [[abuf([64, 64], BF16, f"M3T{h}_{c}") for c in range(2)] for h in range(NH)]
    Z0P = [[abuf([64, 64], F32, f"Z0P{h}_{c}") for c in range(2)] for h in range(NH)]
    Pc = abuf([64, 8], F32, "Pc")
    H32 = [[abuf([64, 64], F32, f"H32_{d}_{h}") for h in range(NH)] for d in range(2)]
    Hhi = [[abuf([64, 64], BF16, f"Hhi_{d}_{h}") for h in range(NH)] for d in range(2)]
    Hlo = [[abuf([64, 64], BF16, f"Hlo_{d}_{h}") for h in range(NH)] for d in range(2)]
    for d in range(2):
        for h in range(NH):
            p.op("pool", "memset", H32[d][h][:], 0.0, writes=[H32[d][h]])
            p.op("pool", "memset", Hhi[d][h][:], 0.0, writes=[Hhi[d][h]])
            p.op("pool", "memset", Hlo[d][h][:], 0.0, writes=[Hlo[d][h]])
    ytile = [abuf([128, 256], F32, f"ytile{i}") for i in range(2)]
    yf = abuf([128, 256], F32, "yf")
    og = {n: abuf([128, 256], F32, "og_" + n) for n in ("g", "bon", "yc", "sq")}
    st4 = [abuf([128, 4], F32, f"st4_{i}") for i in range(2)]
    outs = []
    ycnt = [0]
    pcum, pA, pB, pS0, pS1, pYc, pHc = Q
    psm = [pS0, pS1]
    smi = [0]

    def nps():
        smi[0] += 1
        return psm[smi[0] % 2]

    def v4(t):
        return t[:, :].rearrange("p (h d) -> p h d", d=64)

    def b4(s):
        return s[:, 0:4].unsqueeze(2).to_broadcast([128, 4, 64])

    tcount = [0]

    def do_tile(d, t):
        mk = masks[d]
        rows = slice(t * 128, (t + 1) * 128)
        i = tcount[0] % 2
        tcount[0] += 1
        srcs = (("r", f_r), ("v", f_v), ("kk", f_kk), ("kd", f_kd[d]), ("b", f_b[d]), ("lw", f_lw[d]))
        for n, src in srcs:
            p.dma("sp", ld[n][i][:], src[rows, :], reads=[fence], writes=[ld[n][i]])
        lw = ld["lw"][i]
        p.op("pool", "tensor_copy", out=lwh[:], in_=lw[:], reads=[lw], writes=[lwh])
        p.op("dve", "tensor_tensor", out=lwl[:], in0=lw[:], in1=lwh[:], op=ALU.subtract, reads=[lw, lwh], writes=[lwl])
        for (c0, mm) in ((0, mk["mib"]), (256, mk["msb"])):
            p.op("pe", "matmul", pcum[:, c0:c0 + 256], lhsT=mm[:, :], rhs=lwh[:, :], start=(c0 == 0), stop=False,
                 reads=[mm, lwh], writes=[pcum])
            p.op("pe", "matmul", pcum[:, c0:c0 + 256], lhsT=mm[:, :], rhs=lwl[:, :], start=False, stop=True,
                 reads=[mm, lwl], writes=[pcum])
        p.op("act", "activation", out=Pm["p"][:], in_=pcum[:, 0:256], func=AF.Exp, reads=[pcum], writes=[Pm["p"]])
        p.op("act", "activation", out=Pm["inv"][:], in_=pcum[:, 0:256], func=AF.Exp, scale=-1.0, reads=[pcum], writes=[Pm["inv"]])
        p.op("act", "activation", out=Pm["ex"][:], in_=pcum[:, 256:512], func=AF.Exp, reads=[pcum], writes=[Pm["ex"]])
        for h in range(NH):
            for j, lt in enumerate((lwh, lwl)):
                p.op("pe", "matmul", pHc[0:64, 64 + 2 * h:64 + 2 * h + 2], lhsT=lt[:, h * 64:(h + 1) * 64], rhs=cind[:, :],
                     start=(h == 0 and j == 0), stop=(j == 1), reads=[lt, cind], writes=[pHc])
        p.op("act", "activation", out=Pc[:], in_=pHc[0:64, 64:72], func=AF.Exp, reads=[pHc], writes=[Pc])
        p.op("dve", "tensor_tensor", out=rcb[:], in0=ld["r"][i][:], in1=Pm["p"][:], op=ALU.mult, reads=[ld["r"][i], Pm["p"]], writes=[rcb])
        p.op("pool", "tensor_tensor", out=khb[:], in0=ld["kd"][i][:], in1=Pm["inv"][:], op=ALU.mult,
             reads=[ld["kd"][i], Pm["inv"]], writes=[khb])
        p.op("dve", "scalar_tensor_tensor", out=nbb[:], in0=ld["b"][i][:], scalar=-1.0, in1=Pm["inv"][:], op0=ALU.mult, op1=ALU.mult,
             reads=[ld["b"][i], Pm["inv"]], writes=[nbb])
        p.op("pool", "tensor_copy", out=vb[:], in_=ld["v"][i][:], reads=[ld["v"][i]], writes=[vb])
        W = [Wall[h][0] for h in range(NH)]
        for h in range(NH):
            hs = slice(h * 64, (h + 1) * 64)
            p.op("dve" if h % 2 else "pool", "tensor_tensor", out=W[h][:, 64:128], in0=ld["kk"][i][:, hs], in1=Pm["ex"][:, hs],
                 op=ALU.mult, reads=[ld["kk"][i], Pm["ex"]], writes=[W[h]])
        for h in range(NH):
            hs = slice(h * 64, (h + 1) * 64)
            for j, (src, sl) in enumerate(((W[h], slice(64, 128)), (rcb, hs), (khb, hs), (nbb, hs))):
                p.op("pe", "transpose", pT[0:64, j * 128:(j + 1) * 128], src[:, sl], ident[:], reads=[src, ident], writes=[pT])
            p.op("act", "copy", out=FT[h][:, :, :], in_=pT[0:64, 0:512].rearrange("p (j n) -> p j n", n=128), reads=[pT], writes=[FT[h]])
        X = [Xm[h][0] for h in range(NH)]
        Y = [None] * NH
        for h in range(NH):
            hs = slice(h * 64, (h + 1) * 64)
            rhs2 = FT[h][:, 0:2, :].rearrange("p j n -> p (j n)")
            p.op("pe", "matmul", pA[:, 0:256], lhsT=FT[h][:, 3, :], rhs=rhs2, start=True, stop=True, reads=[FT[h]], writes=[pA])
            p.op("dve", "tensor_tensor", out=YN[h][:], in0=pA[:, 0:256], in1=mk["m2"][:], op=ALU.mult, reads=[pA, mk["m2"]], writes=[YN[h]])
            p.op("pe", "matmul", pB[:, 0:256], lhsT=FT[h][:, 2, :], rhs=rhs2, start=True, stop=True, reads=[FT[h]], writes=[pB])
            p.op("dve", "tensor_tensor", out=AG[h][:], in0=pB[:, 0:256], in1=mk["m2"][:], op=ALU.mult, reads=[pB, mk["m2"]], writes=[AG[h]])
            ps = nps()
            p.op("pe", "matmul", ps[:, 0:128], lhsT=FT[h][:, 0, :], rhs=FT[h][:, 3, :], start=True, stop=True, reads=[FT[h]], writes=[ps])
            p.op("dve", "tensor_tensor", out=X[h][:], in0=ps[:, 0:128], in1=mk["mst"][:], op=ALU.mult, reads=[ps, mk["mst"]], writes=[X[h]])
            ps = nps()
            p.op("pe", "matmul", ps[:, 0:64], lhsT=AG[h][:, 0:128], rhs=vb[:, hs], start=True, stop=True, reads=[AG[h], vb], writes=[ps])
            p.op("act", "copy", out=W[h][:, 0:64], in_=ps[:, 0:64], reads=[ps], writes=[W[h]])
        Ycur = [(YN[h], slice(0, 128)) for h in range(NH)]
        for j in range(6):
            for h in range(NH):
                yb, ysl = Ycur[h]
                wi, wo_ = Wall[h][j % 2], Wall[h][(j + 1) % 2]
                ps = nps()
                p.op("pe", "matmul", ps[:, 0:128], lhsT=yb[:, ysl], rhs=wi[:, :], start=True, stop=False, reads=[yb, wi], writes=[ps])
                p.op("pe", "matmul", ps[:, 0:128], lhsT=ident[:, :], rhs=wi[:, :], start=False, stop=True, reads=[ident, wi], writes=[ps])
                p.op("act" if h % 2 else "dve", "copy" if h % 2 else "tensor_copy", out=wo_[:], in_=ps[:, 0:128], reads=[ps], writes=[wo_])
            if j < 5:
                for h in range(NH):
                    yb, ysl = Ycur[h]
                    xi = Xm[h][j % 2]
                    xo, yo = Xm[h][(j + 1) % 2], Ym[h][(j + 1) % 2]
                    ps = nps()
                    p.op("pe", "matmul", ps[:, 0:128], lhsT=xi[:, :], rhs=yb[:, ysl], start=True, stop=True, reads=[xi, yb], writes=[ps])
                    p.op("act", "copy", out=yo[:], in_=ps[:, 0:128], reads=[ps], writes=[yo])
                    ps = nps()
                    p.op("pe", "matmul", ps[:, 0:128], lhsT=yb[:, ysl], rhs=xi[:, :], start=True, stop=True, reads=[xi, yb], writes=[ps])
                    p.op("dve", "tensor_copy", out=xo[:], in_=ps[:, 0:128], reads=[ps], writes=[xo])
                    Ycur[h] = (yo, slice(0, 128))
        TW = [Wall[h][0] for h in range(NH)]
        for h in range(NH):
            hs = slice(h * 64, (h + 1) * 64)
            ps = nps()
            p.op("pe", "matmul", ps[:, 0:128], lhsT=YN[h][:, 128:256], rhs=TW[h][:, :], start=True, stop=False, reads=[YN[h], TW[h]], writes=[ps])
            p.op("pe", "matmul", ps[:, 0:64], lhsT=AG[h][:, 128:256], rhs=vb[:, hs], start=False, stop=False, reads=[AG[h], vb], writes=[ps])
            p.op("pe", "matmul", ps[:, 64:128], lhsT=ident[:, :], rhs=rcb[:, hs], start=False, stop=True, reads=[ident, rcb], writes=[ps])
            p.op("act", "copy", out=Y0f[h][:], in_=ps[:, 0:64], reads=[ps], writes=[Y0f[h]])
            p.op("dve", "tensor_copy", out=M2b[h][:], in_=ps[:, 64:128], reads=[ps], writes=[M2b[h]])
            p.op("pe", "transpose", pT[0:64, 0:128], M2b[h][:, :], ident[:], reads=[M2b[h], ident], writes=[pT])
            p.op("act", "copy", out=M2T[h][:], in_=pT[0:64, 0:128], reads=[pT], writes=[M2T[h]])
            for c in range(2):
                cr = slice(c * 64, (c + 1) * 64)
                ps = nps()
                p.op("pe", "matmul", ps[0:64, 0:64], lhsT=TW[h][cr, 64:128], rhs=nbb[cr, hs], start=True, stop=True,
                     reads=[TW[h], nbb], writes=[ps])
                p.op("dve", "tensor_tensor", out=M3T[h][c][:], in0=ps[0:64, 0:64], in1=identf[0:64, 0:64], op=ALU.add,
                     reads=[ps, identf], writes=[M3T[h][c]])
                ps = nps()
                p.op("pe", "matmul", ps[0:64, 0:64], lhsT=khb[cr, hs], rhs=vb[cr, hs], start=True, stop=False, reads=[khb, vb], writes=[ps])
                p.op("pe", "matmul", ps[0:64, 0:64], lhsT=nbb[cr, hs], rhs=TW[h][cr, 0:64], start=False, stop=True,
                     reads=[nbb, TW[h]], writes=[ps])
                p.op("dve", "tensor_scalar", out=Z0P[h][c][:], in0=ps[0:64, 0:64], scalar1=Pc[:, 2 * h + c:2 * h + c + 1], scalar2=None,
                     op0=ALU.mult, reads=[ps, Pc], writes=[Z0P[h][c]])
        yt = ytile[ycnt[0] % 2]
        ycnt[0] += 1
        for c in ((0, 1) if d == 0 else (1, 0)):
            cr = slice(c * 64, (c + 1) * 64)
            for h in range(NH):
                hs = slice(h * 64, (h + 1) * 64)
                hh, hl, h32 = Hhi[d][h], Hlo[d][h], H32[d][h]
                p.op("pe", "matmul", pYc[cr, hs], lhsT=M2T[h][:, cr], rhs=hh[:, :], start=True, stop=False, reads=[M2T[h], hh], writes=[pYc])
                p.op("pe", "matmul", pYc[cr, hs], lhsT=M2T[h][:, cr], rhs=hl[:, :], start=False, stop=True, reads=[M2T[h], hl], writes=[pYc])
                p.op("pe", "matmul", pHc[0:64, 0:64], lhsT=M3T[h][c][:, :], rhs=hh[:, :], start=True, stop=False, reads=[M3T[h][c], hh], writes=[pHc])
                p.op("pe", "matmul", pHc[0:64, 0:64], lhsT=M3T[h][c][:, :], rhs=hl[:, :], start=False, stop=True, reads=[M3T[h][c], hl], writes=[pHc])
                p.op("dve", "tensor_tensor", out=yt[cr, hs], in0=pYc[cr, hs], in1=Y0f[h][cr, :], op=ALU.add, reads=[pYc, Y0f[h]], writes=[yt])
                p.op("dve", "scalar_tensor_tensor", out=h32[:], in0=pHc[0:64, 0:64], scalar=Pc[:, 2 * h + c:2 * h + c + 1], in1=Z0P[h][c][:],
                     op0=ALU.mult, op1=ALU.add, reads=[pHc, Pc, Z0P[h][c]], writes=[h32])
                p.op("act", "copy", out=hh[:], in_=h32[:], reads=[h32], writes=[hh])
                p.op("dve", "tensor_tensor", out=hl[:], in0=h32[:], in1=hh[:], op=ALU.subtract, reads=[h32, hh], writes=[hl])
        if d == 0:
            fy = p.dma("sp", f_y[rows, :], yt[:], reads=[yt])
            fence_y.r.append(fy)
        else:
            p.dma("sp", yf[:], f_y[rows, :], reads=[fence_y], writes=[yf])
            p.dma("sp", og["g"][:], f_g[rows, :], reads=[fence], writes=[og["g"]])
            p.dma("sp", og["bon"][:], f_bon[rows, :], reads=[fence], writes=[og["bon"]])
            p.op("pool", "tensor_tensor", out=yt[:], in0=yt[:], in1=yf[:], op=ALU.add, reads=[yt, yf], writes=[yt])
            p.op("dve", "tensor_reduce", out=st4[0][:], in_=v4(yt), axis=AX.X, op=ALU.add, reads=[yt], writes=[st4[0]])
            p.op("dve", "tensor_scalar", out=st4[0][:], in0=st4[0][:], scalar1=1.0 / 64, scalar2=None, op0=ALU.mult, reads=[st4[0]], writes=[st4[0]])
            p.op("dve", "tensor_tensor", out=v4(og["yc"]), in0=v4(yt), in1=b4(st4[0]), op=ALU.subtract, reads=[yt, st4[0]], writes=[og["yc"]])
            p.op("act", "activation", out=og["sq"][:], in_=og["yc"][:], func=AF.Square, reads=[og["yc"]], writes=[og["sq"]])
            p.op("dve", "tensor_reduce", out=st4[1][:], in_=v4(og["sq"]), axis=AX.X, op=ALU.add, reads=[og["sq"]], writes=[st4[1]])
            p.op("act", "activation", out=st4[1][:], in_=st4[1][:], func=AF.Sqrt, bias=eps_ln[:, 0:1], scale=1.0 / 64,
                 reads=[st4[1], eps_ln], writes=[st4[1]])
            p.op("dve", "reciprocal", out=st4[1][:], in_=st4[1][:], reads=[st4[1]], writes=[st4[1]])
            p.op("dve", "tensor_tensor", out=v4(og["yc"]), in0=v4(og["yc"]), in1=b4(st4[1]), op=ALU.mult, reads=[og["yc"], st4[1]], writes=[og["yc"]])
            p.op("pool", "tensor_tensor", out=og["yc"][:], in0=og["yc"][:], in1=lnwb[:], op=ALU.mult, reads=[og["yc"], lnwb], writes=[og["yc"]])
            p.op("pool", "tensor_tensor", out=og["yc"][:], in0=og["yc"][:], in1=lnbb[:], op=ALU.add, reads=[og["yc"], lnbb], writes=[og["yc"]])
            p.op("pool", "tensor_tensor", out=og["yc"][:], in0=og["yc"][:], in1=og["bon"][:], op=ALU.add, reads=[og["yc"], og["bon"]], writes=[og["yc"]])
            p.op("pool", "tensor_tensor", out=og["sq"][:], in0=og["yc"][:], in1=og["g"][:], op=ALU.mult, reads=[og["yc"], og["g"]], writes=[og["sq"]])
            outs.append(p.dma("sp", o_out[rows, :], og["sq"][:], reads=[og["sq"]]))

    fence_y = Buf(None, "fence_y")
    nlat = ntiles - 2
    order_f = [0, 1] + [2 + i for i in range(nlat)]
    order_b = [1, 0] + [2 + i for i in range(nlat - 1, -1, -1)]
    for t in order_f:
        do_tile(0, t)
    p.op("pool", "memset", fdum[:], 0.0, writes=[fence_y, fdum])
    for t in order_b:
        do_tile(1, t)
    p.emit(final_waits=outs)


def _shift_rows(a, k):
    out = np.zeros_like(a)
    if k == 1:
        out[1:] = a[:-1]
    else:
        out[:-1] = a[1:]
    return out


def rwkv_inputs(x_lat, x_ctx, c, c_ctx, mod_w, mod_b, n1g, P):
    maps = []
    for ci in range(8):
        b, hg = ci // 4, ci % 4
        cs = slice(hg * 256, (hg + 1) * 256)
        xa = np.concatenate([x_ctx[b], x_lat[b]], 0)
        xp = np.concatenate([_shift_rows(x_ctx[b], 1), _shift_rows(x_lat[b], 1)], 0)
        xn = np.concatenate([_shift_rows(x_ctx[b], -1), _shift_rows(x_lat[b], -1)], 0)
        mpn = np.ones((RT_TOK, 2), np.float32)
        mpn[0, 0] = 0
        mpn[256, 0] = 0
        mpn[255, 1] = 0
        mpn[RT_TOK - 1, 1] = 0
        vecs = np.stack([P["w0"][0][cs], P["w0"][1][cs], P["a0"][0][cs], P["a0"][1][cs], P["k_k"][cs], P["k_a"][cs],
                         P["r_k"].reshape(-1)[cs], P["ln_w"][cs], P["ln_b"][cs]], 0)
        maps.append(dict(
            xa=np.ascontiguousarray(xa), xp=np.ascontiguousarray(xp), xn=np.ascontiguousarray(xn), mpn=mpn,
            cnd=_cnd(c[b], c_ctx), modw=np.ascontiguousarray(mod_w[:, 0:2048]), modb=np.ascontiguousarray(mod_b[None, 0:2048]),
            n1g=np.ascontiguousarray(n1g[None, :]), mu=np.ascontiguousarray(P["mu"]),
            w_r=np.ascontiguousarray(P["w_r"][:, cs]), w_k=np.ascontiguousarray(P["w_k"][:, cs]),
            w_v=np.ascontiguousarray(P["w_v"][:, cs]), g1=np.ascontiguousarray(P["g1"]),
            g2=np.ascontiguousarray(P["g2"][:, cs]),
            w1=np.ascontiguousarray(np.concatenate([P["w1"][0], P["w1"][1]], 1)),
            w2=np.ascontiguousarray(np.concatenate([P["w2"][0][:, cs], P["w2"][1][:, cs]], 1)),
            a1=np.ascontiguousarray(np.concatenate([P["a1"][0], P["a1"][1]], 1)),
            a2=np.ascontiguousarray(np.concatenate([P["a2"][0][:, cs], P["a2"][1][:, cs]], 1)),
            vecs=np.ascontiguousarray(vecs.astype(np.float32))))
    return maps


_PROGS = {}


def _prog(key, fn):
    if key not in _PROGS:
        _PROGS[key] = fn()
    return _PROGS[key]


def _run(nc, maps):
    return run_bass_kernel_spmd(nc, maps, core_ids=list(range(8))).results


def kernel(x, c, ctx, c_ctx, mod_w, mod_b, norm1_g, norm2_g, a_w_qkv, a_w_o, a_q_gain, a_k_gain,
           b_mu, b_w_r, b_w_k, b_w_v, b_w_o, b_decay_w0, b_decay_w1, b_decay_w2, b_iclr_a0, b_iclr_a1,
           b_iclr_a2, b_gate_g1, b_gate_g2, b_k_k, b_k_a, b_r_k, b_ln_w, b_ln_b, c_w_qkv, c_w_o, c_sink,
           moe_w_group, moe_b_group, moe_w_expert, moe_b_expert, moe_w1, moe_w3, moe_w2, final_g):
    f32 = lambda a: np.ascontiguousarray(np.asarray(a, dtype=np.float32))
    x_lat, x_ctx = f32(x), f32(ctx)
    c, c_ctx = f32(c), f32(c_ctx)
    mod_w, mod_b = f32(mod_w), f32(mod_b)
    y_last = None
    for l in range(4):
        kind, j = l % 3, l // 3
        mw, mb = mod_w[l], mod_b[l]
        n1g, n2g = f32(norm1_g[l]), f32(norm2_g[l])
        if kind == 0:
            nc = _prog("attn_d", lambda: build_attn(True))
            maps = attn_inputs(True, x_lat, x_ctx, c, c_ctx, mw, mb, n1g, f32(a_w_qkv[j]), f32(a_q_gain[j]),
                               f32(a_k_gain[j]), np.zeros(16, np.float32))
            res = _run(nc, maps)
            wo = f32(a_w_o[j])
        elif kind == 2:
            nc = _prog("attn_w", lambda: build_attn(False))
            ones = np.ones(64, np.float32)
            maps = attn_inputs(False, x_lat, x_ctx, c, c_ctx, mw, mb, n1g, f32(c_w_qkv[j]), ones, ones, f32(c_sink[j]))
            res = _run(nc, maps)
            wo = f32(c_w_o[j])
        else:
            nc = _prog("rwkv", lambda: build_rwkv())
            P = dict(mu=f32(b_mu[j]), w_r=f32(b_w_r[j]), w_k=f32(b_w_k[j]), w_v=f32(b_w_v[j]), w0=f32(b_decay_w0[j]),
                     w1=f32(b_decay_w1[j]), w2=f32(b_decay_w2[j]), a0=f32(b_iclr_a0[j]), a1=f32(b_iclr_a1[j]),
                     a2=f32(b_iclr_a2[j]), g1=f32(b_gate_g1[j]), g2=f32(b_gate_g2[j]), k_k=f32(b_k_k[j]), k_a=f32(b_k_a[j]),
                     r_k=f32(b_r_k[j]), ln_w=f32(b_ln_w[j]), ln_b=f32(b_ln_b[j]))
            maps = rwkv_inputs(x_lat, x_ctx, c, c_ctx, mw, mb, n1g, P)
            res = _run(nc, maps)
            wo = f32(b_w_o[j])
        o_lat = np.zeros((2, 8192, 1024), np.float32)
        o_ctx = np.zeros((2, 256, 1024), np.float32)
        if kind == 1:
            for ci in range(8):
                b, hg = ci // 4, ci % 4
                o = res[ci]["o_out"]
                o_ctx[b][:, hg * 256:(hg + 1) * 256] = o[:256]
                o_lat[b][:, hg * 256:(hg + 1) * 256] = o[256:]
        else:
            for ci in range(8):
                b, qi = ci // 4, ci % 4
                o_lat[b, qi * 2048:(qi + 1) * 2048] = res[ci]["o_lat"]
                if qi == 0:
                    o_ctx[b] = res[ci]["o_ctx"]
        del res
        nc1 = _prog("post1", build_post1)
        res1 = _run(nc1, post1_inputs(x_lat, x_ctx, o_lat, o_ctx, c, c_ctx, mw, mb, n2g, wo, f32(moe_w_group[l]),
                                      f32(moe_b_group[l]), f32(moe_w_expert[l]), f32(moe_b_expert[l])))
        nc2 = _prog("post2", build_post2)
        res2 = _run(nc2, post2_inputs(res1, c, c_ctx, mw, mb, f32(final_g), f32(moe_w1[l]), f32(moe_w3[l]), f32(moe_w2[l])))
        x_lat, x_ctx = _unrows(res2, "x_out")
        if l == 3:
            y_last, _ = _unrows(res2, "y_fin")
    return y_last
```

```python
import contextlib
import numpy as np
import concourse.bass as bass
import concourse.mybir as mybir
from concourse.bass_utils import run_bass_kernel_spmd

F32 = mybir.dt.float32
BF16 = mybir.dt.bfloat16
I32 = mybir.dt.int32
AF = mybir.ActivationFunctionType
ALU = mybir.AluOpType
AX = mybir.AxisListType


class Buf:
    __slots__ = ("ap", "w", "r", "name", "psum")

    def __init__(self, ap, name="", psum=False):
        self.psum = psum
        self.ap = ap
        self.w = None
        self.r = []
        self.name = name

    def __getitem__(self, idx):
        return self.ap[idx]


class Ins:
    __slots__ = ("eng", "fn", "deps", "is_dma", "idx", "slot", "target", "need", "cnt", "prev_slot")

    def __init__(self, eng, fn, is_dma):
        self.eng = eng
        self.fn = fn
        self.deps = {}
        self.is_dma = is_dma
        self.need = False
        self.cnt = 0
        self.slot = None
        self.target = 0
        self.prev_slot = None


COMPUTE = ("pe", "dve", "act", "pool")
_DBG = {}
NSLOT = 8


class Prog:
    def __init__(self, nc):
        self.nc = nc
        self.es = contextlib.ExitStack()
        self.q = {e: [] for e in ("pe", "dve", "act", "pool", "sp")}
        self.dma_count = {e: 0 for e in self.q}
        self.slot_last = {}
        self.n_sb = 0

    def sb(self, shape, dt, name=None):
        self.n_sb += 1
        t = self.es.enter_context(self.nc.sbuf_tensor(name or f"sb{self.n_sb}", list(shape), dt))
        return t

    def ps(self, shape, dt, name=None):
        self.n_sb += 1
        t = self.es.enter_context(self.nc.psum_tensor(name or f"ps{self.n_sb}", list(shape), dt))
        return t

    def buf(self, shape, dt, name=None):
        return Buf(self.sb(shape, dt, name), name or "")

    def pbuf(self, shape, dt, name=None):
        return Buf(self.ps(shape, dt, name), name or "", psum=True)

    def _deps(self, ins, reads, writes):
        cand = []
        for b in reads:
            if b.w is not None:
                cand.append(b.w)
            if b.psum:
                cand.extend(x for x in b.r if x.eng != ins.eng)
        for b in writes:
            if b.w is not None:
                cand.append(b.w)
            cand.extend(b.r)
        for d in cand:
            if d is ins:
                continue
            if d.is_dma:
                ins.deps[("dma", id(d))] = d
            else:
                if d.eng == "pe" and ins.eng == "pe" and not ins.is_dma:
                    continue
                k = ("c", d.eng)
                if k not in ins.deps or ins.deps[k].idx < d.idx:
                    ins.deps[k] = d
        for b in reads:
            b.r.append(ins)
        for b in writes:
            b.w = ins
            b.r = []

    def op(self, eng, name, *args, reads=(), writes=(), **kw):
        fn = (lambda e: getattr(e, name)(*args, **kw))
        ins = Ins(eng, fn, False)
        ins.idx = len(self.q[eng])
        self._deps(ins, reads, writes)
        self.q[eng].append(ins)
        return ins

    def dma(self, eng, out, in_, reads=(), writes=(), **kw):
        ins = Ins(eng, lambda e: e.dma_start(out=out, in_=in_, **kw), True)
        ins.idx = len(self.q[eng])
        k = self.dma_count[eng]
        self.dma_count[eng] += 1
        ins.slot = (eng, k % NSLOT)
        ins.target = 16 * (k // NSLOT + 1)
        ins.prev_slot = self.slot_last.get(ins.slot)
        self.slot_last[ins.slot] = ins
        self._deps(ins, reads, writes)
        self.q[eng].append(ins)
        return ins

    def emit(self, final_waits=()):
        nc = self.nc
        es = self.es
        sem = {e: es.enter_context(nc.semaphore(f"s_{e}")) for e in COMPUTE}
        dsem = {}
        for e in ("sp", "pool", "act"):
            if self.dma_count[e]:
                for s in range(NSLOT):
                    dsem[(e, s)] = es.enter_context(nc.semaphore(f"d_{e}{s}"))
        for e, lst in self.q.items():
            for ins in lst:
                for k, d in ins.deps.items():
                    if k[0] == "c":
                        d.need = True
        for e in COMPUTE:
            c = 0
            for ins in self.q[e]:
                if ins.is_dma:
                    continue
                if ins.need:
                    c += 1
                    ins.cnt = c
        engobj = {"pe": "tensor", "dve": "vector", "act": "scalar", "pool": "gpsimd", "sp": "sync"}
        final = list(final_waits)
        block = es.enter_context(nc.Block())

        def run(e):
            def body(eng):
                waited = {}
                def wait(s, key, v):
                    if waited.get(key, 0) >= v:
                        return
                    waited[key] = v
                    eng.wait_ge(s, v)
                for ins in self.q[e]:
                    if ins.is_dma and ins.prev_slot is not None:
                        wait(dsem[ins.slot], ("d",) + ins.slot, ins.prev_slot.target)
                    for k, d in ins.deps.items():
                        if k[0] == "c":
                            wait(sem[d.eng], ("c", d.eng), d.cnt)
                        else:
                            wait(dsem[d.slot], ("d",) + d.slot, d.target)
                    bi = ins.fn(eng)
                    if ins.is_dma:
                        bi.then_inc(dsem[ins.slot], 16)
                    elif ins.need:
                        bi.then_inc(sem[e], 1)
                if e == "sp":
                    for d in final:
                        wait(dsem[d.slot], ("d",) + d.slot, d.target)
            return body
        for e in ("sp", "pe", "dve", "act", "pool"):
            if not self.q[e] and e != "sp":
                continue
            getattr(block, engobj[e])(run(e))
        es.close()


def mk_ident(p, dt_list=(BF16,)):
    identf = p.buf([128, 128], F32, "identf")
    p.op("pool", "memset", identf[:], 0.0, writes=[identf])
    p.op("pool", "affine_select", out=identf[:], in_=identf[:], pattern=[[-1, 128]],
                                           compare_op=ALU.not_equal, fill=1.0, base=0, channel_multiplier=1,
         reads=[identf], writes=[identf])
    outs = {F32: identf}
    for d in dt_list:
        if d == F32:
            continue
        t = p.buf([128, 128], d, "identb")
        p.op("dve", "tensor_copy", out=t[:], in_=identf[:], reads=[identf], writes=[t])
        outs[d] = t
    return outs


def mk_sel2(p):
    sels = []
    for r in range(2):
        s = p.buf([2, 128], F32, f"sel{r}")
        p.op("pool", "memset", s[:], 1.0, writes=[s])
        p.op("pool", "affine_select", out=s[:], in_=s[:], pattern=[[0, 128]],
                                                         compare_op=ALU.is_equal, fill=0.0, base=-r, channel_multiplier=1,
             reads=[s], writes=[s])
        sels.append(s)
    return sels


def modulation(p, cnd, modw, modb, ncol, ps_small, stage, consumer):
    sc = p.buf([128, 8, 2], F32, "sc")
    p.dma("sp", sc[:], cnd[:, :, :], writes=[sc])
    p.op("act", "activation", out=sc[:], in_=sc[:], func=AF.Silu, reads=[sc], writes=[sc])
    Mc = p.buf([2, 512], F32, "Mc")
    mb = p.buf([2, 512], F32, "mb")
    for c in range(ncol // 512):
        p.dma("sp", mb[:], modb[0:1, c * 512:(c + 1) * 512].to_broadcast([2, 512]), writes=[mb])
        stl = stage if isinstance(stage, list) else [stage]
        nper = 8 // len(stl)
        for si, st in enumerate(stl):
            p.dma("sp", st[:, 0:nper * 512].rearrange("p (k n) -> p k n", n=512),
                  modw[si * nper * 128:(si + 1) * nper * 128, c * 512:(c + 1) * 512].rearrange("(k q) n -> q k n", q=128),
                  writes=[st])
        for k in range(8):
            st = stl[k // nper]
            p.op("pe", "matmul", ps_small[0:2, 0:512], lhsT=sc[:, k, :], rhs=st[:, (k % nper) * 512:(k % nper + 1) * 512],
                 start=(k == 0), stop=(k == 7), reads=[sc, st], writes=[ps_small])
        p.op("dve", "tensor_tensor", out=Mc[:], in0=ps_small[0:2, 0:512], in1=mb[:], op=ALU.add,
             reads=[ps_small, mb], writes=[Mc])
        consumer(c, Mc)


def bcast_chunk(p, sels, src, dst_lat, dst_ctx, col0, ps):
    for r, dst in ((0, dst_lat), (1, dst_ctx)):
        if dst is None:
            continue
        p.op("pe", "matmul", ps[:, 0:512], lhsT=sels[r][:, :], rhs=src[:, :], start=True, stop=True,
             reads=[sels[r], src], writes=[ps])
        p.op("act", "copy", out=dst[:, col0:col0 + 512], in_=ps[:, 0:512], reads=[ps], writes=[dst])


def bcast_rows(p, sels, src, col0, dst_lat, dst_ctx, ps):
    for r, dst in ((0, dst_lat), (1, dst_ctx)):
        if dst is None:
            continue
        for c in range(2):
            p.op("pe", "matmul", ps[:, 0:512], lhsT=sels[r][:, :],
                                                    rhs=src[:, col0 + c * 512: col0 + (c + 1) * 512],
                                                    start=True, stop=True,
                 reads=[sels[r], src], writes=[ps])
            p.op("act", "copy", out=dst[:, c * 512:(c + 1) * 512], in_=ps[:, 0:512],
                 reads=[ps], writes=[dst])


def build_attn(dense):
    NKV = 4 if dense else 2
    KVW = NKV * 64
    QKVC = 1024 + 2 * KVW
    NKT = 64 if dense else 18
    NT = 2 + NKT
    GRP = 16 // NKV
    nc = bass.Bass("TRN2", target_bir_lowering=False)

    def din(name, shape, dt=F32):
        return nc.dram_tensor(name, list(shape), dt, kind="ExternalInput").ap()
    xkv = din("xkv", [NKT * 128, 1024])
    xq = din("xq", [2048, 1024])
    xc = din("xc", [256, 1024])
    cnd = din("cnd", [128, 8, 2])
    modw = din("modw", [1024, 2048])
    modb = din("modb", [1, 2048])
    n1g = din("n1g", [1, 1024])
    wqkv = din("wqkv", [1024, QKVC])
    qg = din("qg", [1, 64])
    kg = din("kg", [1, 64])
    sink = din("sink", [1, 16])
    cos_kv = din("cos_kv", [NKT * 128, 64])
    sin_kv = din("sin_kv", [NKT * 128, 64])
    cos_q = din("cos_q", [2048, 64])
    sin_q = din("sin_q", [2048, 64])
    kvalid = din("kvalid", [128, NT])
    o_lat = nc.dram_tensor("o_lat", [2048, 1024], F32, kind="ExternalOutput").ap()
    o_ctx = nc.dram_tensor("o_ctx", [256, 1024], F32, kind="ExternalOutput").ap()

    p = Prog(nc)
    ident = mk_ident(p)[BF16]
    sels = mk_sel2(p)
    pT = p.pbuf([128, 1024], BF16, "pT")
    pq = p.pbuf([128, 1024], F32, "pq")
    pkv = p.pbuf([128, 512], F32, "pkv")
    pss = [p.pbuf([128, 512], F32, f"pss{i}") for i in range(2)]
    pacc = [p.pbuf([128, 512], F32, f"pacc{i}") for i in range(2)]

    stage = p.buf([128, 4096], F32, "stage")
    g2 = p.buf([2, 1024], F32, "g2")
    p.dma("sp", g2[:], n1g[0:1, :].to_broadcast([2, 1024]), writes=[g2])
    Gvc = p.buf([2, 512], F32, "Gvc")
    Gb = [p.buf([128, 1024], F32, f"Gb{r}") for r in range(2)]
    Sb = [p.buf([128, 1024], F32, f"Sb{r}") for r in range(2)]

    def mod_consumer(c, Mc):
        if c < 2:
            bcast_chunk(p, sels, Mc, Sb[0], Sb[1], c * 512, pq)
        else:
            cc = c - 2
            p.op("dve", "scalar_tensor_tensor", out=Gvc[:], in0=Mc[:], scalar=1.0, in1=g2[:, cc * 512:(cc + 1) * 512],
                 op0=ALU.add, op1=ALU.mult, reads=[Mc, g2], writes=[Gvc])
            bcast_chunk(p, sels, Gvc, Gb[0], Gb[1], cc * 512, pq)
    modulation(p, cnd, modw, modb, 2048, pkv, stage, mod_consumer)

    eps = p.buf([128, 1], F32, "eps")
    p.op("pool", "memset", eps[:], 1e-6, writes=[eps])
    qgb = p.buf([128, 64], F32, "qgb")
    kgb = p.buf([128, 64], F32, "kgb")
    p.dma("sp", qgb[:], qg[0:1, :].to_broadcast([128, 64]), writes=[qgb])
    p.dma("sp", kgb[:], kg[0:1, :].to_broadcast([128, 64]), writes=[kgb])
    esink = p.buf([128, 16], F32, "esink")
    p.dma("sp", esink[:], sink[0:1, :].to_broadcast([128, 16]), writes=[esink])
    p.op("act", "activation", out=esink[:], in_=esink[:], func=AF.Exp, reads=[esink], writes=[esink])
    kval = p.buf([128, NT], F32, "kval")
    p.dma("sp", kval[:], kvalid[:, :], writes=[kval])

    wb = p.buf([128, 8, QKVC], BF16, "wb")
    for k in range(8):
        p.dma("sp", stage[:, 0:QKVC], wqkv[k * 128:(k + 1) * 128, :], writes=[stage])
        p.op("pool", "tensor_copy", out=wb[:, k, :], in_=stage[:, 0:QKVC], reads=[stage], writes=[wb])

    mprev = p.buf([128, 128], BF16, "mprev")
    mnext = p.buf([128, 128], BF16, "mnext")
    if not dense:
        mf = p.buf([128, 128], F32, "mf")
        for (dst, sg) in ((mprev, 1), (mnext, -1)):
            p.op("pool", "memset", mf[:], 1.0, writes=[mf])
            p.op("pool", "affine_select", out=mf[:], in_=mf[:], pattern=[[-sg, 128]], compare_op=ALU.is_ge,
                                                            fill=0.0, base=0, channel_multiplier=sg,
                 reads=[mf], writes=[mf])
            p.op("dve", "tensor_copy", out=dst[:], in_=mf[:], reads=[mf], writes=[dst])

    KT = p.sb([128, NKV // 2, NT * 128], BF16, "KT")
    VE = p.sb([128, NT, NKV, 65], BF16, "VE")
    KTb = [Buf(KT, f"KT{t}") for t in range(NT)]
    VEb = [Buf(VE, f"VE{t}") for t in range(NT)]
    veall = Buf(VE, "VEall")
    ins0 = p.op("pool", "memset", VE[:, :, :, 64:65], 1.0, writes=[veall] + VEb)
    QT = [p.buf([128, 8, 512], BF16, f"QT{i}") for i in range(2)]

    xt = [p.buf([128, 1024], F32, f"xt{i}") for i in range(2)]
    qn = p.buf([128, 1024], F32, "qn")
    t1 = p.buf([128, 1024], F32, "t1")
    qr = p.buf([128, 1024], BF16, "qr")
    cs = [p.buf([128, 64], F32, f"cs{i}") for i in range(2)]
    sn = [p.buf([128, 64], F32, f"sn{i}") for i in range(2)]
    WS = []
    for wi in range(2):
        WS.append(dict(junk=p.buf([128, 1024], F32, f"junk{wi}"), ss=p.buf([128, 1], F32, f"ss{wi}"),
                       hn=p.buf([128, 1024], F32, f"hn{wi}"), hb=p.buf([128, 1024], BF16, f"hb{wi}"),
                       hT=p.buf([128, 1024], BF16, f"hT{wi}"), ssq=p.buf([128, 16], F32, f"ssq{wi}"),
                       kn=p.buf([128, KVW], F32, f"kn{wi}"), k1=p.buf([128, KVW], F32, f"k1{wi}"),
                       kr=p.buf([128, KVW], BF16, f"kr{wi}")))
    W_ = dict(WS[0])
    cnt = [0]

    def headnorm(src_ps, dst, nh, gainb):
        w = nh * 64
        if not dense:
            p.op("act", "copy", out=dst[:, 0:w], in_=src_ps, reads=[srcbuf[0]], writes=[dst])
            return
        p.op("act", "activation", out=W_["junk"][:, 0:w], in_=src_ps, func=AF.Square, reads=[srcbuf[0]], writes=[W_["junk"]])
        p.op("dve", "tensor_reduce", out=W_["ssq"][:, 0:nh], in_=W_["junk"][:, 0:w].rearrange("p (h d) -> p h d", d=64),
                                              axis=AX.X, op=ALU.add, reads=[W_["junk"]], writes=[W_["ssq"]])
        p.op("act", "activation", out=W_["ssq"][:, 0:nh], in_=W_["ssq"][:, 0:nh], func=AF.Sqrt, bias=eps[:, 0:1], scale=1.0 / 64,
             reads=[W_["ssq"], eps], writes=[W_["ssq"]])
        p.op("dve", "reciprocal", out=W_["ssq"][:, 0:nh], in_=W_["ssq"][:, 0:nh], reads=[W_["ssq"]], writes=[W_["ssq"]])
        p.op("dve", "tensor_tensor", out=dst[:, 0:w].rearrange("p (h d) -> p h d", d=64),
                                              in0=src_ps.rearrange("p (h d) -> p h d", d=64),
                                              in1=W_["ssq"][:, 0:nh].unsqueeze(2).to_broadcast([128, nh, 64]), op=ALU.mult,
             reads=[srcbuf[0], W_["ssq"]], writes=[dst])
        p.op("pool", "tensor_tensor", out=dst[:, 0:w].rearrange("p (h d) -> p h d", d=64),
                                               in0=dst[:, 0:w].rearrange("p (h d) -> p h d", d=64),
                                               in1=gainb[:, :].unsqueeze(1).to_broadcast([128, nh, 64]), op=ALU.mult,
             reads=[dst, gainb], writes=[dst])
    srcbuf = [None]

    def rope(src, tmp, dst, nh, c_t, s_t):
        w = nh * 64
        v3 = lambda t: t[:, 0:w].rearrange("p (h d) -> p h d", d=64)
        v4 = lambda t: t[:, 0:w].rearrange("p (h a t d) -> p (h a) t d", a=2, t=2, d=16)
        s4 = s_t[:, :].rearrange("p (a t d) -> p a t d", a=2, t=2, d=16)
        p.op("dve", "tensor_tensor", out=v3(tmp), in0=v3(src), in1=c_t[:, :].unsqueeze(1).to_broadcast([128, nh, 64]),
                                              op=ALU.mult, reads=[src, c_t], writes=[tmp])
        for tt in range(2):
            p.op("pool", "tensor_tensor", out=W_["junk"][:, 0:w].rearrange("p (h a t d) -> p h a t d", a=2, t=2, d=16)[:, :, :, tt, :],
                in0=src[:, 0:w].rearrange("p (h a t d) -> p h a t d", a=2, t=2, d=16)[:, :, :, 1 - tt, :],
                in1=s4[:, :, tt, :].unsqueeze(1).to_broadcast([128, nh, 2, 16]), op=ALU.mult,
                reads=[src, s_t], writes=[W_["junk"]])
        p.op("dve", "tensor_tensor", out=dst[:, 0:w], in0=tmp[:, 0:w], in1=W_["junk"][:, 0:w], op=ALU.add,
             reads=[tmp, W_["junk"]], writes=[dst])

    def proc_tile(rows_ap, r, need_q, need_kv, tix, qt_buf, qcol, cos_ap, sin_ap):
        i = cnt[0] % 2
        cnt[0] += 1
        W_.update(WS[i])
        x = xt[i]
        p.dma("sp", x[:], rows_ap, writes=[x])
        if r == 0:
            p.dma("sp", cs[i][:], cos_ap, writes=[cs[i]])
            p.dma("sp", sn[i][:], sin_ap, writes=[sn[i]])
        p.op("act", "activation", out=W_["junk"][:], in_=x[:], func=AF.Square, accum_out=W_["ss"][:], reads=[x], writes=[W_["junk"], W_["ss"]])
        p.op("act", "activation", out=W_["ss"][:], in_=W_["ss"][:], func=AF.Sqrt, bias=eps[:, 0:1], scale=1.0 / 1024,
             reads=[W_["ss"], eps], writes=[W_["ss"]])
        p.op("dve", "reciprocal", out=W_["ss"][:], in_=W_["ss"][:], reads=[W_["ss"]], writes=[W_["ss"]])
        p.op("dve", "scalar_tensor_tensor", out=W_["hn"][:], in0=x[:], scalar=W_["ss"][:, 0:1], in1=Gb[r][:],
                                                     op0=ALU.mult, op1=ALU.mult, reads=[x, W_["ss"], Gb[r]], writes=[W_["hn"]])
        p.op("pool", "tensor_tensor", out=W_["hb"][:], in0=W_["hn"][:], in1=Sb[r][:], op=ALU.add, reads=[W_["hn"], Sb[r]], writes=[W_["hb"]])
        for k in range(8):
            p.op("pe", "transpose", pT[:, k * 128:(k + 1) * 128], W_["hb"][:, k * 128:(k + 1) * 128], ident[:],
                 reads=[W_["hb"], ident], writes=[pT])
        p.op("act", "copy", out=W_["hT"][:], in_=pT[:], reads=[pT], writes=[W_["hT"]])
        if need_q:
            for c in range(2):
                for k in range(8):
                    p.op("pe", "matmul", pq[:, c * 512:(c + 1) * 512], lhsT=W_["hT"][:, k * 128:(k + 1) * 128],
                                                            rhs=wb[:, k, c * 512:(c + 1) * 512], start=(k == 0), stop=(k == 7),
                         reads=[W_["hT"], wb], writes=[pq])
            srcbuf[0] = pq
            headnorm(pq[:, :], qn, 16, qgb)
            if r == 0:
                rope(qn, t1, qr, 16, cs[i], sn[i])
            else:
                p.op("dve", "tensor_copy", out=qr[:], in_=qn[:], reads=[qn], writes=[qr])
            for j in range(8):
                p.op("pe", "transpose", pT[:, j * 128:(j + 1) * 128], qr[:, j * 128:(j + 1) * 128], ident[:],
                     reads=[qr, ident], writes=[pT])
            p.op("act", "copy", out=qt_buf[:, :, qcol:qcol + 128], in_=pT[:, :].rearrange("p (j n) -> p j n", n=128),
                 reads=[pT], writes=[qt_buf])
        if need_kv:
            for k in range(8):
                p.op("pe", "matmul", pkv[:, 0:2 * KVW], lhsT=W_["hT"][:, k * 128:(k + 1) * 128],
                                                   rhs=wb[:, k, 1024:1024 + 2 * KVW], start=(k == 0), stop=(k == 7),
                     reads=[W_["hT"], wb], writes=[pkv])
            srcbuf[0] = pkv
            headnorm(pkv[:, 0:KVW], W_["kn"], NKV, kgb)
            if r == 0:
                rope(W_["kn"], W_["k1"], W_["kr"], NKV, cs[i], sn[i])
            else:
                p.op("dve", "tensor_copy", out=W_["kr"][:], in_=W_["kn"][:], reads=[W_["kn"]], writes=[W_["kr"]])
            nch = KVW // 128
            for j in range(nch):
                p.op("pe", "transpose", pT[:, j * 128:(j + 1) * 128], W_["kr"][:, j * 128:(j + 1) * 128], ident[:],
                     reads=[W_["kr"], ident], writes=[pT])
            p.op("act", "copy", out=KT[:, :, tix * 128:(tix + 1) * 128],
                                         in_=pT[:, 0:nch * 128].rearrange("p (j n) -> p j n", n=128),
                 reads=[pT], writes=[KTb[tix]])
            p.op("act", "copy", out=VE[:, tix, :, 0:64], in_=pkv[:, KVW:2 * KVW].rearrange("p (g d) -> p g d", d=64),
                 reads=[pkv], writes=[VEb[tix]])
            if not dense and r == 0:
                p.op("dve", "tensor_scalar", out=VE[:, tix, :, :], in0=VE[:, tix, :, :], scalar1=kval[:, tix:tix + 1],
                                                      scalar2=None, op0=ALU.mult, reads=[VEb[tix], kval], writes=[VEb[tix]])

    for t in range(2):
        proc_tile(xc[t * 128:(t + 1) * 128, :], 1, False, True, t, None, 0, None, None)
    for t in range(NKT):
        proc_tile(xkv[t * 128:(t + 1) * 128, :], 0, False, True, 2 + t, None, 0,
                  cos_kv[t * 128:(t + 1) * 128, :], sin_kv[t * 128:(t + 1) * 128, :])

    pts = [p.buf([128, 512], BF16, f"pt{i}") for i in range(3)]
    otile = [Buf(stage.ap[:, i * 1024:(i + 1) * 1024], f"ot{i}") for i in range(4)]
    rden = p.buf([128, 1], F32, "rden")
    outs = []
    state = {"mm": 0, "pt": 0, "acc": 0, "blk": 0}

    def attn_block(qtiles, keylists, out_aps, is_ctx):
        b = state["blk"] % 2
        state["blk"] += 1
        qt_buf = QT[b]
        for qi in range(qtiles):
            if is_ctx:
                proc_tile(xc[qi * 128:(qi + 1) * 128, :], 1, True, False, 0, qt_buf, qi * 128, None, None)
            else:
                r0 = out_aps[qi][1]
                proc_tile(xq[r0:r0 + 128, :], 0, True, False, 0, qt_buf, qi * 128,
                          cos_q[r0:r0 + 128, :], sin_q[r0:r0 + 128, :])
        same = all(keylists[qi] == keylists[0] for qi in range(qtiles))
        groups = [list(range(qtiles))] if same else [[qi] for qi in range(qtiles)]
        for h in range(16):
            half, j = h // 8, h % 8
            g = h // GRP
            ki = g % (NKV // 2)
            ps_rows = slice(half * 64, half * 64 + 64)
            pa = pacc[state["acc"] % 2]
            state["acc"] += 1
            for grp in groups:
                kl = keylists[grp[0]]
                c0, c1 = grp[0] * 128, (grp[-1] + 1) * 128
                n = c1 - c0
                def emit_mm2(idx, kt, pt):
                    for qq, qi in enumerate(grp):
                        p.op("pe", "matmul", pa[:, qi * 128:qi * 128 + 65], lhsT=pt[:, qq * 128:(qq + 1) * 128], rhs=VE[:, kt, g, :],
                             start=(idx == 0 and qq == 0 and grp is groups[0]), stop=(idx == len(kl) - 1),
                             reads=[pt, VEb[kt]], writes=[pa])
                pend = None
                for idx, (kt, mask) in enumerate(kl):
                    ps_ = pss[state["mm"] % 2]
                    state["mm"] += 1
                    pt = pts[state["pt"] % 3]
                    state["pt"] += 1
                    p.op("pe", "matmul", ps_[:, 0:n], lhsT=KT[ps_rows, ki, kt * 128:(kt + 1) * 128],
                         rhs=qt_buf[ps_rows, j, c0:c1], start=True, stop=True, reads=[KTb[kt], qt_buf], writes=[ps_])
                    p.op("act", "activation", out=pt[:, 0:n], in_=ps_[:, 0:n], func=AF.Exp, scale=0.125, reads=[ps_], writes=[pt])
                    if mask is not None:
                        p.op("dve", "tensor_tensor", out=pt[:, 0:n], in0=pt[:, 0:n], in1=mask[:, :], op=ALU.mult,
                             reads=[pt, mask], writes=[pt])
                    if pend is not None:
                        emit_mm2(*pend)
                    pend = (idx, kt, pt)
                emit_mm2(*pend)
            for qi in range(qtiles):
                ot = out_aps[qi][2]
                if dense:
                    p.op("dve", "reciprocal", out=rden[:], in_=pa[:, qi * 128 + 64:qi * 128 + 65],
                         reads=[pa], writes=[rden])
                else:
                    p.op("dve", "tensor_tensor", out=rden[:], in0=pa[:, qi * 128 + 64:qi * 128 + 65],
                                                                 in1=esink[:, h:h + 1], op=ALU.add,
                         reads=[pa, esink], writes=[rden])
                    p.op("dve", "reciprocal", out=rden[:], in_=rden[:], reads=[rden], writes=[rden])
                p.op("dve", "tensor_scalar", out=ot[:, h * 64:(h + 1) * 64], in0=pa[:, qi * 128:qi * 128 + 64],
                                                                    scalar1=rden[:, 0:1], scalar2=None, op0=ALU.mult,
                     reads=[pa, rden], writes=[ot])
        for qi in range(qtiles):
            dst, r0, ot = out_aps[qi]
            outs.append(p.dma("sp", dst[r0:r0 + 128, :], ot[:], reads=[ot]))

    if _DBG.get('p1only'):
        p.emit(final_waits=outs)
        return nc
    attn_block(2, [[(0, None), (1, None)]] * 2, [(o_ctx, 0, otile[0]), (o_ctx, 128, otile[1])], True)
    if dense:
        allk = [(t, None) for t in range(NT)]
        for qb in range(4):
            attn_block(4, [allk] * 4, [(o_lat, (qb * 4 + qi) * 128, otile[qi]) for qi in range(4)], False)
    else:
        for qt in range(16):
            kl = [(0, None), (1, None), (2 + qt, mprev), (3 + qt, None), (4 + qt, mnext)]
            attn_block(1, [kl], [(o_lat, qt * 128, otile[qt % 4])], False)
    p.emit(final_waits=outs)
    return nc


def _rope_tables():
    rows = 128
    row = np.repeat(np.arange(rows, dtype=np.float32), 64)
    col = np.tile(np.arange(64, dtype=np.float32), rows)
    inv = (np.float32(10000.0) ** (-np.arange(0, 32, 2, dtype=np.float32) / np.float32(32))).astype(np.float32)
    ar = row[:, None] * inv
    ac = col[:, None] * inv
    ang = np.concatenate([ar, ar, ac, ac], -1)
    cos = np.cos(ang).astype(np.float32)
    sin = np.sin(ang).astype(np.float32)
    sgn = np.tile(np.concatenate([-np.ones(16, np.float32), np.ones(16, np.float32)]), 2)
    return cos, sin * sgn


def _perm_qkv_cols(w, nkv):
    qcols = []
    for j in range(8):
        qcols += list(range(j * 64, j * 64 + 64)) + list(range((j + 8) * 64, (j + 8) * 64 + 64))
    kcols = []
    for i in range(nkv // 2):
        for g in (i, i + nkv // 2):
            kcols += list(range(1024 + g * 64, 1024 + g * 64 + 64))
    vcols = list(range(1024 + nkv * 64, 1024 + 2 * nkv * 64))
    return np.ascontiguousarray(w[:, qcols + kcols + vcols])


def attn_inputs(dense, x, ctx, c, c_ctx, mod_w, mod_b, n1g, wqkv, qg, kg, sink):
    nkv = 4 if dense else 2
    cos, sin = _rope_tables()
    wp = _perm_qkv_cols(wqkv, nkv)
    maps = []
    for ci in range(8):
        b, qi = ci // 4, ci % 4
        r0 = qi * 2048
        cnd = np.ascontiguousarray(np.stack([c[b], c_ctx], -1).reshape(8, 128, 2).transpose(1, 0, 2))
        if dense:
            xkv = x[b]
            ckv, skv = cos, sin
            kvalid = np.ones((128, 66), np.float32)
        else:
            xkv = np.zeros((18 * 128, 1024), np.float32)
            ckv = np.zeros((18 * 128, 64), np.float32)
            skv = np.zeros((18 * 128, 64), np.float32)
            lo, hi = max(0, r0 - 128), min(8192, r0 + 2048 + 128)
            off = lo - (r0 - 128)
            xkv[off:off + hi - lo] = x[b, lo:hi]
            ckv[off:off + hi - lo] = cos[lo:hi]
            skv[off:off + hi - lo] = sin[lo:hi]
            kvalid = np.ones((128, 20), np.float32)
            if r0 - 128 < 0:
                kvalid[:, 2] = 0
            if r0 + 2048 + 128 > 8192:
                kvalid[:, 19] = 0
        maps.append(dict(
            xkv=np.ascontiguousarray(xkv), xq=np.ascontiguousarray(x[b, r0:r0 + 2048]), xc=np.ascontiguousarray(ctx[b]),
            cnd=cnd, modw=np.ascontiguousarray(mod_w[:, 0:2048]), modb=np.ascontiguousarray(mod_b[None, 0:2048]),
            n1g=np.ascontiguousarray(n1g[None, :]), wqkv=wp, qg=np.ascontiguousarray(qg[None, :]),
            kg=np.ascontiguousarray(kg[None, :]), sink=np.ascontiguousarray(sink[None, :]),
            cos_kv=np.ascontiguousarray(ckv), sin_kv=np.ascontiguousarray(skv),
            cos_q=np.ascontiguousarray(cos[r0:r0 + 2048]), sin_q=np.ascontiguousarray(sin[r0:r0 + 2048]),
            kvalid=kvalid))
    return maps


NTOK = 2304
NTILE = 18


def build_post1(ntile=NTILE, upto=99):
    nc = bass.Bass("TRN2", target_bir_lowering=False)

    def din(name, shape, dt=F32):
        return nc.dram_tensor(name, list(shape), dt, kind="ExternalInput").ap()

    def dout(name, shape, dt=F32):
        return nc.dram_tensor(name, list(shape), dt, kind="ExternalOutput").ap()
    xr = din("xr", [NTOK, 1024])
    o = din("o", [NTOK, 1024])
    cnd = din("cnd", [128, 8, 2])
    modw = din("modw", [1024, 3072])
    modb = din("modb", [1, 3072])
    n2g = din("n2g", [1, 1024])
    wo = din("wo", [1024, 1024])
    wr = din("wr", [1024, 36])
    br = din("br", [1, 36])
    x_mid = dout("x_mid", [NTOK, 1024])
    h2T = dout("h2T", [128, 8, NTOK], BF16)
    gates = dout("gates", [NTOK, 32])

    p = Prog(nc)
    idt = mk_ident(p)
    ident, identf = idt[BF16], idt[F32]
    sels = mk_sel2(p)
    pT = p.pbuf([128, 1024], BF16, "pT")
    pq = p.pbuf([128, 1024], F32, "pq")
    pkv = p.pbuf([128, 512], F32, "pkv")

    stage = p.buf([128, 4096], F32, "stage")
    g2 = p.buf([2, 1024], F32, "g2")
    p.dma("sp", g2[:], n2g[0:1, :].to_broadcast([2, 1024]), writes=[g2])
    Gvc = p.buf([2, 512], F32, "Gvc")
    G1b = [p.buf([128, 1024], F32, f"G1b{r}") for r in range(2)]
    S2b = [p.buf([128, 1024], F32, f"S2b{r}") for r in range(2)]
    G2b = [p.buf([128, 1024], F32, f"G2b{r}") for r in range(2)]

    def mod_consumer(c, Mc):
        if c < 2:
            bcast_chunk(p, sels, Mc, G1b[0], G1b[1], c * 512, pq)
        elif c < 4:
            bcast_chunk(p, sels, Mc, S2b[0], S2b[1], (c - 2) * 512, pq)
        else:
            cc = c - 4
            p.op("dve", "scalar_tensor_tensor", out=Gvc[:], in0=Mc[:], scalar=1.0, in1=g2[:, cc * 512:(cc + 1) * 512],
                 op0=ALU.add, op1=ALU.mult, reads=[Mc, g2], writes=[Gvc])
            bcast_chunk(p, sels, Gvc, G2b[0], G2b[1], cc * 512, pq)
    modulation(p, cnd, modw, modb, 3072, pkv, stage, mod_consumer)

    eps = p.buf([128, 1], F32, "eps")
    p.op("pool", "memset", eps[:], 1e-6, writes=[eps])
    wob = p.buf([128, 8, 1024], BF16, "wob")
    for k in range(8):
        p.dma("sp", stage[:, 0:1024], wo[k * 128:(k + 1) * 128, :], writes=[stage])
        p.op("pool", "tensor_copy", out=wob[:, k, :], in_=stage[:, 0:1024], reads=[stage], writes=[wob])
    wrt = p.buf([128, 8, 36], F32, "wrt")
    p.dma("sp", wrt[:], wr[:, :].rearrange("(k q) n -> q k n", q=128), writes=[wrt])
    brb = p.buf([128, 36], F32, "brb")
    p.dma("sp", brb[:], br[0:1, :].to_broadcast([128, 36]), writes=[brb])

    xt = [p.buf([128, 1024], F32, f"xt{i}") for i in range(2)]
    ot = [p.buf([128, 1024], F32, f"ot{i}") for i in range(2)]
    ob_ = [p.buf([128, 1024], BF16, f"ob{_i}") for _i in range(2)]
    oT_ = [p.buf([128, 1024], BF16, f"oT{_i}") for _i in range(2)]
    tmp_ = [p.buf([128, 1024], F32, f"tmp{_i}") for _i in range(2)]
    xm = [p.buf([128, 1024], F32, f"xm{i}") for i in range(2)]
    junk_ = [p.buf([128, 1024], F32, f"junk{_i}") for _i in range(2)]
    ss_ = [p.buf([128, 1], F32, f"ss{_i}") for _i in range(2)]
    h2_ = [p.buf([128, 1024], F32, f"h2{_i}") for _i in range(2)]
    hhi_ = [p.buf([128, 1024], BF16, f"hhi{_i}") for _i in range(2)]
    hlo_ = [p.buf([128, 1024], BF16, f"hlo{_i}") for _i in range(2)]
    loT_ = [p.buf([128, 1024], BF16, f"loT{_i}") for _i in range(2)]
    wr_hi = p.buf([128, 8, 36], BF16, "wr_hi")
    wr_lo = p.buf([128, 8, 36], BF16, "wr_lo")
    p.op("pool", "tensor_copy", out=wr_hi[:], in_=wrt[:], reads=[wrt], writes=[wr_hi])
    p.op("dve", "tensor_tensor", out=wr_lo[:], in0=wrt[:], in1=wr_hi[:], op=ALU.subtract, reads=[wrt, wr_hi], writes=[wr_lo])
    h2Tb = [p.buf([128, 1024], BF16, f"h2Tb{i}") for i in range(2)]
    lg_ = [p.buf([128, 36], F32, f"lg{_i}") for _i in range(2)]
    sm2 = [{n: p.buf([128, w], F32, f"{n}_{_i}") for n, w in (("gmax", 1), ("ngmax", 1), ("goh", 4), ("gexp", 4), ("gsum", 1), ("gw", 1),
                                                   ("tmp48", 32), ("esel", 8), ("m1", 1), ("oh1", 8), ("e2", 8), ("m2", 1),
                                                   ("oh2", 8), ("d", 1), ("ed", 1), ("den", 1), ("w1", 1), ("w2", 1),
                                                   ("ga", 8), ("gsel", 8))} for _i in range(2)]
    gt = [p.buf([128, 32], F32, f"gt{i}") for i in range(2)]
    outs = []

    cur = [0]

    def S(n):
        return sm2[cur[0]][n]

    for t in range(ntile):
        r = 1 if t < 2 else 0
        i = t % 2
        rows = slice(t * 128, (t + 1) * 128)
        cur[0] = i
        ob, oT, tmp, junk, ss, h2, hhi, hlo, loT, lg = (x_[i] for x_ in (ob_, oT_, tmp_, junk_, ss_, h2_, hhi_, hlo_, loT_, lg_))
        p.dma("sp", ot[i][:], o[rows, :], writes=[ot[i]])
        p.dma("sp", xt[i][:], xr[rows, :], writes=[xt[i]])
        p.op("pool", "tensor_copy", out=ob[:], in_=ot[i][:], reads=[ot[i]], writes=[ob])
        for k in range(8):
            p.op("pe", "transpose", pT[:, k * 128:(k + 1) * 128], ob[:, k * 128:(k + 1) * 128], ident[:],
                 reads=[ob, ident], writes=[pT])
        p.op("act", "copy", out=oT[:], in_=pT[:], reads=[pT], writes=[oT])
        for c in range(2):
            for k in range(8):
                p.op("pe", "matmul", pq[:, c * 512:(c + 1) * 512], lhsT=oT[:, k * 128:(k + 1) * 128],
                     rhs=wob[:, k, c * 512:(c + 1) * 512], start=(k == 0), stop=(k == 7), reads=[oT, wob], writes=[pq])
        p.op("dve", "tensor_tensor", out=tmp[:], in0=pq[:, :], in1=G1b[r][:], op=ALU.mult, reads=[pq, G1b[r]], writes=[tmp])
        p.op("pool", "tensor_tensor", out=xm[i][:], in0=tmp[:], in1=xt[i][:], op=ALU.add, reads=[tmp, xt[i]], writes=[xm[i]])
        outs.append(p.dma("sp", x_mid[rows, :], xm[i][:], reads=[xm[i]]))
        if upto < 1:
            continue
        p.op("act", "activation", out=junk[:], in_=xm[i][:], func=AF.Square, accum_out=ss[:], reads=[xm[i]], writes=[junk, ss])
        p.op("act", "activation", out=ss[:], in_=ss[:], func=AF.Sqrt, bias=eps[:, 0:1], scale=1.0 / 1024,
             reads=[ss, eps], writes=[ss])
        p.op("dve", "reciprocal", out=ss[:], in_=ss[:], reads=[ss], writes=[ss])
        p.op("dve", "scalar_tensor_tensor", out=tmp[:], in0=xm[i][:], scalar=ss[:, 0:1], in1=G2b[r][:], op0=ALU.mult, op1=ALU.mult,
             reads=[xm[i], ss, G2b[r]], writes=[tmp])
        p.op("pool", "tensor_tensor", out=h2[:], in0=tmp[:], in1=S2b[r][:], op=ALU.add, reads=[tmp, S2b[r]], writes=[h2])
        if upto < 2:
            continue
        p.op("pool", "tensor_copy", out=hhi[:], in_=h2[:], reads=[h2], writes=[hhi])
        p.op("dve", "tensor_tensor", out=hlo[:], in0=h2[:], in1=hhi[:], op=ALU.subtract, reads=[h2, hhi], writes=[hlo])
        for k in range(8):
            p.op("pe", "transpose", pT[:, k * 128:(k + 1) * 128], hhi[:, k * 128:(k + 1) * 128], ident[:],
                 reads=[hhi, ident], writes=[pT])
        p.op("act", "copy", out=h2Tb[i][:], in_=pT[:], reads=[pT], writes=[h2Tb[i]])
        for k in range(8):
            p.op("pe", "transpose", pT[:, k * 128:(k + 1) * 128], hlo[:, k * 128:(k + 1) * 128], ident[:],
                 reads=[hlo, ident], writes=[pT])
        p.op("act", "copy", out=loT[:], in_=pT[:], reads=[pT], writes=[loT])
        if upto >= 2.5:
            outs.append(p.dma("sp", h2T[:, :, t * 128:(t + 1) * 128], h2Tb[i][:, :].rearrange("p (k n) -> p k n", n=128),
                              reads=[h2Tb[i]]))
        if upto < 3:
            continue
        n_mm = 0
        for (lt, wt) in ((h2Tb[i], wr_hi), (h2Tb[i], wr_lo), (loT, wr_hi)):
            for k in range(8):
                p.op("pe", "matmul", pkv[:, 0:36], lhsT=lt[:, k * 128:(k + 1) * 128], rhs=wt[:, k, :],
                     start=(n_mm == 0), stop=(n_mm == 23), reads=[lt, wt], writes=[pkv])
                n_mm += 1
        p.op("dve", "tensor_tensor", out=lg[:], in0=pkv[:, 0:36], in1=brb[:], op=ALU.add, reads=[pkv, brb], writes=[lg])
        if upto < 4:
            continue
        gl = lg[:, 0:4]
        el = lg[:, 4:36].rearrange("p (g e) -> p g e", e=8)
        p.op("dve", "tensor_reduce", out=S("gmax")[:], in_=gl, axis=AX.X, op=ALU.max, reads=[lg], writes=[S("gmax")])
        p.op("dve", "tensor_scalar", out=S("goh")[:], in0=gl, scalar1=S("gmax")[:, 0:1], scalar2=None, op0=ALU.is_equal,
             reads=[lg, S("gmax")], writes=[S("goh")])
        p.op("dve", "tensor_scalar", out=S("ngmax")[:], in0=S("gmax")[:], scalar1=-1.0, scalar2=None, op0=ALU.mult,
             reads=[S("gmax")], writes=[S("ngmax")])
        p.op("act", "activation", out=S("gexp")[:], in_=gl, func=AF.Exp, bias=S("ngmax")[:, 0:1], scale=1.0,
             accum_out=S("gsum")[:], reads=[lg, S("ngmax")], writes=[S("gexp"), S("gsum")])
        p.op("dve", "reciprocal", out=S("gw")[:], in_=S("gsum")[:], reads=[S("gsum")], writes=[S("gw")])
        p.op("dve", "tensor_tensor", out=S("tmp48")[:, :].rearrange("p (g e) -> p g e", e=8), in0=el,
             in1=S("goh")[:, :].unsqueeze(2).to_broadcast([128, 4, 8]), op=ALU.mult, reads=[lg, S("goh")], writes=[S("tmp48")])
        p.op("dve", "tensor_reduce", out=S("esel")[:], in_=S("tmp48")[:, :].rearrange("p (g e) -> p e g", e=8), axis=AX.X,
             op=ALU.add, reads=[S("tmp48")], writes=[S("esel")])
        p.op("dve", "tensor_reduce", out=S("m1")[:], in_=S("esel")[:], axis=AX.X, op=ALU.max, reads=[S("esel")], writes=[S("m1")])
        p.op("dve", "tensor_scalar", out=S("oh1")[:], in0=S("esel")[:], scalar1=S("m1")[:, 0:1], scalar2=None, op0=ALU.is_equal,
             reads=[S("esel"), S("m1")], writes=[S("oh1")])
        p.op("dve", "scalar_tensor_tensor", out=S("e2")[:], in0=S("oh1")[:], scalar=-1e30, in1=S("esel")[:], op0=ALU.mult,
             op1=ALU.add, reads=[S("oh1"), S("esel")], writes=[S("e2")])
        p.op("dve", "tensor_reduce", out=S("m2")[:], in_=S("e2")[:], axis=AX.X, op=ALU.max, reads=[S("e2")], writes=[S("m2")])
        p.op("dve", "tensor_scalar", out=S("oh2")[:], in0=S("e2")[:], scalar1=S("m2")[:, 0:1], scalar2=None, op0=ALU.is_equal,
             reads=[S("e2"), S("m2")], writes=[S("oh2")])
        p.op("dve", "tensor_tensor", out=S("d")[:], in0=S("m2")[:], in1=S("m1")[:], op=ALU.subtract,
             reads=[S("m2"), S("m1")], writes=[S("d")])
        p.op("act", "activation", out=S("ed")[:], in_=S("d")[:], func=AF.Exp, reads=[S("d")], writes=[S("ed")])
        p.op("dve", "tensor_scalar", out=S("den")[:], in0=S("ed")[:], scalar1=1.0, scalar2=None, op0=ALU.add,
             reads=[S("ed")], writes=[S("den")])
        p.op("dve", "reciprocal", out=S("w1")[:], in_=S("den")[:], reads=[S("den")], writes=[S("w1")])
        p.op("dve", "tensor_tensor", out=S("w1")[:], in0=S("w1")[:], in1=S("gw")[:], op=ALU.mult,
             reads=[S("w1"), S("gw")], writes=[S("w1")])
        p.op("dve", "tensor_tensor", out=S("w2")[:], in0=S("w1")[:], in1=S("ed")[:], op=ALU.mult,
             reads=[S("w1"), S("ed")], writes=[S("w2")])
        p.op("dve", "tensor_scalar", out=S("ga")[:], in0=S("oh1")[:], scalar1=S("w1")[:, 0:1], scalar2=None, op0=ALU.mult,
             reads=[S("oh1"), S("w1")], writes=[S("ga")])
        p.op("dve", "scalar_tensor_tensor", out=S("gsel")[:], in0=S("oh2")[:], scalar=S("w2")[:, 0:1], in1=S("ga")[:],
             op0=ALU.mult, op1=ALU.add, reads=[S("oh2"), S("w2"), S("ga")], writes=[S("gsel")])
        p.op("dve", "tensor_tensor", out=gt[i][:, :].rearrange("p (g e) -> p g e", e=8),
             in0=S("goh")[:, :].unsqueeze(2).to_broadcast([128, 4, 8]),
             in1=S("gsel")[:, :].unsqueeze(1).to_broadcast([128, 4, 8]), op=ALU.mult,
             reads=[S("goh"), S("gsel")], writes=[gt[i]])
        outs.append(p.dma("sp", gates[rows, :], gt[i][:], reads=[gt[i]]))
    p.emit(final_waits=outs)
    return nc


def build_post2():
    nc = bass.Bass("TRN2", target_bir_lowering=False)

    def din(name, shape, dt=F32):
        return nc.dram_tensor(name, list(shape), dt, kind="ExternalInput").ap()

    def dout(name, shape, dt=F32):
        return nc.dram_tensor(name, list(shape), dt, kind="ExternalOutput").ap()
    x_mid = din("x_mid", [NTOK, 1024])
    h2T = din("h2T", [128, 8, NTOK], BF16)
    gates = din("gates", [NTOK, 32])
    cnd = din("cnd", [128, 8, 2])
    modw = din("modw", [1024, 1024])
    modb = din("modb", [1, 1024])
    fg = din("fg", [1, 1024])
    w1 = din("w1", [32, 1024, 768])
    w3 = din("w3", [32, 1024, 768])
    w2 = din("w2", [32, 768, 1024])
    x_out = dout("x_out", [NTOK, 1024])
    y_fin = dout("y_fin", [NTOK, 1024])

    p = Prog(nc)
    sels = mk_sel2(p)
    ph1 = [p.pbuf([128, 512], F32, f"ph1{i}") for i in range(2)]
    ph3 = [p.pbuf([128, 512], F32, f"ph3{i}") for i in range(2)]
    py = [p.pbuf([128, 512], F32, f"py{i}") for i in range(2)]
    pm = p.pbuf([128, 512], F32, "pm")
    pm2 = p.pbuf([128, 512], F32, "pm2")

    stg = [p.buf([128, 2048], F32, f"stg{i}") for i in range(2)]
    GTb = [p.buf([128, 1024], F32, f"GTb{r}") for r in range(2)]

    def mod_consumer(c, Mc):
        bcast_chunk(p, sels, Mc, GTb[0], GTb[1], c * 512, pm2)
    modulation(p, cnd, modw, modb, 1024, pm, stg, mod_consumer)
    fgb = p.buf([128, 1024], F32, "fgb")
    p.dma("sp", fgb[:], fg[0:1, :].to_broadcast([128, 1024]), writes=[fgb])
    eps = p.buf([128, 1], F32, "eps")
    p.op("pool", "memset", eps[:], 1e-6, writes=[eps])

    hT = p.buf([128, 8, NTOK], BF16, "hT")
    p.dma("sp", hT[:], h2T[:, :, :], writes=[hT])
    gts = p.buf([128, NTILE, 32], F32, "gts")
    p.dma("sp", gts[:], gates[:, :].rearrange("(t q) e -> q t e", q=128), writes=[gts])
    HT = NTILE // 2
    accs = [p.buf([128, 1024], F32, f"acc{t}") for t in range(HT)]
    w1b = [p.buf([128, 8, 768], BF16, f"w1b{i}") for i in range(2)]
    w3b = [p.buf([128, 8, 768], BF16, f"w3b{i}") for i in range(2)]
    w2b = [p.buf([128, 6, 1024], BF16, f"w2b{i}") for i in range(2)]
    hid = [p.buf([128, 6, 384], BF16, f"hid{i}") for i in range(2)]
    s1 = [p.buf([128, 384], F32, f"s1{i}") for i in range(2)]
    xo = [p.buf([128, 1024], F32, f"xo{i}") for i in range(2)]
    yo = [p.buf([128, 1024], F32, f"yo{i}") for i in range(2)]
    ss = p.buf([128, 1], F32, "ss")
    outs = []
    cnts = {"stg": 0, "h": 0, "y": 0, "g": 0}

    def load_w(e, par):
        for kk in range(4):
            for (src, dst) in ((w1, w1b[par]), (w3, w3b[par])):
                s = stg[cnts["stg"] % 2]
                cnts["stg"] += 1
                p.dma("sp", s[:, 0:1536].rearrange("p (k n) -> p k n", n=768),
                      src[e, kk * 256:(kk + 1) * 256, :].rearrange("(k q) n -> q k n", q=128), writes=[s])
                p.op("pool", "tensor_copy", out=dst[:, 2 * kk:2 * kk + 2, :], in_=s[:, 0:1536].rearrange("p (k n) -> p k n", n=768),
                     reads=[s], writes=[dst])
        for kk in range(3):
            s = stg[cnts["stg"] % 2]
            cnts["stg"] += 1
            p.dma("sp", s[:, :].rearrange("p (k n) -> p k n", n=1024),
                  w2[e, kk * 256:(kk + 1) * 256, :].rearrange("(k q) n -> q k n", q=128), writes=[s])
            p.op("pool", "tensor_copy", out=w2b[par][:, 2 * kk:2 * kk + 2, :], in_=s[:, :].rearrange("p (k n) -> p k n", n=1024),
                 reads=[s], writes=[w2b[par]])

    pend_y = [None]

    def emit_y(e, par, gi, hd, t0):
        for tt in range(3):
            tl = gi * 3 + tt
            tg = t0 + tl
            for c in range(2):
                j = cnts["y"] % 2
                cnts["y"] += 1
                for f in range(6):
                    p.op("pe", "matmul", py[j][:, 0:512], lhsT=hd[:, f, tt * 128:(tt + 1) * 128],
                         rhs=w2b[par][:, f, c * 512:(c + 1) * 512], start=(f == 0), stop=(f == 5),
                         reads=[hd, w2b[par]], writes=[py[j]])
                if e == 0:
                    p.op("dve", "tensor_scalar", out=accs[tl][:, c * 512:(c + 1) * 512], in0=py[j][:, 0:512],
                         scalar1=gts[:, tg, e:e + 1], scalar2=None, op0=ALU.mult,
                         reads=[py[j], gts], writes=[accs[tl]])
                else:
                    p.op("dve", "scalar_tensor_tensor", out=accs[tl][:, c * 512:(c + 1) * 512], in0=py[j][:, 0:512],
                         scalar=gts[:, tg, e:e + 1], in1=accs[tl][:, c * 512:(c + 1) * 512], op0=ALU.mult, op1=ALU.add,
                         reads=[py[j], gts, accs[tl]], writes=[accs[tl]])

    for half in range(2):
        t0 = half * HT
        for e in range(32):
            par = e % 2
            load_w(e, par)
            for gi in range(HT // 3):
                tok0 = (t0 + gi * 3) * 128
                hd = hid[cnts["g"] % 2]
                cnts["g"] += 1
                for f in range(6):
                    j = cnts["h"] % 2
                    cnts["h"] += 1
                    for k in range(8):
                        p.op("pe", "matmul", ph1[j][:, 0:384], lhsT=w1b[par][:, k, f * 128:(f + 1) * 128],
                             rhs=hT[:, k, tok0:tok0 + 384], start=(k == 0), stop=(k == 7), reads=[w1b[par], hT], writes=[ph1[j]])
                    for k in range(8):
                        p.op("pe", "matmul", ph3[j][:, 0:384], lhsT=w3b[par][:, k, f * 128:(f + 1) * 128],
                             rhs=hT[:, k, tok0:tok0 + 384], start=(k == 0), stop=(k == 7), reads=[w3b[par], hT], writes=[ph3[j]])
                    p.op("act", "activation", out=s1[j][:], in_=ph1[j][:, 0:384], func=AF.Silu, reads=[ph1[j]], writes=[s1[j]])
                    p.op("dve", "tensor_tensor", out=hd[:, f, :], in0=s1[j][:], in1=ph3[j][:, 0:384], op=ALU.mult,
                         reads=[s1[j], ph3[j]], writes=[hd])
                if pend_y[0] is not None:
                    emit_y(*pend_y[0])
                pend_y[0] = (e, par, gi, hd, t0)
        emit_y(*pend_y[0])
        pend_y[0] = None
        for tl in range(HT):
            tg = t0 + tl
            r = 1 if tg < 2 else 0
            i = tl % 2
            rows = slice(tg * 128, (tg + 1) * 128)
            p.dma("sp", xo[i][:], x_mid[rows, :], writes=[xo[i]])
            p.op("pool", "tensor_tensor", out=accs[tl][:], in0=accs[tl][:], in1=GTb[r][:], op=ALU.mult,
                 reads=[accs[tl], GTb[r]], writes=[accs[tl]])
            p.op("pool", "tensor_tensor", out=xo[i][:], in0=xo[i][:], in1=accs[tl][:], op=ALU.add,
                 reads=[xo[i], accs[tl]], writes=[xo[i]])
            outs.append(p.dma("sp", x_out[rows, :], xo[i][:], reads=[xo[i]]))
            p.op("act", "activation", out=yo[i][:], in_=xo[i][:], func=AF.Square, accum_out=ss[:], reads=[xo[i]], writes=[yo[i], ss])
            p.op("act", "activation", out=ss[:], in_=ss[:], func=AF.Sqrt, bias=eps[:, 0:1], scale=1.0 / 1024,
                 reads=[ss, eps], writes=[ss])
            p.op("dve", "reciprocal", out=ss[:], in_=ss[:], reads=[ss], writes=[ss])
            p.op("dve", "scalar_tensor_tensor", out=yo[i][:], in0=xo[i][:], scalar=ss[:, 0:1], in1=fgb[:], op0=ALU.mult, op1=ALU.mult,
                 reads=[xo[i], ss, fgb], writes=[yo[i]])
            outs.append(p.dma("sp", y_fin[rows, :], yo[i][:], reads=[yo[i]]))
    p.emit(final_waits=outs)
    return nc


def _cnd(c_b, c_ctx):
    return np.ascontiguousarray(np.stack([c_b, c_ctx], -1).reshape(8, 128, 2).transpose(1, 0, 2))


def _rows(x_lat, x_ctx, ci):
    b, qi = ci // 4, ci % 4
    return np.ascontiguousarray(np.concatenate([x_ctx[b], x_lat[b, qi * 2048:(qi + 1) * 2048]], 0))


def post1_inputs(x_lat, x_ctx, o_lat, o_ctx, c, c_ctx, mod_w, mod_b, n2g, wo, wg, bg, we, be):
    wr = np.ascontiguousarray(np.concatenate([wg, we], 1))
    br = np.ascontiguousarray(np.concatenate([bg, be])[None, :])
    maps = []
    for ci in range(8):
        b = ci // 4
        maps.append(dict(xr=_rows(x_lat, x_ctx, ci), o=_rows(o_lat, o_ctx, ci), cnd=_cnd(c[b], c_ctx),
                         modw=np.ascontiguousarray(mod_w[:, 2048:5120]), modb=np.ascontiguousarray(mod_b[None, 2048:5120]),
                         n2g=np.ascontiguousarray(n2g[None, :]), wo=np.ascontiguousarray(wo), wr=wr, br=br))
    return maps


def post2_inputs(res1, c, c_ctx, mod_w, mod_b, fg, w1, w3, w2):
    maps = []
    for ci in range(8):
        b = ci // 4
        r = res1[ci]
        maps.append(dict(x_mid=r["x_mid"], h2T=r["h2T"], gates=r["gates"], cnd=_cnd(c[b], c_ctx),
                         modw=np.ascontiguousarray(mod_w[:, 5120:6144]), modb=np.ascontiguousarray(mod_b[None, 5120:6144]),
                         fg=np.ascontiguousarray(fg[None, :]), w1=w1, w3=w3, w2=w2))
    return maps


def _unrows(res, key):
    lat = np.zeros((2, 8192, 1024), np.float32)
    ctx = np.zeros((2, 256, 1024), np.float32)
    for ci in range(8):
        b, qi = ci // 4, ci % 4
        a = res[ci][key]
        lat[b, qi * 2048:(qi + 1) * 2048] = a[256:]
        if qi == 0:
            ctx[b] = a[:256]
    return lat, ctx


RT_TOK = 8448
RT_TILES = 66
NDEC = -0.6065306597126334


def build_rwkv(ntiles=RT_TILES, phase_c=True):
    nc = bass.Bass("TRN2", target_bir_lowering=False)

    def din(name, shape, dt=F32):
        return nc.dram_tensor(name, list(shape), dt, kind="ExternalInput").ap()

    def dscr(name, shape, dt=F32, kind="Internal"):
        return nc.dram_tensor(name, list(shape), dt, kind=kind).ap()
    xa = din("xa", [RT_TOK, 1024])
    xp = din("xp", [RT_TOK, 1024])
    xn = din("xn", [RT_TOK, 1024])
    mpn = din("mpn", [RT_TOK, 2])
    cnd = din("cnd", [128, 8, 2])
    modw = din("modw", [1024, 2048])
    modb = din("modb", [1, 2048])
    n1g = din("n1g", [1, 1024])
    mu = din("mu", [6, 1024])
    w_r = din("w_r", [1024, 256])
    w_k = din("w_k", [1024, 256])
    w_v = din("w_v", [1024, 256])
    g1 = din("g1", [1024, 128])
    g2 = din("g2", [128, 256])
    w1 = din("w1", [1024, 128])
    w2 = din("w2", [64, 512])
    a1 = din("a1", [1024, 128])
    a2 = din("a2", [64, 512])
    vecs = din("vecs", [9, 256])
    o_out = nc.dram_tensor("o_out", [RT_TOK, 256], F32, kind="ExternalOutput").ap()
    FK = "ExternalOutput" if not phase_c else "Internal"
    f_r = dscr("f_r", [RT_TOK, 256], kind=FK)
    f_v = dscr("f_v", [RT_TOK, 256], kind=FK)
    f_kk = dscr("f_kk", [RT_TOK, 256], kind=FK)
    f_g = dscr("f_g", [RT_TOK, 256], kind=FK)
    f_bon = dscr("f_bon", [RT_TOK, 256], kind=FK)
    f_kd = [dscr(f"f_kd{d}", [RT_TOK, 256], kind=FK) for d in range(2)]
    f_b = [dscr(f"f_b{d}", [RT_TOK, 256], kind=FK) for d in range(2)]
    f_lw = [dscr(f"f_lw{d}", [RT_TOK, 256], kind=FK) for d in range(2)]
    f_y = dscr("f_y", [RT_TOK, 256])
    f_y2 = dscr("f_y2", [RT_TOK, 256])

    p = Prog(nc)
    idt = mk_ident(p)
    ident, identf = idt[BF16], idt[F32]
    sels = mk_sel2(p)
    pT = p.pbuf([128, 1024], BF16, "pT")
    Q = [p.pbuf([128, 512], F32, f"q{i}") for i in range(7)]

    stage = p.buf([128, 4096], F32, "stage")
    g2n = p.buf([2, 1024], F32, "g2n")
    p.dma("sp", g2n[:], n1g[0:1, :].to_broadcast([2, 1024]), writes=[g2n])
    Gvc = p.buf([2, 512], F32, "Gvc")
    Gb = [p.buf([128, 1024], F32, f"Gb{r}") for r in range(2)]
    Sb = [p.buf([128, 1024], F32, f"Sb{r}") for r in range(2)]

    def mod_consumer(c, Mc):
        if c < 2:
            bcast_chunk(p, sels, Mc, Sb[0], Sb[1], c * 512, Q[1])
        else:
            cc = c - 2
            p.op("dve", "scalar_tensor_tensor", out=Gvc[:], in0=Mc[:], scalar=1.0, in1=g2n[:, cc * 512:(cc + 1) * 512],
                 op0=ALU.add, op1=ALU.mult, reads=[Mc, g2n], writes=[Gvc])
            bcast_chunk(p, sels, Gvc, Gb[0], Gb[1], cc * 512, Q[1])
    modulation(p, cnd, modw, modb, 2048, Q[0], stage, mod_consumer)

    eps = p.buf([128, 1], F32, "eps")
    p.op("pool", "memset", eps[:], 1e-6, writes=[eps])
    eps_ln = p.buf([128, 1], F32, "eps_ln")
    p.op("pool", "memset", eps_ln[:], 64e-5, writes=[eps_ln])
    MUb = [p.buf([128, 1024], F32, f"MUb{i}") for i in range(6)]
    for i in range(6):
        p.dma("sp", MUb[i][:], mu[i:i + 1, :].to_broadcast([128, 1024]), writes=[MUb[i]])
    VC = [p.buf([128, 256], F32, f"vc{i}") for i in range(9)]
    for i in range(9):
        p.dma("sp", VC[i][:], vecs[i:i + 1, :].to_broadcast([128, 256]), writes=[VC[i]])
    w0b, a0b, kkb, kab, rkb, lnwb, lnbb = VC[0:2], VC[2:4], VC[4], VC[5], VC[6], VC[7], VC[8]

    def load_bf(src_ap, shape, name, view):
        dst = p.buf(shape, BF16, name)
        n = shape[-1]
        p.dma("sp", stage[:, 0:8 * n].rearrange("p (k n) -> p k n", n=n), src_ap.rearrange("(k q) n -> q k n", q=128), writes=[stage])
        p.op("pool", "tensor_copy", out=dst[:], in_=stage[:, 0:8 * n].rearrange("p (k n) -> p k n", n=n), reads=[stage], writes=[dst])
        return dst
    wrb = load_bf(w_r[:, :], [128, 8, 256], "wrb", None)
    wkb = load_bf(w_k[:, :], [128, 8, 256], "wkb", None)
    wvb = load_bf(w_v[:, :], [128, 8, 256], "wvb", None)
    g1b = load_bf(g1[:, :], [128, 8, 128], "g1b", None)
    w1b = load_bf(w1[:, :], [128, 8, 128], "w1b", None)
    a1b = load_bf(a1[:, :], [128, 8, 128], "a1b", None)

    def load_small_bf(src_ap, shape, name):
        dst = p.buf(shape, BF16, name)
        p.dma("sp", stage[0:shape[0], 0:shape[1]], src_ap, writes=[stage])
        p.op("pool", "tensor_copy", out=dst[:], in_=stage[0:shape[0], 0:shape[1]], reads=[stage], writes=[dst])
        return dst
    g2b = load_small_bf(g2[:, :], [128, 256], "g2b")
    w2b = load_small_bf(w2[:, :], [64, 512], "w2b")
    a2b = load_small_bf(a2[:, :], [64, 512], "a2b")

    xt3 = [p.buf([128, 1024], F32, f"x3_{i}") for i in range(3)]
    h3 = [p.buf([128, 1024], F32, f"h3_{i}") for i in range(3)]
    msk = p.buf([128, 2], F32, "msk")
    junk = p.buf([128, 1024], F32, "junk")
    ss3 = [p.buf([128, 1], F32, f"ss3_{i}") for i in range(3)]
    xx = p.buf([128, 1024], F32, "xx")
    tmpx = [p.buf([128, 1024], F32, f"tmpx{i}") for i in range(2)]
    xib = [p.buf([128, 1024], BF16, f"xib{i}") for i in range(2)]
    xT = [p.buf([128, 1024], BF16, f"xT{i}") for i in range(6)]
    sgT = p.buf([128, 128], BF16, "sgT")
    lorT = [p.buf([64, 128], BF16, f"lorT{i}") for i in range(4)]
    names = ["r32", "k32", "v32", "g32", "kkr", "kk32", "bon", "t0", "t1", "ks"] + \
            [f"{n}{d}" for n in ("wl", "a32", "lw", "kd", "bb") for d in range(2)]
    T_ = {n: p.buf([128, 256], F32, n) for n in names}
    sm4 = [p.buf([128, 4], F32, f"sm4_{i}") for i in range(3)]
    ew_i = [0]

    def ew():
        ew_i[0] += 1
        return "dve" if ew_i[0] % 2 else "pool"

    def v4(t):
        return t[:, :].rearrange("p (h d) -> p h d", d=64)

    def b4(s):
        return s[:, 0:4].unsqueeze(2).to_broadcast([128, 4, 64])

    feat_out = []
    for t in range(ntiles):
        rr = 1 if t < 2 else 0
        rows = slice(t * 128, (t + 1) * 128)
        for i, src in enumerate((xa, xp, xn)):
            p.dma("sp", xt3[i][:], src[rows, :], writes=[xt3[i]])
        p.dma("sp", msk[:], mpn[rows, :], writes=[msk])
        for i in range(3):
            p.op("act", "activation", out=junk[:], in_=xt3[i][:], func=AF.Square, accum_out=ss3[i][:], reads=[xt3[i]], writes=[junk, ss3[i]])
            p.op("act", "activation", out=ss3[i][:], in_=ss3[i][:], func=AF.Sqrt, bias=eps[:, 0:1], scale=1.0 / 1024,
                 reads=[ss3[i], eps], writes=[ss3[i]])
            p.op("dve", "reciprocal", out=ss3[i][:], in_=ss3[i][:], reads=[ss3[i]], writes=[ss3[i]])
            p.op("dve", "scalar_tensor_tensor", out=h3[i][:], in0=xt3[i][:], scalar=ss3[i][:, 0:1], in1=Gb[rr][:], op0=ALU.mult,
                 op1=ALU.mult, reads=[xt3[i], ss3[i], Gb[rr]], writes=[h3[i]])
            p.op("pool", "tensor_tensor", out=h3[i][:], in0=h3[i][:], in1=Sb[rr][:], op=ALU.add, reads=[h3[i], Sb[rr]], writes=[h3[i]])
        h = h3[0]
        p.op("dve", "tensor_scalar", out=xx[:], in0=h3[1][:], scalar1=msk[:, 0:1], scalar2=None, op0=ALU.mult,
             reads=[h3[1], msk], writes=[xx])
        p.op("dve", "scalar_tensor_tensor", out=xx[:], in0=h3[2][:], scalar=msk[:, 1:2], in1=xx[:], op0=ALU.mult, op1=ALU.add,
             reads=[h3[2], msk, xx], writes=[xx])
        p.op("dve", "scalar_tensor_tensor", out=xx[:], in0=xx[:], scalar=0.5, in1=h[:], op0=ALU.mult, op1=ALU.subtract,
             reads=[xx, h], writes=[xx])
        for i in range(6):
            tm, xb_ = tmpx[i % 2], xib[i % 2]
            p.op("pool", "tensor_tensor", out=tm[:], in0=xx[:], in1=MUb[i][:], op=ALU.mult, reads=[xx, MUb[i]], writes=[tm])
            p.op("dve", "tensor_tensor", out=xb_[:], in0=tm[:], in1=h[:], op=ALU.add, reads=[tm, h], writes=[xb_])
            for k in range(8):
                p.op("pe", "transpose", pT[:, k * 128:(k + 1) * 128], xb_[:, k * 128:(k + 1) * 128], ident[:],
                     reads=[xb_, ident], writes=[pT])
            p.op("act", "copy", out=xT[i][:], in_=pT[:], reads=[pT], writes=[xT[i]])
        for (qi, c0, src, wt) in ((0, 0, xT[0], wrb), (0, 256, xT[2], wkb), (1, 0, xT[3], wvb)):
            for k in range(8):
                p.op("pe", "matmul", Q[qi][:, c0:c0 + 256], lhsT=src[:, k * 128:(k + 1) * 128], rhs=wt[:, k, :],
                     start=(k == 0 and c0 == 0), stop=(k == 7), reads=[src, wt], writes=[Q[qi]])
        for k in range(8):
            p.op("pe", "matmul", Q[2][:, 0:128], lhsT=g1b[:, k, :], rhs=xT[5][:, k * 128:(k + 1) * 128],
                 start=(k == 0), stop=(k == 7), reads=[g1b, xT[5]], writes=[Q[2]])
        p.op("act", "activation", out=sgT[:], in_=Q[2][:, 0:128], func=AF.Sigmoid, reads=[Q[2]], writes=[sgT])
        p.op("pe", "matmul", Q[1][:, 256:512], lhsT=sgT[:, :], rhs=g2b[:, :], start=False, stop=True,
             reads=[sgT, g2b], writes=[Q[1]])
        for li, (src, wa, wb_, fn) in enumerate(((xT[1], w1b, w2b, AF.Tanh), (xT[4], a1b, a2b, AF.Copy))):
            for d in range(2):
                for k in range(8):
                    p.op("pe", "matmul", Q[3][0:64, 0:128], lhsT=wa[:, k, d * 64:(d + 1) * 64], rhs=src[:, k * 128:(k + 1) * 128],
                         start=(k == 0), stop=(k == 7), reads=[wa, src], writes=[Q[3]])
                lt = lorT[li * 2 + d]
                p.op("act", "activation", out=lt[:], in_=Q[3][0:64, 0:128], func=fn, reads=[Q[3]], writes=[lt])
                p.op("pe", "matmul", Q[4 + li][:, d * 256:(d + 1) * 256], lhsT=lt[:, :], rhs=wb_[:, d * 256:(d + 1) * 256],
                     start=(d == 0), stop=True, reads=[lt, wb_], writes=[Q[4 + li]])
        p.op("act", "copy", out=T_["r32"][:], in_=Q[0][:, 0:256], reads=[Q[0]], writes=[T_["r32"]])
        p.op("act", "copy", out=T_["k32"][:], in_=Q[0][:, 256:512], reads=[Q[0]], writes=[T_["k32"]])
        p.op("act", "copy", out=T_["v32"][:], in_=Q[1][:, 0:256], reads=[Q[1]], writes=[T_["v32"]])
        p.op("act", "copy", out=T_["g32"][:], in_=Q[1][:, 256:512], reads=[Q[1]], writes=[T_["g32"]])
        for d in range(2):
            wl, a32, lw = T_[f"wl{d}"], T_[f"a32{d}"], T_[f"lw{d}"]
            p.op("dve", "tensor_tensor", out=wl[:], in0=Q[4][:, d * 256:(d + 1) * 256], in1=w0b[d][:], op=ALU.add,
                 reads=[Q[4], w0b[d]], writes=[wl])
            p.op("act", "activation", out=wl[:], in_=wl[:], func=AF.Sigmoid, reads=[wl], writes=[wl])
            p.op("pool", "tensor_scalar", out=lw[:], in0=wl[:], scalar1=NDEC, scalar2=None, op0=ALU.mult, reads=[wl], writes=[lw])
            p.op("dve", "tensor_tensor", out=a32[:], in0=Q[5][:, d * 256:(d + 1) * 256], in1=a0b[d][:], op=ALU.add,
                 reads=[Q[5], a0b[d]], writes=[a32])
            p.op("act", "activation", out=a32[:], in_=a32[:], func=AF.Sigmoid, reads=[a32], writes=[a32])
        p.op("pool", "tensor_tensor", out=T_["kkr"][:], in0=T_["k32"][:], in1=kkb[:], op=ALU.mult, reads=[T_["k32"], kkb], writes=[T_["kkr"]])
        p.op("act", "activation", out=T_["t0"][:], in_=T_["kkr"][:], func=AF.Square, reads=[T_["kkr"]], writes=[T_["t0"]])
        p.op("dve", "tensor_reduce", out=sm4[0][:], in_=v4(T_["t0"]), axis=AX.X, op=ALU.add, reads=[T_["t0"]], writes=[sm4[0]])
        p.op("act", "activation", out=sm4[0][:], in_=sm4[0][:], func=AF.Sqrt, reads=[sm4[0]], writes=[sm4[0]])
        p.op("dve", "tensor_scalar", out=sm4[0][:], in0=sm4[0][:], scalar1=1e-12, scalar2=None, op0=ALU.max, reads=[sm4[0]], writes=[sm4[0]])
        p.op("dve", "reciprocal", out=sm4[0][:], in_=sm4[0][:], reads=[sm4[0]], writes=[sm4[0]])
        p.op("dve", "tensor_tensor", out=v4(T_["kk32"]), in0=v4(T_["kkr"]), in1=b4(sm4[0]), op=ALU.mult,
             reads=[T_["kkr"], sm4[0]], writes=[T_["kk32"]])
        for d in range(2):
            a32, kd, bb = T_[f"a32{d}"], T_[f"kd{d}"], T_[f"bb{d}"]
            e1 = ew()
            p.op(e1, "scalar_tensor_tensor", out=T_["t1"][:], in0=a32[:], scalar=-1.0, in1=kab[:], op0=ALU.add, op1=ALU.mult,
                 reads=[a32, kab], writes=[T_["t1"]])
            p.op(e1, "scalar_tensor_tensor", out=kd[:], in0=T_["t1"][:], scalar=1.0, in1=T_["k32"][:], op0=ALU.add, op1=ALU.mult,
                 reads=[T_["t1"], T_["k32"]], writes=[kd])
            p.op(ew(), "tensor_tensor", out=bb[:], in0=T_["kk32"][:], in1=a32[:], op=ALU.mult, reads=[T_["kk32"], a32], writes=[bb])
        p.op("pool", "tensor_tensor", out=T_["ks"][:], in0=T_["kd0"][:], in1=T_["kd1"][:], op=ALU.add,
             reads=[T_["kd0"], T_["kd1"]], writes=[T_["ks"]])
        p.op("pool", "tensor_tensor", out=T_["ks"][:], in0=T_["ks"][:], in1=T_["r32"][:], op=ALU.mult, reads=[T_["ks"], T_["r32"]], writes=[T_["ks"]])
        p.op("pool", "tensor_tensor", out=T_["ks"][:], in0=T_["ks"][:], in1=rkb[:], op=ALU.mult, reads=[T_["ks"], rkb], writes=[T_["ks"]])
        p.op("dve", "tensor_reduce", out=sm4[1][:], in_=v4(T_["ks"]), axis=AX.X, op=ALU.add, reads=[T_["ks"]], writes=[sm4[1]])
        p.op("dve", "tensor_tensor", out=v4(T_["bon"]), in0=v4(T_["v32"]), in1=b4(sm4[1]), op=ALU.mult,
             reads=[T_["v32"], sm4[1]], writes=[T_["bon"]])
        for (dst, srcn) in ((f_r, "r32"), (f_v, "v32"), (f_kk, "kk32"), (f_g, "g32"), (f_bon, "bon"),
                            (f_kd[0], "kd0"), (f_kd[1], "kd1"), (f_b[0], "bb0"), (f_b[1], "bb1"),
                            (f_lw[0], "lw0"), (f_lw[1], "lw1")):
            feat_out.append(p.dma("sp", dst[rows, :], T_[srcn][:], reads=[T_[srcn]]))

    if not phase_c:
        p.emit(final_waits=feat_out)
        return nc
    rwkv_phase_c(p, nc, locals())
    return nc


def rwkv_phase_c(p, nc, L):
    ntiles = L["ntiles"]
    ident, identf, pT, Q = L["ident"], L["identf"], L["pT"], L["Q"]
    f_r, f_v, f_kk, f_g, f_bon, f_kd, f_b, f_lw, f_y, o_out = (L[k] for k in
        ("f_r", "f_v", "f_kk", "f_g", "f_bon", "f_kd", "f_b", "f_lw", "f_y", "o_out"))
    lnwb, lnbb, eps_ln, feat_out = L["lnwb"], L["lnbb"], L["eps_ln"], L["feat_out"]

    fence = Buf(None, "fence")
    for d in feat_out:
        fence.r.append(d)
    fdum = p.buf([1, 8], F32, "fdum")
    fd2 = p.buf([1, 8], F32, "fd2")
    fd3 = p.buf([1, 8], F32, "fd3")
    p.op("pool", "memset", fdum[:], 0.0, writes=[fence, fdum])
    p.op("dve", "tensor_copy", out=fd2[:], in_=fdum[:], reads=[fdum], writes=[fd2])
    p.op("act", "copy", out=fd3[:], in_=fdum[:], reads=[fdum], writes=[fd3])
    arena_src = [(b.ap, 1024) for b in (L["MUb"] + L["xt3"] + L["h3"] + L["tmpx"] + [L["xx"], L["junk"]] + L["Gb"] + L["Sb"])]
    arena_src += [(L["stage"].ap[:, i * 1024:(i + 1) * 1024], 1024) for i in range(4)]
    arena_src += [(b.ap[:, :].bitcast(F32), 512) for b in (L["xT"] + L["xib"])]
    ar = {"i": 0, "off": 0}

    def abuf(shape, dt, name):
        nparts = shape[0]
        ncols = 1
        for v_ in shape[1:]:
            ncols *= v_
        nf = ncols if dt == F32 else (ncols + 1) // 2
        while ar["off"] + nf > arena_src[ar["i"]][1]:
            ar["i"] += 1
            ar["off"] = 0
        t = arena_src[ar["i"]][0]
        ap = t[0:nparts, ar["off"]:ar["off"] + nf]
        ar["off"] += nf
        if dt != F32:
            ap = ap.bitcast(dt)
        if len(shape) == 3:
            ap = ap.rearrange("p (j n) -> p j n", n=shape[2])
        return Buf(ap, name)

    def tri_mask(name, sg, strict, transpose=False):
        mf = abuf([128, 128], F32, name + "f")
        p.op("pool", "memset", mf[:], 1.0, writes=[mf])
        s_ = -sg if transpose else sg
        p.op("pool", "affine_select", out=mf[:], in_=mf[:], pattern=[[s_, 128]], compare_op=ALU.is_ge, fill=0.0,
             base=(-1 if strict else 0), channel_multiplier=-s_, reads=[mf], writes=[mf])
        p.op("pool", "memset", mf[0:64, 64:128], 0.0, reads=[mf], writes=[mf])
        p.op("pool", "memset", mf[64:128, 0:64], 0.0, reads=[mf], writes=[mf])
        return mf
    masks = []
    for d in range(2):
        sg = 1 if d == 0 else -1
        ms = tri_mask(f"ms{d}", sg, True)
        mi = tri_mask(f"mi{d}", sg, False)
        mst = tri_mask(f"mst{d}", sg, True, transpose=True)
        m2 = abuf([128, 256], F32, f"m2_{d}")
        p.op("dve", "tensor_copy", out=m2[:, 0:128], in_=ms[:], reads=[ms], writes=[m2])
        p.op("dve", "tensor_copy", out=m2[:, 128:256], in_=mi[:], reads=[mi], writes=[m2])
        msb = abuf([128, 128], BF16, f"msb{d}")
        mib = abuf([128, 128], BF16, f"mib{d}")
        p.op("dve", "tensor_copy", out=msb[:], in_=ms[:], reads=[ms], writes=[msb])
        p.op("dve", "tensor_copy", out=mib[:], in_=mi[:], reads=[mi], writes=[mib])
        masks.append(dict(m2=m2, mst=mst, msb=msb, mib=mib))
    cind = abuf([128, 2], BF16, "cind")
    p.op("pool", "memset", cind[:], 0.0, writes=[cind])
    p.op("pool", "memset", cind[0:64, 0:1], 1.0, reads=[cind], writes=[cind])
    p.op("pool", "memset", cind[64:128, 1:2], 1.0, reads=[cind], writes=[cind])

    NH = 4

    def alloc_set(sx):
        ld = {n: [abuf([128, 256], F32, sx + f"ld_{n}{i}") for i in range(2)] for n in ("r", "v", "kk", "kd", "b", "lw")}
        lwh = abuf([128, 256], BF16, sx + "lwh")
        lwl = abuf([128, 256], BF16, sx + "lwl")
        Pm = {n: abuf([128, 256], F32, "P" + n) for n in ("p", "inv", "ex")}
        rcb = abuf([128, 256], BF16, sx + "rcb")
        khb = abuf([128, 256], BF16, sx + "khb")
        nbb = abuf([128, 256], BF16, sx + "nbb")
        vb = abuf([128, 256], BF16, sx + "vb")
        Wall = [[abuf([128, 128], BF16, sx + f"W{h}_{i}") for i in range(2)] for h in range(NH)]
        FT = [abuf([64, 4, 128], BF16, sx + f"FT{h}") for h in range(NH)]
        YN = [abuf([128, 256], BF16, sx + f"YN{h}") for h in range(NH)]
        AG = [abuf([128, 256], BF16, sx + f"AG{h}") for h in range(NH)]
        Xm = [[abuf([128, 128], BF16, sx + f"X{h}_{i}") for i in range(2)] for h in range(NH)]
        Ym = [[abuf([128, 128], BF16, sx + f"Y{h}_{i}") for i in range(2)] for h in range(NH)]
        Y0f = [abuf([128, 64], F32, sx + f"Y0f{h}") for h in range(NH)]
        M2b = [abuf([128, 64], BF16, sx + f"M2b{h}") for h in range(NH)]
        M2T = [abuf([64, 128], BF16, sx + f"M2T{h}") for h in range(NH)]
        M3T = [[abuf([64, 64], BF16, sx + f"M3T{h}_{c}") for c in range(2)] for h in range(NH)]
        Z0P = [[abuf([64, 64], F32, sx + f"Z0P{h}_{c}") for c in range(2)] for h in range(NH)]
        Pc = abuf([64, 8], F32, sx + "Pc")
        ytile = [abuf([128, 256], F32, sx + f"ytile{i}") for i in range(2)]
        yf = abuf([128, 256], F32, sx + "yf")
        og = {n: abuf([128, 256], F32, sx + "og_" + n) for n in ("g", "bon", "yc", "sq")}
        st4 = [abuf([128, 4], F32, sx + f"st4_{i}") for i in range(2)]
        return dict(locals())
    BS = [alloc_set("d0_"), alloc_set("d1_")]
    H32 = [[abuf([64, 64], F32, f"H32_{d}_{h}") for h in range(NH)] for d in range(2)]
    Hhi = [[abuf([64, 64], BF16, f"Hhi_{d}_{h}") for h in range(NH)] for d in range(2)]
    Hlo = [[abuf([64, 64], BF16, f"Hlo_{d}_{h}") for h in range(NH)] for d in range(2)]
    for d in range(2):
        for h in range(NH):
            p.op("pool", "memset", H32[d][h][:], 0.0, writes=[H32[d][h]])
            p.op("pool", "memset", Hhi[d][h][:], 0.0, writes=[Hhi[d][h]])
            p.op("pool", "memset", Hlo[d][h][:], 0.0, writes=[Hlo[d][h]])
    outs = []
    tcnt = [0, 0]
    f_ys = [f_y, L["f_y2"]]
    ywr = [[Buf(None, f"ywr{d}_{t}") for t in range(ntiles)] for d in range(2)]
    pcum, pA, pB, pS0, pS1, pYc, pHc = Q
    psm = [pS0, pS1]
    smi = [0]

    def nps():
        smi[0] += 1
        return psm[smi[0] % 2]

    def v4(t):
        return t[:, :].rearrange("p (h d) -> p h d", d=64)

    def b4(s):
        return s[:, 0:4].unsqueeze(2).to_broadcast([128, 4, 64])

    tcount = [0]

    def do_tile(d, t, do_out):
        B = BS[d]
        mk = masks[d]
        rows = slice(t * 128, (t + 1) * 128)
        i = tcnt[d] % 2
        tcnt[d] += 1
        srcs = (("r", f_r), ("v", f_v), ("kk", f_kk), ("kd", f_kd[d]), ("b", f_b[d]), ("lw", f_lw[d]))
        for n, src in srcs:
            p.dma("sp", B["ld"][n][i][:], src[rows, :], reads=[fence], writes=[B["ld"][n][i]])
        lw = B["ld"]["lw"][i]
        p.op("pool", "tensor_copy", out=B["lwh"][:], in_=lw[:], reads=[lw], writes=[B["lwh"]])
        p.op("dve", "tensor_tensor", out=B["lwl"][:], in0=lw[:], in1=B["lwh"][:], op=ALU.subtract, reads=[lw, B["lwh"]], writes=[B["lwl"]])
        for (c0, mm) in ((0, mk["mib"]), (256, mk["msb"])):
            p.op("pe", "matmul", pcum[:, c0:c0 + 256], lhsT=mm[:, :], rhs=B["lwh"][:, :], start=(c0 == 0), stop=False,
                 reads=[mm, B["lwh"]], writes=[pcum])
            p.op("pe", "matmul", pcum[:, c0:c0 + 256], lhsT=mm[:, :], rhs=B["lwl"][:, :], start=False, stop=True,
                 reads=[mm, B["lwl"]], writes=[pcum])
        p.op("act", "activation", out=B["Pm"]["p"][:], in_=pcum[:, 0:256], func=AF.Exp, reads=[pcum], writes=[B["Pm"]["p"]])
        p.op("act", "activation", out=B["Pm"]["inv"][:], in_=pcum[:, 0:256], func=AF.Exp, scale=-1.0, reads=[pcum], writes=[B["Pm"]["inv"]])
        p.op("act", "activation", out=B["Pm"]["ex"][:], in_=pcum[:, 256:512], func=AF.Exp, reads=[pcum], writes=[B["Pm"]["ex"]])
        for h in range(NH):
            for j, lt in enumerate((B["lwh"], B["lwl"])):
                p.op("pe", "matmul", pHc[0:64, 64 + 2 * h:64 + 2 * h + 2], lhsT=lt[:, h * 64:(h + 1) * 64], rhs=cind[:, :],
                     start=(h == 0 and j == 0), stop=(j == 1), reads=[lt, cind], writes=[pHc])
        p.op("act", "activation", out=B["Pc"][:], in_=pHc[0:64, 64:72], func=AF.Exp, reads=[pHc], writes=[B["Pc"]])
        yield
        p.op("dve", "tensor_tensor", out=B["rcb"][:], in0=B["ld"]["r"][i][:], in1=B["Pm"]["p"][:], op=ALU.mult, reads=[B["ld"]["r"][i], B["Pm"]["p"]], writes=[B["rcb"]])
        p.op("pool", "tensor_tensor", out=B["khb"][:], in0=B["ld"]["kd"][i][:], in1=B["Pm"]["inv"][:], op=ALU.mult,
             reads=[B["ld"]["kd"][i], B["Pm"]["inv"]], writes=[B["khb"]])
        p.op("dve", "scalar_tensor_tensor", out=B["nbb"][:], in0=B["ld"]["b"][i][:], scalar=-1.0, in1=B["Pm"]["inv"][:], op0=ALU.mult, op1=ALU.mult,
             reads=[B["ld"]["b"][i], B["Pm"]["inv"]], writes=[B["nbb"]])
        p.op("pool", "tensor_copy", out=B["vb"][:], in_=B["ld"]["v"][i][:], reads=[B["ld"]["v"][i]], writes=[B["vb"]])
        W = [B["Wall"][h][0] for h in range(NH)]
        for h in range(NH):
            hs = slice(h * 64, (h + 1) * 64)
            p.op("dve" if h % 2 else "pool", "tensor_tensor", out=W[h][:, 64:128], in0=B["ld"]["kk"][i][:, hs], in1=B["Pm"]["ex"][:, hs],
                 op=ALU.mult, reads=[B["ld"]["kk"][i], B["Pm"]["ex"]], writes=[W[h]])
        for h in range(NH):
            hs = slice(h * 64, (h + 1) * 64)
            for j, (src, sl) in enumerate(((W[h], slice(64, 128)), (B["rcb"], hs), (B["khb"], hs), (B["nbb"], hs))):
                p.op("pe", "transpose", pT[0:64, j * 128:(j + 1) * 128], src[:, sl], ident[:], reads=[src, ident], writes=[pT])
            p.op("act", "copy", out=B["FT"][h][:, :, :], in_=pT[0:64, 0:512].rearrange("p (j n) -> p j n", n=128), reads=[pT], writes=[B["FT"][h]])
        yield
        X = [B["Xm"][h][0] for h in range(NH)]
        Y = [None] * NH
        for h in range(NH):
            hs = slice(h * 64, (h + 1) * 64)
            rhs2 = B["FT"][h][:, 0:2, :].rearrange("p j n -> p (j n)")
            p.op("pe", "matmul", pA[:, 0:256], lhsT=B["FT"][h][:, 3, :], rhs=rhs2, start=True, stop=True, reads=[B["FT"][h]], writes=[pA])
            p.op("dve", "tensor_tensor", out=B["YN"][h][:], in0=pA[:, 0:256], in1=mk["m2"][:], op=ALU.mult, reads=[pA, mk["m2"]], writes=[B["YN"][h]])
            p.op("pe", "matmul", pB[:, 0:256], lhsT=B["FT"][h][:, 2, :], rhs=rhs2, start=True, stop=True, reads=[B["FT"][h]], writes=[pB])
            p.op("dve", "tensor_tensor", out=B["AG"][h][:], in0=pB[:, 0:256], in1=mk["m2"][:], op=ALU.mult, reads=[pB, mk["m2"]], writes=[B["AG"][h]])
            ps = nps()
            p.op("pe", "matmul", ps[:, 0:128], lhsT=B["FT"][h][:, 0, :], rhs=B["FT"][h][:, 3, :], start=True, stop=True, reads=[B["FT"][h]], writes=[ps])
            p.op("dve", "tensor_tensor", out=X[h][:], in0=ps[:, 0:128], in1=mk["mst"][:], op=ALU.mult, reads=[ps, mk["mst"]], writes=[X[h]])
            ps = nps()
            p.op("pe", "matmul", ps[:, 0:64], lhsT=B["AG"][h][:, 0:128], rhs=B["vb"][:, hs], start=True, stop=True, reads=[B["AG"][h], B["vb"]], writes=[ps])
            p.op("act", "copy", out=W[h][:, 0:64], in_=ps[:, 0:64], reads=[ps], writes=[W[h]])
        Ycur = [(B["YN"][h], slice(0, 128)) for h in range(NH)]
        for j in range(6):
            yield
            for h in range(NH):
                yb, ysl = Ycur[h]
                wi, wo_ = B["Wall"][h][j % 2], B["Wall"][h][(j + 1) % 2]
                ps = nps()
                p.op("pe", "matmul", ps[:, 0:128], lhsT=yb[:, ysl], rhs=wi[:, :], start=True, stop=False, reads=[yb, wi], writes=[ps])
                p.op("pe", "matmul", ps[:, 0:128], lhsT=ident[:, :], rhs=wi[:, :], start=False, stop=True, reads=[ident, wi], writes=[ps])
                p.op("act" if h % 2 else "dve", "copy" if h % 2 else "tensor_copy", out=wo_[:], in_=ps[:, 0:128], reads=[ps], writes=[wo_])
            if j < 5:
                yield
                for h in range(NH):
                    yb, ysl = Ycur[h]
                    xi = B["Xm"][h][j % 2]
                    xo, yo = B["Xm"][h][(j + 1) % 2], B["Ym"][h][(j + 1) % 2]
                    ps = nps()
                    p.op("pe", "matmul", ps[:, 0:128], lhsT=xi[:, :], rhs=yb[:, ysl], start=True, stop=True, reads=[xi, yb], writes=[ps])
                    p.op("act", "copy", out=yo[:], in_=ps[:, 0:128], reads=[ps], writes=[yo])
                    ps = nps()
                    p.op("pe", "matmul", ps[:, 0:128], lhsT=yb[:, ysl], rhs=xi[:, :], start=True, stop=True, reads=[xi, yb], writes=[ps])
                    p.op("dve", "tensor_copy", out=xo[:], in_=ps[:, 0:128], reads=[ps], writes=[xo])
                    Ycur[h] = (yo, slice(0, 128))
        yield
        TW = [B["Wall"][h][0] for h in range(NH)]
        for h in range(NH):
            hs = slice(h * 64, (h + 1) * 64)
            ps = nps()
            p.op("pe", "matmul", ps[:, 0:128], lhsT=B["YN"][h][:, 128:256], rhs=TW[h][:, :], start=True, stop=False, reads=[B["YN"][h], TW[h]], writes=[ps])
            p.op("pe", "matmul", ps[:, 0:64], lhsT=B["AG"][h][:, 128:256], rhs=B["vb"][:, hs], start=False, stop=False, reads=[B["AG"][h], B["vb"]], writes=[ps])
            p.op("pe", "matmul", ps[:, 64:128], lhsT=ident[:, :], rhs=B["rcb"][:, hs], start=False, stop=True, reads=[ident, B["rcb"]], writes=[ps])
            p.op("act", "copy", out=B["Y0f"][h][:], in_=ps[:, 0:64], reads=[ps], writes=[B["Y0f"][h]])
            p.op("dve", "tensor_copy", out=B["M2b"][h][:], in_=ps[:, 64:128], reads=[ps], writes=[B["M2b"][h]])
            p.op("pe", "transpose", pT[0:64, 0:128], B["M2b"][h][:, :], ident[:], reads=[B["M2b"][h], ident], writes=[pT])
            p.op("act", "copy", out=B["M2T"][h][:], in_=pT[0:64, 0:128], reads=[pT], writes=[B["M2T"][h]])
            for c in range(2):
                cr = slice(c * 64, (c + 1) * 64)
                ps = nps()
                p.op("pe", "matmul", ps[0:64, 0:64], lhsT=TW[h][cr, 64:128], rhs=B["nbb"][cr, hs], start=True, stop=True,
                     reads=[TW[h], B["nbb"]], writes=[ps])
                p.op("dve", "tensor_tensor", out=B["M3T"][h][c][:], in0=ps[0:64, 0:64], in1=identf[0:64, 0:64], op=ALU.add,
                     reads=[ps, identf], writes=[B["M3T"][h][c]])
                ps = nps()
                p.op("pe", "matmul", ps[0:64, 0:64], lhsT=B["khb"][cr, hs], rhs=B["vb"][cr, hs], start=True, stop=False, reads=[B["khb"], B["vb"]], writes=[ps])
                p.op("pe", "matmul", ps[0:64, 0:64], lhsT=B["nbb"][cr, hs], rhs=TW[h][cr, 0:64], start=False, stop=True,
                     reads=[B["nbb"], TW[h]], writes=[ps])
                p.op("dve", "tensor_scalar", out=B["Z0P"][h][c][:], in0=ps[0:64, 0:64], scalar1=B["Pc"][:, 2 * h + c:2 * h + c + 1], scalar2=None,
                     op0=ALU.mult, reads=[ps, B["Pc"]], writes=[B["Z0P"][h][c]])
        yield
        yt = B["ytile"][i]
        for c in ((0, 1) if d == 0 else (1, 0)):
            cr = slice(c * 64, (c + 1) * 64)
            for h in range(NH):
                hs = slice(h * 64, (h + 1) * 64)
                hh, hl, h32 = Hhi[d][h], Hlo[d][h], H32[d][h]
                p.op("pe", "matmul", pYc[cr, hs], lhsT=B["M2T"][h][:, cr], rhs=hh[:, :], start=True, stop=False, reads=[B["M2T"][h], hh], writes=[pYc])
                p.op("pe", "matmul", pYc[cr, hs], lhsT=B["M2T"][h][:, cr], rhs=hl[:, :], start=False, stop=True, reads=[B["M2T"][h], hl], writes=[pYc])
                p.op("pe", "matmul", pHc[0:64, 0:64], lhsT=B["M3T"][h][c][:, :], rhs=hh[:, :], start=True, stop=False, reads=[B["M3T"][h][c], hh], writes=[pHc])
                p.op("pe", "matmul", pHc[0:64, 0:64], lhsT=B["M3T"][h][c][:, :], rhs=hl[:, :], start=False, stop=True, reads=[B["M3T"][h][c], hl], writes=[pHc])
                p.op("dve", "tensor_tensor", out=yt[cr, hs], in0=pYc[cr, hs], in1=B["Y0f"][h][cr, :], op=ALU.add, reads=[pYc, B["Y0f"][h]], writes=[yt])
                p.op("dve", "scalar_tensor_tensor", out=h32[:], in0=pHc[0:64, 0:64], scalar=B["Pc"][:, 2 * h + c:2 * h + c + 1], in1=B["Z0P"][h][c][:],
                     op0=ALU.mult, op1=ALU.add, reads=[pHc, B["Pc"], B["Z0P"][h][c]], writes=[h32])
                p.op("act", "copy", out=hh[:], in_=h32[:], reads=[h32], writes=[hh])
                p.op("dve", "tensor_tensor", out=hl[:], in0=h32[:], in1=hh[:], op=ALU.subtract, reads=[h32, hh], writes=[hl])
        yield
        if not do_out:
            p.dma("sp", f_ys[d][rows, :], yt[:], reads=[yt], writes=[ywr[d][t]])
        else:
            p.dma("sp", B["yf"][:], f_ys[1 - d][rows, :], reads=[ywr[1 - d][t]], writes=[B["yf"]])
            p.dma("sp", B["og"]["g"][:], f_g[rows, :], reads=[fence], writes=[B["og"]["g"]])
            p.dma("sp", B["og"]["bon"][:], f_bon[rows, :], reads=[fence], writes=[B["og"]["bon"]])
            p.op("pool", "tensor_tensor", out=yt[:], in0=yt[:], in1=B["yf"][:], op=ALU.add, reads=[yt, B["yf"]], writes=[yt])
            p.op("dve", "tensor_reduce", out=B["st4"][0][:], in_=v4(yt), axis=AX.X, op=ALU.add, reads=[yt], writes=[B["st4"][0]])
            p.op("dve", "tensor_scalar", out=B["st4"][0][:], in0=B["st4"][0][:], scalar1=1.0 / 64, scalar2=None, op0=ALU.mult, reads=[B["st4"][0]], writes=[B["st4"][0]])
            p.op("dve", "tensor_tensor", out=v4(B["og"]["yc"]), in0=v4(yt), in1=b4(B["st4"][0]), op=ALU.subtract, reads=[yt, B["st4"][0]], writes=[B["og"]["yc"]])
            p.op("act", "activation", out=B["og"]["sq"][:], in_=B["og"]["yc"][:], func=AF.Square, reads=[B["og"]["yc"]], writes=[B["og"]["sq"]])
            p.op("dve", "tensor_reduce", out=B["st4"][1][:], in_=v4(B["og"]["sq"]), axis=AX.X, op=ALU.add, reads=[B["og"]["sq"]], writes=[B["st4"][1]])
            p.op("act", "activation", out=B["st4"][1][:], in_=B["st4"][1][:], func=AF.Sqrt, bias=eps_ln[:, 0:1], scale=1.0 / 64,
                 reads=[B["st4"][1], eps_ln], writes=[B["st4"][1]])
            p.op("dve", "reciprocal", out=B["st4"][1][:], in_=B["st4"][1][:], reads=[B["st4"][1]], writes=[B["st4"][1]])
            p.op("dve", "tensor_tensor", out=v4(B["og"]["yc"]), in0=v4(B["og"]["yc"]), in1=b4(B["st4"][1]), op=ALU.mult, reads=[B["og"]["yc"], B["st4"][1]], writes=[B["og"]["yc"]])
            p.op("pool", "tensor_tensor", out=B["og"]["yc"][:], in0=B["og"]["yc"][:], in1=lnwb[:], op=ALU.mult, reads=[B["og"]["yc"], lnwb], writes=[B["og"]["yc"]])
            p.op("pool", "tensor_tensor", out=B["og"]["yc"][:], in0=B["og"]["yc"][:], in1=lnbb[:], op=ALU.add, reads=[B["og"]["yc"], lnbb], writes=[B["og"]["yc"]])
            p.op("pool", "tensor_tensor", out=B["og"]["yc"][:], in0=B["og"]["yc"][:], in1=B["og"]["bon"][:], op=ALU.add, reads=[B["og"]["yc"], B["og"]["bon"]], writes=[B["og"]["yc"]])
            p.op("pool", "tensor_tensor", out=B["og"]["sq"][:], in0=B["og"]["yc"][:], in1=B["og"]["g"][:], op=ALU.mult, reads=[B["og"]["yc"], B["og"]["g"]], writes=[B["og"]["sq"]])
            outs.append(p.dma("sp", o_out[rows, :], B["og"]["sq"][:], reads=[B["og"]["sq"]]))

    nlat = ntiles - 2
    order = [[0, 1] + [2 + i for i in range(nlat)], [1, 0] + [2 + i for i in range(nlat - 1, -1, -1)]]
    pos = [{t: i for i, t in enumerate(order[d])} for d in range(2)]
    for i in range(ntiles):
        gens = []
        for d in range(2):
            t = order[d][i]
            gens.append(do_tile(d, t, pos[d][t] > pos[1 - d][t]))
        alive = True
        while alive:
            alive = False
            for g_ in gens:
                try:
                    next(g_)
                    alive = True
                except StopIteration:
                    pass
    p.emit(final_waits=outs)


def _shift_rows(a, k):
    out = np.zeros_like(a)
    if k == 1:
        out[1:] = a[:-1]
    else:
        out[:-1] = a[1:]
    return out


def rwkv_inputs(x_lat, x_ctx, c, c_ctx, mod_w, mod_b, n1g, P):
    maps = []
    for ci in range(8):
        b, hg = ci // 4, ci % 4
        cs = slice(hg * 256, (hg + 1) * 256)
        xa = np.concatenate([x_ctx[b], x_lat[b]], 0)
        xp = np.concatenate([_shift_rows(x_ctx[b], 1), _shift_rows(x_lat[b], 1)], 0)
        xn = np.concatenate([_shift_rows(x_ctx[b], -1), _shift_rows(x_lat[b], -1)], 0)
        mpn = np.ones((RT_TOK, 2), np.float32)
        mpn[0, 0] = 0
        mpn[256, 0] = 0
        mpn[255, 1] = 0
        mpn[RT_TOK - 1, 1] = 0
        vecs = np.stack([P["w0"][0][cs], P["w0"][1][cs], P["a0"][0][cs], P["a0"][1][cs], P["k_k"][cs], P["k_a"][cs],
                         P["r_k"].reshape(-1)[cs], P["ln_w"][cs], P["ln_b"][cs]], 0)
        maps.append(dict(
            xa=np.ascontiguousarray(xa), xp=np.ascontiguousarray(xp), xn=np.ascontiguousarray(xn), mpn=mpn,
            cnd=_cnd(c[b], c_ctx), modw=np.ascontiguousarray(mod_w[:, 0:2048]), modb=np.ascontiguousarray(mod_b[None, 0:2048]),
            n1g=np.ascontiguousarray(n1g[None, :]), mu=np.ascontiguousarray(P["mu"]),
            w_r=np.ascontiguousarray(P["w_r"][:, cs]), w_k=np.ascontiguousarray(P["w_k"][:, cs]),
            w_v=np.ascontiguousarray(P["w_v"][:, cs]), g1=np.ascontiguousarray(P["g1"]),
            g2=np.ascontiguousarray(P["g2"][:, cs]),
            w1=np.ascontiguousarray(np.concatenate([P["w1"][0], P["w1"][1]], 1)),
            w2=np.ascontiguousarray(np.concatenate([P["w2"][0][:, cs], P["w2"][1][:, cs]], 1)),
            a1=np.ascontiguousarray(np.concatenate([P["a1"][0], P["a1"][1]], 1)),
            a2=np.ascontiguousarray(np.concatenate([P["a2"][0][:, cs], P["a2"][1][:, cs]], 1)),
            vecs=np.ascontiguousarray(vecs.astype(np.float32))))
    return maps


_PROGS = {}


def _prog(key, fn):
    if key not in _PROGS:
        _PROGS[key] = fn()
    return _PROGS[key]


def _run(nc, maps):
    return run_bass_kernel_spmd(nc, maps, core_ids=list(range(8))).results


def kernel(x, c, ctx, c_ctx, mod_w, mod_b, norm1_g, norm2_g, a_w_qkv, a_w_o, a_q_gain, a_k_gain,
           b_mu, b_w_r, b_w_k, b_w_v, b_w_o, b_decay_w0, b_decay_w1, b_decay_w2, b_iclr_a0, b_iclr_a1,
           b_iclr_a2, b_gate_g1, b_gate_g2, b_k_k, b_k_a, b_r_k, b_ln_w, b_ln_b, c_w_qkv, c_w_o, c_sink,
           moe_w_group, moe_b_group, moe_w_expert, moe_b_expert, moe_w1, moe_w3, moe_w2, final_g):
    f32 = lambda a: np.ascontiguousarray(np.asarray(a, dtype=np.float32))
    x_lat, x_ctx = f32(x), f32(ctx)
    c, c_ctx = f32(c), f32(c_ctx)
    mod_w, mod_b = f32(mod_w), f32(mod_b)
    y_last = None
    for l in range(4):
        kind, j = l % 3, l // 3
        mw, mb = mod_w[l], mod_b[l]
        n1g, n2g = f32(norm1_g[l]), f32(norm2_g[l])
        if kind == 0:
            nc = _prog("attn_d", lambda: build_attn(True))
            maps = attn_inputs(True, x_lat, x_ctx, c, c_ctx, mw, mb, n1g, f32(a_w_qkv[j]), f32(a_q_gain[j]),
                               f32(a_k_gain[j]), np.zeros(16, np.float32))
            res = _run(nc, maps)
            wo = f32(a_w_o[j])
        elif kind == 2:
            nc = _prog("attn_w", lambda: build_attn(False))
            ones = np.ones(64, np.float32)
            maps = attn_inputs(False, x_lat, x_ctx, c, c_ctx, mw, mb, n1g, f32(c_w_qkv[j]), ones, ones, f32(c_sink[j]))
            res = _run(nc, maps)
            wo = f32(c_w_o[j])
        else:
            nc = _prog("rwkv", lambda: build_rwkv())
            P = dict(mu=f32(b_mu[j]), w_r=f32(b_w_r[j]), w_k=f32(b_w_k[j]), w_v=f32(b_w_v[j]), w0=f32(b_decay_w0[j]),
                     w1=f32(b_decay_w1[j]), w2=f32(b_decay_w2[j]), a0=f32(b_iclr_a0[j]), a1=f32(b_iclr_a1[j]),
                     a2=f32(b_iclr_a2[j]), g1=f32(b_gate_g1[j]), g2=f32(b_gate_g2[j]), k_k=f32(b_k_k[j]), k_a=f32(b_k_a[j]),
                     r_k=f32(b_r_k[j]), ln_w=f32(b_ln_w[j]), ln_b=f32(b_ln_b[j]))
            maps = rwkv_inputs(x_lat, x_ctx, c, c_ctx, mw, mb, n1g, P)
            res = _run(nc, maps)
            wo = f32(b_w_o[j])
        o_lat = np.zeros((2, 8192, 1024), np.float32)
        o_ctx = np.zeros((2, 256, 1024), np.float32)
        if kind == 1:
            for ci in range(8):
                b, hg = ci // 4, ci % 4
                o = res[ci]["o_out"]
                o_ctx[b][:, hg * 256:(hg + 1) * 256] = o[:256]
                o_lat[b][:, hg * 256:(hg + 1) * 256] = o[256:]
        else:
            for ci in range(8):
                b, qi = ci // 4, ci % 4
                o_lat[b, qi * 2048:(qi + 1) * 2048] = res[ci]["o_lat"]
                if qi == 0:
                    o_ctx[b] = res[ci]["o_ctx"]
        del res
        nc1 = _prog("post1", build_post1)
        res1 = _run(nc1, post1_inputs(x_lat, x_ctx, o_lat, o_ctx, c, c_ctx, mw, mb, n2g, wo, f32(moe_w_group[l]),
                                      f32(moe_b_group[l]), f32(moe_w_expert[l]), f32(moe_b_expert[l])))
        nc2 = _prog("post2", build_post2)
        res2 = _run(nc2, post2_inputs(res1, c, c_ctx, mw, mb, f32(final_g), f32(moe_w1[l]), f32(moe_w3[l]), f32(moe_w2[l])))
        x_lat, x_ctx = _unrows(res2, "x_out")
        if l == 3:
            y_last, _ = _unrows(res2, "y_fin")
    return y_last
```
